# Optimizing a Trainium2 kernel written in Bass

```python
import math
import jax, jax.numpy as jnp
from jax import lax
import numpy as np

D_MODEL = 1024
BATCH = 8
SEQ = 4096
DEPTH = 1

GRID_W = 64
CTX_LEN = 256

N_FOURIER_GROUPS = 4
FOURIER_GROUP = 128
D_FOURIER = N_FOURIER_GROUPS * FOURIER_GROUP

D_RNN = D_MODEL
N_RNN_HEADS = 8
RNN_HEAD = D_RNN // N_RNN_HEADS
CONV_W = 4
CONV_LEFT = 2
LRU_C = 8.0
N_DIRS = 2

COL_F = D_FOURIER
COL_X = COL_F + D_RNN
COL_G = COL_X + D_RNN
COL_A = COL_G + D_MODEL
D_IN = COL_A + D_MODEL

N_EXPERTS = 16
CAPACITY_FACTOR = 2
D_EXPERT = 1536

N_MOD = 6
EPS = 1e-6
POS_MAX_PERIOD = 10000.0

kernel_name = 'hybrid_fourier_rglru_ec_moe_dit'


def rmsnorm(x, g):
    xf = x.astype(jnp.float32)
    y = xf * lax.rsqrt(jnp.mean(xf * xf, axis=-1, keepdims=True) + EPS)
    return (y * g.astype(jnp.float32)).astype(x.dtype)


def ada_params(cvec, w_ada, b_ada):
    m = jnp.einsum('nd,de->ne', jax.nn.silu(cvec), w_ada) + b_ada
    return [t[:, None, :] for t in jnp.split(m, N_MOD, axis=-1)]


def modulate(h, shift, scale):
    return h * (1.0 + scale) + shift


def grid_pos_embed(n_tokens, dtype):
    rows = n_tokens // GRID_W
    quarter = D_MODEL // 4
    freqs = jnp.exp(-math.log(POS_MAX_PERIOD) * jnp.arange(quarter, dtype=jnp.float32) / quarter)
    ang_r = jnp.arange(rows, dtype=jnp.float32)[:, None] * freqs
    ang_c = jnp.arange(GRID_W, dtype=jnp.float32)[:, None] * freqs
    emb_r = jnp.concatenate([jnp.sin(ang_r), jnp.cos(ang_r)], axis=-1)
    emb_c = jnp.concatenate([jnp.sin(ang_c), jnp.cos(ang_c)], axis=-1)
    emb = jnp.concatenate([
        jnp.broadcast_to(emb_r[:, None, :], (rows, GRID_W, D_MODEL // 2)),
        jnp.broadcast_to(emb_c[None, :, :], (rows, GRID_W, D_MODEL // 2)),
    ], axis=-1)
    return emb.reshape(rows * GRID_W, D_MODEL).astype(dtype)


def fourier_mix(u):
    B, T, _ = u.shape
    g = u.reshape(B, T, N_FOURIER_GROUPS, FOURIER_GROUP).astype(jnp.float32)
    f = jnp.fft.fft2(g, axes=(1, 3), norm='ortho').real
    return f.reshape(B, T, D_FOURIER).astype(u.dtype)


def dwconv(u, w, b):
    T = u.shape[1]
    up = jnp.pad(u, ((0, 0), (CONV_LEFT, CONV_W - 1 - CONV_LEFT), (0, 0)))
    out = b
    for k in range(CONV_W):
        out = out + w[k] * up[:, k:k + T]
    return out


def lru_coeffs(u, lam, wa, ba, wi, bi):
    B, T, _ = u.shape
    uh = u.reshape(B, T, N_RNN_HEADS, RNN_HEAD)
    r = jax.nn.sigmoid(jnp.einsum('bthi,hij->bthj', uh, wa).reshape(B, T, D_RNN) + ba)
    i = jax.nn.sigmoid(jnp.einsum('bthi,hij->bthj', uh, wi).reshape(B, T, D_RNN) + bi)
    log_a = -LRU_C * r.astype(jnp.float32) * jax.nn.softplus(-lam.astype(jnp.float32))
    a = jnp.exp(log_a)
    b = jnp.sqrt(-jnp.expm1(2.0 * log_a)) * (i * u).astype(jnp.float32)
    return a, b


def _combine(left, right):
    a_l, b_l = left
    a_r, b_r = right
    return a_l * a_r, a_r * b_l + b_r


def linear_scan(a, b, h0):
    b = b.at[:, 0].add(a[:, 0] * h0)
    _, h = lax.associative_scan(_combine, (a, b), axis=1)
    return h


def rglru_bidir(u_lat, u_ctx, lam, wa, ba, wi, bi, ctx_out):
    y_lat = None
    y_ctx = None
    h0 = jnp.zeros((u_ctx.shape[0], D_RNN), jnp.float32)
    for d in range(N_DIRS):
        a_c, b_c = lru_coeffs(u_ctx, lam[d], wa[d], ba[d], wi[d], bi[d])
        a_l, b_l = lru_coeffs(u_lat, lam[d], wa[d], ba[d], wi[d], bi[d])
        if d == 1:
            a_c, b_c, a_l, b_l = [jnp.flip(t, axis=1) for t in (a_c, b_c, a_l, b_l)]
        h_c = linear_scan(a_c, b_c, h0)
        h_l = linear_scan(a_l, b_l, h_c[:, -1])
        if d == 1:
            h_c = jnp.flip(h_c, axis=1)
            h_l = jnp.flip(h_l, axis=1)
        y_lat = h_l if y_lat is None else y_lat + h_l
        if ctx_out:
            y_ctx = h_c if y_ctx is None else y_ctx + h_c
    y_lat = y_lat.astype(u_lat.dtype)
    if ctx_out:
        y_ctx = y_ctx.astype(u_ctx.dtype)
    return y_lat, y_ctx


def merge_branches(z, r, w_four, w_lru, w_out):
    y_four = jnp.einsum('btf,fd->btd', fourier_mix(z[..., :COL_F]), w_four)
    y_rnn = jnp.einsum('btr,rd->btd', jax.nn.gelu(z[..., COL_X:COL_G]) * r, w_lru)
    g_four = jax.nn.sigmoid(z[..., COL_G:COL_A])
    g_rnn = jax.nn.sigmoid(z[..., COL_A:])
    return jnp.einsum('btd,de->bte', g_four * y_four + g_rnn * y_rnn, w_out)


def token_mixer(h_lat, h_ctx, w_in, w_four, conv_w, conv_b, lam, wa, ba, wi, bi, w_lru, w_out, ctx_out):
    z_lat = jnp.einsum('btd,de->bte', h_lat, w_in)
    if ctx_out:
        z_ctx = jnp.einsum('btd,de->bte', h_ctx, w_in)
        x_ctx = z_ctx[..., COL_F:COL_X]
    else:
        z_ctx = None
        x_ctx = jnp.einsum('btd,de->bte', h_ctx, w_in[:, COL_F:COL_X])
    u_lat = dwconv(z_lat[..., COL_F:COL_X], conv_w, conv_b)
    u_ctx = dwconv(x_ctx, conv_w, conv_b)
    r_lat, r_ctx = rglru_bidir(u_lat, u_ctx, lam, wa, ba, wi, bi, ctx_out)
    y_lat = merge_branches(z_lat, r_lat, w_four, w_lru, w_out)
    y_ctx = merge_branches(z_ctx, r_ctx, w_four, w_lru, w_out) if ctx_out else None
    return y_lat, y_ctx


def expert_choice_ffn(h, w_router, w_gate, w_up, w_down):
    B, T, D = h.shape
    cap = CAPACITY_FACTOR * T // N_EXPERTS
    scores = jax.nn.softmax(jnp.einsum('btd,de->bte', h, w_router).astype(jnp.float32), axis=-1)
    g, idx = lax.top_k(jnp.swapaxes(scores, 1, 2), cap)
    xg = jax.vmap(lambda hb, ib: hb[ib])(h, idx)
    hid = jax.nn.silu(jnp.einsum('becd,edf->becf', xg, w_gate)) * jnp.einsum('becd,edf->becf', xg, w_up)
    out = jnp.einsum('becf,efd->becd', hid, w_down) * g[..., None].astype(h.dtype)
    return jax.vmap(lambda ib, ob: jnp.zeros((T, D), h.dtype).at[ib.reshape(-1)].add(ob.reshape(-1, D)))(idx, out)


def setup_inputs(seed: int = 0) -> dict:
    key = jax.random.key(seed)
    ks = jax.random.split(key, 24)
    f32 = jnp.float32

    def nrm(k, shape, fan_in):
        return jax.random.normal(k, shape, f32) * (fan_in ** -0.5)

    u = jax.random.uniform(ks[10], (DEPTH, N_DIRS, D_RNN), f32, 0.9, 0.999)
    s = u ** (1.0 / LRU_C)
    lru_lambda = jnp.log(s) - jnp.log1p(-s)
    return {
        'x': jax.random.normal(ks[0], (BATCH, SEQ, D_MODEL), f32),
        'c': jax.random.normal(ks[1], (BATCH, D_MODEL), f32),
        'ctx': jax.random.normal(ks[2], (BATCH, CTX_LEN, D_MODEL), f32),
        'c_ctx': jax.random.normal(ks[3], (D_MODEL,), f32),
        'w_ada': nrm(ks[4], (DEPTH, D_MODEL, N_MOD * D_MODEL), D_MODEL),
        'b_ada': 0.01 * jax.random.normal(ks[5], (DEPTH, N_MOD * D_MODEL), f32),
        'norm1_g': 1.0 + 0.02 * jax.random.normal(ks[6], (DEPTH, D_MODEL), f32),
        'norm2_g': 1.0 + 0.02 * jax.random.normal(ks[7], (DEPTH, D_MODEL), f32),
        'w_in': nrm(ks[8], (DEPTH, D_MODEL, D_IN), D_MODEL),
        'w_four': nrm(ks[9], (DEPTH, D_FOURIER, D_MODEL), D_FOURIER),
        'conv_w': nrm(ks[11], (DEPTH, CONV_W, D_RNN), CONV_W),
        'conv_b': 0.01 * jax.random.normal(ks[12], (DEPTH, D_RNN), f32),
        'lru_lambda': lru_lambda,
        'lru_wa': nrm(ks[13], (DEPTH, N_DIRS, N_RNN_HEADS, RNN_HEAD, RNN_HEAD), RNN_HEAD),
        'lru_ba': 0.01 * jax.random.normal(ks[14], (DEPTH, N_DIRS, D_RNN), f32),
        'lru_wi': nrm(ks[15], (DEPTH, N_DIRS, N_RNN_HEADS, RNN_HEAD, RNN_HEAD), RNN_HEAD),
        'lru_bi': 0.01 * jax.random.normal(ks[16], (DEPTH, N_DIRS, D_RNN), f32),
        'w_lru': nrm(ks[17], (DEPTH, D_RNN, D_MODEL), D_RNN),
        'w_out': nrm(ks[18], (DEPTH, D_MODEL, D_MODEL), D_MODEL),
        'w_router': nrm(ks[19], (DEPTH, D_MODEL, N_EXPERTS), D_MODEL),
        'w_gate_e': nrm(ks[20], (DEPTH, N_EXPERTS, D_MODEL, D_EXPERT), D_MODEL),
        'w_up_e': nrm(ks[21], (DEPTH, N_EXPERTS, D_MODEL, D_EXPERT), D_MODEL),
        'w_down_e': nrm(ks[22], (DEPTH, N_EXPERTS, D_EXPERT, D_MODEL), D_EXPERT),
        'final_g': 1.0 + 0.02 * jax.random.normal(ks[23], (D_MODEL,), f32),
    }


def reference(x, c, ctx, c_ctx, w_ada, b_ada, norm1_g, norm2_g, w_in, w_four, conv_w, conv_b,
              lru_lambda, lru_wa, lru_ba, lru_wi, lru_bi, w_lru, w_out, w_router,
              w_gate_e, w_up_e, w_down_e, final_g):
    x = x + grid_pos_embed(x.shape[1], x.dtype)[None]
    for l in range(DEPTH):
        ctx_out = l < DEPTH - 1
        sh1, sc1, g1, sh2, sc2, g2 = ada_params(c, w_ada[l], b_ada[l])
        csh1, csc1, cg1, csh2, csc2, cg2 = ada_params(c_ctx[None], w_ada[l], b_ada[l])

        h_lat = modulate(rmsnorm(x, norm1_g[l]), sh1, sc1)
        h_ctx = modulate(rmsnorm(ctx, norm1_g[l]), csh1, csc1)
        y_lat, y_ctx = token_mixer(h_lat, h_ctx, w_in[l], w_four[l], conv_w[l], conv_b[l],
                                   lru_lambda[l], lru_wa[l], lru_ba[l], lru_wi[l], lru_bi[l],
                                   w_lru[l], w_out[l], ctx_out)
        x = x + g1 * y_lat
        if ctx_out:
            ctx = ctx + cg1 * y_ctx

        h_lat = modulate(rmsnorm(x, norm2_g[l]), sh2, sc2)
        x = x + g2 * expert_choice_ffn(h_lat, w_router[l], w_gate_e[l], w_up_e[l], w_down_e[l])
        if ctx_out:
            h_ctx = modulate(rmsnorm(ctx, norm2_g[l]), csh2, csc2)
            ctx = ctx + cg2 * expert_choice_ffn(h_ctx, w_router[l], w_gate_e[l], w_up_e[l], w_down_e[l])
    return rmsnorm(x, final_g)
```

```python
import contextlib
import math
import numpy as np
import ml_dtypes
import concourse.bass as bass
import concourse.mybir as mybir
from concourse.bass_utils import run_bass_kernel_spmd

F32 = mybir.dt.float32
BF = mybir.dt.bfloat16
I32 = mybir.dt.int32
ALU = mybir.AluOpType
AF = mybir.ActivationFunctionType
AX = mybir.AxisListType

T = 4096
TC = 256
TT = T + TC
D = 1024
NE = 16
CAP = 512
DF = 1536
EPS = 1e-6
DEBUG = False
STOP_AFTER = 99
CUT = 0
LAGS = [0, 1, 3, 4]


class Prog:
    def __init__(self, nc, es):
        self.nc = nc
        self.q = {k: [] for k in ['pe', 'act', 'dve', 'pool', 'sp']}
        self.sem = {k: es.enter_context(nc.semaphore('s_' + k)) for k in ['pe', 'act', 'dve', 'pool']}
        self.cnt = {k: 0 for k in self.sem}
        self.dsem = {qn: [es.enter_context(nc.semaphore('d_%s%d' % (qn, i))) for i in range(n)]
                     for qn, n in [('sp', 14), ('pool', 10), ('act', 6)]}
        self.dcnt = {qn: 0 for qn in self.dsem}
        self.dtarget = {}
        self.lastw = {}
        self.readers = {}
        self.waited = {}
        self.semobj = {}

    def _sid(self, s):
        self.semobj[id(s)] = s
        return id(s)

    def op(self, queue, fn, reads=(), writes=(), dma=False):
        deps = set()
        for r in reads:
            if r in self.lastw:
                deps.add(self.lastw[r])
        for w in writes:
            if w in self.lastw:
                deps.add(self.lastw[w])
            for rd in self.readers.get(w, ()):
                deps.add(rd)
        if dma:
            pool = self.dsem[queue]
            i = self.dcnt[queue]
            self.dcnt[queue] += 1
            s = pool[i % len(pool)]
            rnd = i // len(pool)
            ev = (self._sid(s), 16 * (rnd + 1))
            if rnd > 0:
                deps.add((self._sid(s), 16 * rnd))
            inc = (s, 16)
            self.dtarget[self._sid(s)] = 16 * (rnd + 1)
        else:
            self.cnt[queue] += 1
            s = self.sem[queue]
            ev = (self._sid(s), self.cnt[queue])
            inc = (s, 1)
        waits = {}
        for (sid, v) in deps:
            if queue == 'pe' and not dma and sid == id(self.sem['pe']):
                continue
            if self.waited.get((queue, sid), 0) >= v:
                continue
            waits[sid] = max(waits.get(sid, 0), v)
        for sid, v in waits.items():
            self.waited[(queue, sid)] = v
        self.q[queue].append(([(self.semobj[sid], v) for sid, v in waits.items()], fn, inc))
        for r in reads:
            self.readers.setdefault(r, []).append(ev)
        for w in writes:
            self.lastw[w] = ev
            self.readers[w] = []
        return ev

    def barrier(self):
        evs = []
        for k, s in self.sem.items():
            if self.cnt[k] > 0:
                evs.append((self._sid(s), self.cnt[k]))
        for sid, v in self.dtarget.items():
            evs.append((sid, v))
        for queue in self.q:
            waits = []
            for (sid, v) in evs:
                if self.waited.get((queue, sid), 0) >= v:
                    continue
                self.waited[(queue, sid)] = v
                waits.append((self.semobj[sid], v))
            if waits:
                self.q[queue].append((waits, None, None))

    def emit(self, e, queue):
        for waits, fn, inc in self.q[queue]:
            for s, v in waits:
                e.wait_ge(s, v)
            if fn is not None:
                ins = fn(e)
                ins.then_inc(inc[0], inc[1])


class SBAlloc:
    def __init__(self, nc, base=20736, end=229376):
        self.nc = nc
        self.cur = base
        self.end = end
        self.n = 0

    def alloc(self, shape, dtype):
        sz = 1
        for s in shape[1:]:
            sz *= s
        sz *= {F32: 4, BF: 2, I32: 4}[dtype]
        sz = (sz + 63) // 64 * 64
        assert self.cur + sz <= self.end, "SBUF overflow %d" % (self.cur + sz - self.end)
        self.n += 1
        t = self.nc.alloc_sbuf_tensor_at('t%d' % self.n, list(shape), dtype, offset=self.cur)
        self.cur += sz
        return t

    def mark(self):
        return self.cur

    def reset(self, m):
        self.cur = m


def build_program(debug=False, stop_after=99):
    nc = bass.Bass("TRN2", target_bir_lowering=False)
    es = contextlib.ExitStack()

    def din(name, shape, dt=F32):
        return nc.dram_tensor(name, list(shape), dt, kind="ExternalInput").ap()

    def dscr(name, shape, dt=F32):
        kind = "ExternalOutput" if debug else "Internal"
        return nc.dram_tensor(name, list(shape), dt, kind=kind).ap()

    x_d = din("x", [T, D]); ctx_d = din("ctx", [TC, D]); pos_d = din("pos", [T, D])
    cvec_d = din("cvec", [128, 16]); wada_d = din("w_ada", [D, 6 * D]); bada_d = din("b_ada_fm", [128, 48])
    n1g_d = din("n1g_fm", [128, 8]); n2g_d = din("n2g_fm", [128, 8]); fg_d = din("final_g", [1, D])
    win_d = din("w_in", [D, 4608]); wfour_d = din("w_four", [512, D]); wlru_d = din("w_lru", [D, D]); wout_d = din("w_out", [D, D])
    cw_d = din("conv_w_fm", [128, 32]); cb_d = din("conv_b_fm", [128, 8]); lam_d = din("lam_fm", [128, 16])
    ba_d = din("ba_fm", [128, 16]); bi_d = din("bi_fm", [128, 16])
    wa_d = din("lru_wa", [16, 128, 128]); wi_d = din("lru_wi", [16, 128, 128])
    wr_d = din("w_router_fm", [128, 128])
    wg_d = din("w_gate_e", [NE, D, DF]); wu_d = din("w_up_e", [NE, D, DF]); wd_d = din("w_down_e", [NE, DF, D])
    cs128_d = din("cs128", [128, 256], BF); e1_d = din("e1", [128, 32 * 512], BF); e2_d = din("e2", [128, 256], BF)
    identf_d = din("identf", [128, 128]); identb_d = din("identb", [128, 128], BF)
    iotac_d = din("iota_c", [128, 512]); cval_d = din("cval", [128, 4]); jcol_d = din("jcol", [128, 1])
    out_d = nc.dram_tensor("out", [T, D], F32, kind="ExternalOutput").ap()

    zf_d = dscr("zf_s", [512, T], BF); xr_d = dscr("xr_s", [D, TT], BF); gg_d = dscr("gg_s", [D, T], BF)
    sa_d = dscr("sa_s", [D, T], BF); sb_d = dscr("sb_s", [D, T], BF); yin_d = dscr("yin_s", [D, T], BF)
    yf_d = dscr("yf_s", [512, T], BF); x1_d = dscr("x1_s", [T, D], BF); xn2_d = dscr("xn2_s", [T, D], BF)
    sc_d = dscr("sc_s", [T, NE]); cs_d = dscr("cs_s", [NE, T]); m_d = dscr("m_s", [NE, T]); y_d = dscr("y_s", [T, D])
    idx_dbg = dscr("idx_s", [128, 64])

    P = Prog(nc, es)
    sb = SBAlloc(nc)
    PS = [nc.alloc_psum_tensor('ps%d' % i, [128, 1024], F32) for i in range(4)]
    pst = {'h': 0, 'f': 0}

    def ps_half():
        i = pst['h'] % 8
        pst['h'] += 1
        return PS[i // 2][:, (i % 2) * 512:(i % 2) * 512 + 512], 'ps%d' % i

    def ps_full():
        i = pst['f'] % 4
        pst['f'] += 1
        return PS[i][:, :], ['ps%d' % (2 * i), 'ps%d' % (2 * i + 1)]

    def dma(queue, out, in_, reads, writes, **kw):
        P.op(queue, lambda e, out=out, in_=in_, kw=kw: e.dma_start(out=out, in_=in_, **kw), reads, writes, dma=True)

    def mm(out_ap, pairs, reads, writes):
        def fn(e, out_ap=out_ap, pairs=pairs):
            n = len(pairs)
            for i, (l, r) in enumerate(pairs):
                ins = e.matmul(out_ap, l, r, start=(i == 0), stop=(i == n - 1))
            return ins
        P.op('pe', fn, reads, writes)

    def tr(out_ap, in_ap, ident, reads, writes):
        P.op('pe', lambda e, o=out_ap, i=in_ap, idn=ident: e.transpose(o, i, idn), reads, writes)

    def act(out, in_, func, reads, writes, eng='act', **kw):
        P.op('act', lambda e, out=out, in_=in_, func=func, kw=kw: e.activation(out=out, in_=in_, func=func, **kw), reads, writes)

    def V(eng, meth, reads, writes, *a, **kw):
        P.op(eng, lambda e, meth=meth, a=a, kw=kw: getattr(e, meth)(*a, **kw), reads, writes)

    identf = sb.alloc([128, 128], F32); identb = sb.alloc([128, 128], BF)
    ADA = sb.alloc([128, 96], F32)
    scale1 = sb.alloc([128, 8], F32); cscale1 = sb.alloc([128, 8], F32); scale2 = sb.alloc([128, 8], F32)
    n1g = sb.alloc([128, 8], F32); n2g = sb.alloc([128, 8], F32)
    dma('sp', identf[:], identf_d, [], ['identf']); dma('sp', identb[:], identb_d, [], ['identb'])
    dma('sp', n1g[:], n1g_d, [], ['n1g']); dma('sp', n2g[:], n2g_d, [], ['n2g'])

    def ada(t, k=None, ctx=False):
        base = 48 if ctx else 0
        if k is None:
            return ADA[:, base + t * 8: base + t * 8 + 8]
        return ADA[:, base + t * 8 + k: base + t * 8 + k + 1]

    m0 = sb.mark()
    cv = sb.alloc([128, 16], F32); sg = sb.alloc([128, 16], F32); scv = sb.alloc([128, 16], F32)
    bada = sb.alloc([128, 48], F32)
    wst = [sb.alloc([128, 8, 512], F32) for _ in range(3)]
    wbf0 = [sb.alloc([128, 8, 512], BF) for _ in range(2)]
    dma('sp', cv[:], cvec_d, [], ['cv']); dma('sp', bada[:], bada_d, [], ['bada'])
    act(sg[:], cv[:], AF.Sigmoid, ['cv'], ['sg'])
    scvb = sb.alloc([128, 16], BF)
    V('dve', 'tensor_tensor', ['cv', 'sg'], ['scv'], scvb[:], cv[:], sg[:], ALU.mult)
    pada, kada = ps_half()
    scv3 = scvb[:].rearrange("p (c k) -> p c k", k=8)
    for pc in range(12):
        w = wst[pc % 3]; wb = wbf0[pc % 2]; wk = 'wst%d' % (pc % 3); wbk = 'wbf%d' % (pc % 2)
        dma('sp', w[:], wada_d[:, pc * 512:(pc + 1) * 512].rearrange("(k p) e -> p k e", p=128), [], [wk])
        act(wb[:, 0:4, :], w[:, 0:4, :], AF.Copy, [wk], [wbk])
        V('dve', 'tensor_copy', [wk, wbk], [wbk], wb[:, 4:8, :], w[:, 4:8, :])
        for jj in range(4):
            j = pc * 4 + jj
            mm(pada[:, 2 * j:2 * j + 2], [(wb[:, k, jj * 128:(jj + 1) * 128], scv3[:, :, k]) for k in range(8)],
               [wbk, 'scv'], [kada])
    pv = pada[:, 0:96].rearrange("p (j c) -> p c j", c=2)
    V('dve', 'tensor_tensor', [kada, 'bada'], ['ADA'], ADA[:, 0:48], pv[:, 0, :], bada[:], ALU.add)
    V('dve', 'tensor_tensor', [kada, 'bada', 'ADA'], ['ADA'], ADA[:, 48:96], pv[:, 1, :], bada[:], ALU.add)
    V('dve', 'scalar_tensor_tensor', ['ADA', 'n1g'], ['scale1'], scale1[:], ada(1), 1.0, n1g[:], ALU.add, ALU.mult)
    V('dve', 'scalar_tensor_tensor', ['ADA', 'n1g'], ['cscale1'], cscale1[:], ada(1, ctx=True), 1.0, n1g[:], ALU.add, ALU.mult)
    V('dve', 'scalar_tensor_tensor', ['ADA', 'n2g'], ['scale2'], scale2[:], ada(4), 1.0, n2g[:], ALU.add, ALU.mult)
    P.barrier()
    sb.reset(m0)

    def rms_tile(xt, xkey, ss, sskey, junk):
        act(junk[:], xt[:], AF.Square, [xkey], ['junk', sskey], accum_out=ss[:])
        act(ss[:], ss[:], AF.Sqrt, [sskey], [sskey], scale=1.0 / D, bias=epsb[:])
        V('dve', 'reciprocal', [sskey], [sskey], ss[:], ss[:])

    epsb = sb.alloc([128, 1], F32)
    V('dve', 'memset', [], ['epsb'], epsb[:], EPS)
    zcol = sb.alloc([128, 1], F32)
    V('dve', 'memset', [], ['zcol'], zcol[:], 0.0)
    m1 = sb.mark()
    hT = sb.alloc([128, 8, TT], BF)
    xts = [sb.alloc([128, D], F32) for _ in range(4)]
    pts = [sb.alloc([128, D], F32) for _ in range(3)]
    xn16 = [sb.alloc([128, D], BF) for _ in range(4)]
    junk = sb.alloc([128, D], BF)
    sss = [sb.alloc([128, 1], F32) for _ in range(4)]
    for pr_ in range(17):
        pi = pr_ % 4
        pbf = PS[pi][:, :].bitcast(BF)
        tokp = pr_ * 256
        isctx = (pr_ == 0)
        for t_ in range(2):
            i = pr_ * 2 + t_
            xt = xts[i % 4]; xk = 'xt%d' % (i % 4); ss = sss[i % 4]; sk = 'ss%d' % (i % 4)
            xn = xn16[i % 4]; xnk = 'xn16_%d' % (i % 4)
            if isctx:
                dma('sp', xt[:], ctx_d[i * 128:(i + 1) * 128, :], [], [xk])
            else:
                jx = i - 2
                pt = pts[i % 3]; pk = 'pt%d' % (i % 3)
                dma('sp', xt[:], x_d[jx * 128:(jx + 1) * 128, :], [], [xk])
                dma('sp', pt[:], pos_d[jx * 128:(jx + 1) * 128, :], [], [pk])
                V('pool', 'tensor_tensor', [xk, pk], [xk], xt[:], xt[:], pt[:], ALU.add)
            rms_tile(xt, xk, ss, sk, junk)
            V('dve', 'tensor_scalar', [xk, sk], [xnk], xn[:], xt[:], ss[:, 0:1], None, ALU.mult)
            for k in range(8):
                c0 = k * 256 + t_ * 128
                tr(pbf[:, c0:c0 + 128], xn[:, k * 128:(k + 1) * 128], identb[:], [xnk, 'identb'], ['ps%d' % (2 * pi + k // 4)])
        scl = cscale1 if isctx else scale1
        for k in range(8):
            c0 = k * 256
            pkey = 'ps%d' % (2 * pi + k // 4)
            if k < 4:
                act(hT[:, k, tokp:tokp + 256], pbf[:, c0:c0 + 256], AF.Identity,
                    [pkey, 'scale1', 'cscale1', 'ADA'], ['hT%d_%d' % (pr_, k)], scale=scl[:, k:k + 1], bias=ada(0, k, ctx=isctx))
            else:
                V('dve', 'tensor_scalar', [pkey, 'scale1', 'cscale1', 'ADA'], ['hT%d_%d' % (pr_, k)], hT[:, k, tokp:tokp + 256], pbf[:, c0:c0 + 256],
                  scl[:, k:k + 1], ada(0, k, ctx=isctx), ALU.mult, ALU.add)
    hT_keys = ['hT%d_%d' % (i, k) for i in range(17) for k in range(8)]

    wst = [sb.alloc([128, 8, 512], F32) for _ in range(2)]
    wbf = [sb.alloc([128, 8, 512], BF) for _ in range(2)]
    ost = [sb.alloc([128, TT], BF) for _ in range(2)]
    gt = [sb.alloc([128, 512], F32) for _ in range(6)]
    gz = [sb.alloc([128, 512], F32) for _ in range(6)]
    gctr = [0]
    gpend = []
    chunks = [(0, TC)] + [(TC + 512 * c, 512) for c in range(8)]
    ecount = 0
    def p1_dma(pc):
        dma('sp', wst[pc % 2][:], win_d[:, pc * 512:(pc + 1) * 512].rearrange("(k p) e -> p k e", p=128), [], ['wst%d' % (pc % 2)])

    def p1_cast(pc):
        w = wst[pc % 2]; wb = wbf[pc % 2]; wk = 'wst%d' % (pc % 2); wbk = 'wbf%d' % (pc % 2)
        act(wb[:, 0:4, :], w[:, 0:4, :], AF.Copy, [wk], [wbk])
        V('dve', 'tensor_copy', [wk, wbk], [wbk], wb[:, 4:8, :], w[:, 4:8, :])

    p1_dma(0)
    p1_cast(0)
    p1_dma(1)
    for pc in range(9):
        wb = wbf[pc % 2]; wbk = 'wbf%d' % (pc % 2)
        for jj in range(4):
            ec = pc * 4 + jj
            typ = 'F' if ec < 4 else 'XR' if ec < 12 else 'GG' if ec < 20 else 'SA' if ec < 28 else 'SB'
            o = ost[ecount % 2]; ok = 'ost%d' % (ecount % 2); ecount += 1
            ob = o[:]
            for ci, (t0, n) in enumerate(chunks):
                if ci == 0 and typ != 'XR':
                    continue
                ph, pk = ps_half()
                mm(ph[:, 0:n], [(wb[:, k, jj * 128:(jj + 1) * 128], hT[:, k, t0:t0 + n]) for k in range(8)],
                   [wbk] + hT_keys, [pk])
                if typ == 'XR':
                    act(ob[:, t0:t0 + n], ph[:, 0:n], AF.Copy, [pk], [ok])
                elif typ == 'F':
                    cF = (t0 - TC) // 512
                    zdst = ob[:, 0:T].rearrange("p (b a) -> p a b", a=64)[:, 8 * cF:8 * cF + 8, :]
                    act(zdst, ph[:, 0:n].rearrange("p (a b) -> p a b", b=64), AF.Copy, [pk], [ok])
                elif typ in ('SA', 'SB'):
                    act(ob[:, t0 - TC:t0 - TC + n], ph[:, 0:n], AF.Sigmoid, [pk], [ok])
                else:
                    gi = gctr[0] % 6; gctr[0] += 1
                    g0 = gt[gi]; gk = 'gt%d' % gi; zc = gz[gi]; zk = 'gz%d' % gi
                    act(zc[:], ph, AF.Copy, [pk], [zk])
                    act(g0[:], zc[:], AF.Square, [zk], [gk], scale=0.21145921593541212)
                    V('dve', 'scalar_tensor_tensor', [gk, zk], [gk], g0[:], g0[:], 1.0, zc[:], ALU.add, ALU.mult)
                    def fin(g0=g0, gk=gk, zc=zc, zk=zk, ob=ob, ok=ok, a=t0 - TC, n=n):
                        act(g0[:], g0[:], AF.Sigmoid, [gk], [gk], scale=1.5957691216057308)
                        V('dve', 'tensor_tensor', [gk, zk], [ok], ob[:, a:a + n], g0[:], zc[:], ALU.mult)
                    gpend.append(fin)
                    if len(gpend) > 2:
                        gpend.pop(0)()
            while gpend:
                gpend.pop(0)()
            if typ == 'F':
                dma('pool', zf_d[ec * 128:(ec + 1) * 128, :], ob[:, 0:T], [ok], ['zf_d'])
            elif typ == 'XR':
                dma('pool', xr_d[(ec - 4) * 128:(ec - 3) * 128, :], ob[:, 0:TT], [ok], ['xr_d'])
            else:
                dd = {'GG': gg_d, 'SA': sa_d, 'SB': sb_d}[typ]
                e0 = (ec - 12) % 8
                dma('pool', dd[e0 * 128:(e0 + 1) * 128, :], ob[:, 0:T], [ok], [typ + '_d'])
        if pc + 1 < 9:
            p1_cast(pc + 1)
        if pc + 2 < 9:
            p1_dma(pc + 2)
    P.barrier()
    sb.reset(m1)
    if stop_after <= 1:
        return finish(nc, es, P)

    m2 = sb.mark()
    cw = sb.alloc([128, 32], F32); cb = sb.alloc([128, 8], F32); lam = sb.alloc([128, 16], F32)
    ba = sb.alloc([128, 16], F32); bi = sb.alloc([128, 16], F32); nsp = sb.alloc([128, 16], F32)
    dma('sp', cw[:], cw_d, [], ['cw']); dma('sp', cb[:], cb_d, [], ['cb']); dma('sp', lam[:], lam_d, [], ['lam'])
    dma('sp', ba[:], ba_d, [], ['ba']); dma('sp', bi[:], bi_d, [], ['bi'])
    act(nsp[:], lam[:], AF.Exp, ['lam'], ['nsp'], scale=-1.0)
    act(nsp[:], nsp[:], AF.Ln, ['nsp'], ['nsp'], bias=1.0)
    V('dve', 'tensor_scalar', ['nsp'], ['nsp'], nsp[:], nsp[:], -8.0, None, ALU.mult)
    WA = sb.alloc([128, 16, 128], BF); WI = sb.alloc([128, 16, 128], BF)
    DG = sb.alloc([128, 32, 128], BF)
    ggt = sb.alloc([128, T], BF); yint = sb.alloc([128, T], BF)
    wgs = yint[:].bitcast(F32).rearrange("p (m j) -> p m j", j=128)
    dma('sp', wgs, wa_d.rearrange("m i j -> i m j"), [], ['yint'])
    act(WA[:], wgs, AF.Copy, ['yint'], ['WA'])
    dma('sp', wgs, wi_d.rearrange("m i j -> i m j"), ['yint'], ['yint'])
    act(WI[:], wgs, AF.Copy, ['yint'], ['WI'])
    for h in range(8):
        for k in range(4):
            V('dve', 'tensor_scalar', ['identf', 'cw'], ['DG'], DG[:, h * 4 + k, :], identf[:], cw[:, h * 4 + k:h * 4 + k + 1], None, ALU.mult)
    XPW = 4448
    xpads = [sb.alloc([128, XPW], BF)] * 2
    Ubs = [sb.alloc([128, TT], BF) for _ in range(2)]
    Rb = sb.alloc([128, TT], BF)
    Ibs = [sb.alloc([128, TT], BF) for _ in range(3)]
    Abs = [sb.alloc([128, TT], F32) for _ in range(3)]
    T1s = [sb.alloc([128, TT], F32) for _ in range(2)]; Y = sb.alloc([128, T], F32); Hc = sb.alloc([128, TC], F32)
    V('pool', 'memset', [], ['xpad0'], xpads[0][:], 0.0)
    NCH = len(chunks)

    def ck(nm, ci):
        return '%s%d' % (nm, ci)

    def conv_load(h):
        xp = xpads[0]; xk = 'xpad0'
        dma('sp', xp[:, 32:32 + TC], xr_d[h * 128:(h + 1) * 128, 0:TC], [], [xk])
        dma('sp', xp[:, 320:320 + T], xr_d[h * 128:(h + 1) * 128, TC:TT], [], [xk])

    conv_ps = {}

    def conv_mm(h, ci):
        xp = xpads[0]; xk = 'xpad0'
        t0, n = chunks[ci]
        base = 32 if ci == 0 else 320
        s_ = t0 if ci == 0 else t0 - TC
        ph, pk = ps_half()
        mm(ph[:, 0:n], [(DG[:, h * 4 + k, :], xp[:, base + s_ + k - 2: base + s_ + k - 2 + n]) for k in range(4)],
           ['DG', xk], [pk])
        conv_ps[(h, ci)] = (ph, pk)

    def conv_ev(h, ci):
        t0, n = chunks[ci]
        ph, pk = conv_ps.pop((h, ci))
        V('dve', 'tensor_scalar', [pk, 'cb'], ['Ub%d_%d' % (h % 2, ci)], Ubs[h % 2][:, t0:t0 + n], ph[:, 0:n], cb[:, h:h + 1], None, ALU.add)

    conv_load(0)
    for ci in range(NCH):
        conv_mm(0, ci)
        conv_ev(0, ci)
    ggts = [ggt, ggt]
    yints = [yint]

    def unit_ctx(u):
        h = u // 2; d = u % 2
        return h, d, d * 8 + h, Ubs[h % 2], Ibs[u % 3], Abs[u % 3], T1s[d]

    def ubk_(h, ci):
        return 'Ub%d_%d' % (h % 2, ci)

    def sG(u):
        h, d, col, ub, Ib, Ab, T1 = unit_ctx(u)
        for ci, (t0, n) in enumerate(chunks):
            ph, pk = ps_half()
            mm(ph[:, 0:n], [(WA[:, col, :], ub[:, t0:t0 + n])], ['WA', ubk_(h, ci)], [pk])
            act(Rb[:, t0:t0 + n], ph[:, 0:n], AF.Sigmoid, [pk, 'ba'], [ck('R', ci)], bias=ba[:, col:col + 1], scale=1.0)
            ph, pk = ps_half()
            mm(ph[:, 0:n], [(WI[:, col, :], ub[:, t0:t0 + n])], ['WI', ubk_(h, ci)], [pk])
            act(Ib[:, t0:t0 + n], ph[:, 0:n], AF.Sigmoid, [pk, 'bi'], ['I%d_%d' % (u % 3, ci)], bias=bi[:, col:col + 1], scale=1.0)

    cgroups = [[0, 1, 2], [3, 4, 5], [6, 7, 8]]

    def grange(g_):
        a0 = chunks[g_[0]][0]
        a1 = chunks[g_[-1]][0] + chunks[g_[-1]][1]
        return a0, a1

    def sE(u):
        h, d, col, ub, Ib, Ab, T1 = unit_ctx(u)
        for g_ in cgroups:
            a0, a1 = grange(g_)
            act(Ab[:, a0:a1], Rb[:, a0:a1], AF.Exp, [ck('R', ci) for ci in g_] + ['nsp'], ['A%d_%d' % (u % 3, ci) for ci in g_], scale=nsp[:, col:col + 1])

    def sQ(u):
        h, d, col, ub, Ib, Ab, T1 = unit_ctx(u)
        for ci, (t0, n) in enumerate(chunks):
            V('pool', 'tensor_tensor', ['I%d_%d' % (u % 3, ci), ubk_(h, ci)], ['I%d_%d' % (u % 3, ci)], Ib[:, t0:t0 + n], Ib[:, t0:t0 + n], ub[:, t0:t0 + n], ALU.mult)
        for g_ in cgroups:
            a0, a1 = grange(g_)
            act(T1[:, a0:a1], Ab[:, a0:a1], AF.Square, ['A%d_%d' % (u % 3, ci) for ci in g_], ['T%d_%d' % (d, ci) for ci in g_])

    def sR(u):
        h, d, col, ub, Ib, Ab, T1 = unit_ctx(u)
        for g_ in cgroups:
            a0, a1 = grange(g_)
            act(T1[:, a0:a1], T1[:, a0:a1], AF.Sqrt, ['T%d_%d' % (d, ci) for ci in g_], ['T%d_%d' % (d, ci) for ci in g_], scale=-1.0, bias=1.0)

    def sBm(u):
        h, d, col, ub, Ib, Ab, T1 = unit_ctx(u)
        for ci, (t0, n) in enumerate(chunks):
            V('dve', 'tensor_tensor', ['I%d_%d' % (u % 3, ci), 'T%d_%d' % (d, ci)], ['I%d_%d' % (u % 3, ci)], Ib[:, t0:t0 + n], Ib[:, t0:t0 + n], T1[:, t0:t0 + n], ALU.mult)

    def sSC(u):
        h, d, col, ub, Ib, Ab, T1 = unit_ctx(u)
        allA = ['A%d_%d' % (u % 3, ci) for ci in range(NCH)]; allI = ['I%d_%d' % (u % 3, ci) for ci in range(NCH)]; allT = ['T%d_%d' % (d, ci) for ci in range(NCH)]
        if d == 0:
            V('dve', 'tensor_tensor_scan', [allA[0], allI[0]], ['Hc'], Hc[:], Ab[:, 0:TC], Ib[:, 0:TC], 0.0, ALU.mult, ALU.add)
            V('dve', 'tensor_tensor_scan', allA[1:] + allI[1:] + ['Hc'], ['Y'], Y[:], Ab[:, TC:TT], Ib[:, TC:TT], Hc[:, TC - 1:TC], ALU.mult, ALU.add)
        else:
            V('dve', 'tensor_tensor_scan', [allA[0], allI[0]], ['Hc'], Hc[:, ::-1], Ab[:, 0:TC][:, ::-1], Ib[:, 0:TC][:, ::-1], 0.0, ALU.mult, ALU.add)
            V('dve', 'tensor_tensor_scan', allA[1:] + allI[1:] + allT[1:] + ['Hc'], allT[1:], T1[:, TC:TT][:, ::-1], Ab[:, TC:TT][:, ::-1], Ib[:, TC:TT][:, ::-1],
              Hc[:, 0:1], ALU.mult, ALU.add)
            V('pool', 'tensor_tensor', allT[1:] + ['Y'], ['Y'], Y[:], Y[:], T1[:, TC:TT], ALU.add)
            gg = ggts[0]; ggk = 'ggt0'
            V('pool', 'tensor_tensor', ['Y', ggk], ['yint'], yints[0][:], Y[:], gg[:], ALU.mult)
            dma('pool', yin_d[h * 128:(h + 1) * 128, :], yints[0][:], ['yint'], ['yin_d'])
            if h + 1 < 8:
                head_prefetch(h + 1)

    def head_prefetch(h):
        dma('pool', ggts[0][:], gg_d[h * 128:(h + 1) * 128, :], [], ['ggt0'])

    head_prefetch(0)
    sG(0); sE(0); sQ(0)
    for u in range(16):
        h = u // 2
        if u % 2 == 0 and h + 1 < 8:
            conv_load(h + 1)
        if u + 1 < 16:
            sG(u + 1); sE(u + 1); sQ(u + 1)
        if u % 2 == 0 and h + 1 < 8:
            for ci in range(NCH):
                conv_mm(h + 1, ci)
                conv_ev(h + 1, ci)
        sR(u); sBm(u); sSC(u)
    P.barrier()
    sb.reset(m2)
    if stop_after <= 2:
        return finish(nc, es, P)

    m3 = sb.mark()
    CS = sb.alloc([128, 256], BF); E1 = sb.alloc([128, 32, 512], BF); E2 = sb.alloc([128, 256], BF)
    dma('sp', CS[:], cs128_d, [], ['CS']); dma('sp', E1[:], e1_d.rearrange("p (m c) -> p m c", c=512), [], ['E1'])
    dma('sp', E2[:], e2_d, [], ['E2'])
    ZF = [sb.alloc([128, T], BF) for _ in range(2)]
    Bfm = sb.alloc([128, 2, T], BF)
    Yfm = [sb.alloc([128, T], BF) for _ in range(2)]
    NR = 8
    Zt = [sb.alloc([128, 256], BF) for _ in range(NR)]
    Bt = [sb.alloc([128, 2, 128], BF) for _ in range(NR)]

    def pipeline(n, stages, lags):
        for step in range(n + lags[-1]):
            for st_, lag in zip(stages, lags):
                i_ = step - lag
                if 0 <= i_ < n:
                    st_(i_)

    for g in range(4):
        zf = ZF[g % 2]; zk = 'ZF%d' % (g % 2); yf = Yfm[g % 2]; yk = 'Yfm%d' % (g % 2)
        dma('sp', zf[:], zf_d[g * 128:(g + 1) * 128, :], [], [zk])
        hold = {}

        def sC(m):
            ph, pk = ps_half()
            mm(ph[:, 0:256], [(zf[:, 128 * m:128 * m + 128], CS[:])], [zk, 'CS'], [pk])
            hold[('c', m)] = (ph, pk)

        def sZ(m):
            z1 = Zt[m % NR]; z1k = 'Zt%d' % (m % NR)
            ph, pk = hold.pop(('c', m))
            if m % 2 == 0:
                act(z1[:], ph[:, 0:256], AF.Copy, [pk], [z1k])
            else:
                V('dve', 'tensor_copy', [pk], [z1k], z1[:], ph[:, 0:256])

        def sS(m):
            z1 = Zt[m % NR]; z1k = 'Zt%d' % (m % NR)
            ph, pk = ps_half()
            mm(ph[:, 0:256], [(z1[:, 0:128], E1[:, m, 0:256]), (z1[:, 128:256], E1[:, m, 256:512])], [z1k, 'E1'], [pk])
            hold[('s', m)] = (ph, pk)

        def sB(m):
            ph, pk = hold.pop(('s', m))
            for c in range(2):
                bdst = Bfm[:, c, :].rearrange("p (k t) -> p t k", t=64)[:, 2 * m:2 * m + 2, :]
                bsrc = ph[:, c * 128:(c + 1) * 128].rearrange("p (l k) -> p l k", l=2)
                if c == 0:
                    act(bdst, bsrc, AF.Copy, [pk], ['Bfm%d' % m])
                else:
                    V('dve', 'tensor_copy', [pk, 'Bfm%d' % m], ['Bfm%d' % m], bdst, bsrc)

        pipeline(32, [sC, sZ, sS, sB], LAGS)
        allB = ['Bfm%d' % m for m in range(32)]

        def sT(n):
            ph, pk = ps_half()
            phb = ph.bitcast(BF)
            for c in range(2):
                tr(phb[:, c * 128:(c + 1) * 128], Bfm[:, c, 128 * n:128 * n + 128], identb[:], allB + ['identb'], [pk])
            hold[('t', n)] = (phb, pk)

        def sE(n):
            b1 = Bt[n % NR]; b1k = 'Bt%d' % (n % NR)
            phb, pk = hold.pop(('t', n))
            if n % 2 == 0:
                act(b1[:], phb[:, 0:256].rearrange("p (c n) -> p c n", c=2), AF.Copy, [pk], [b1k])
            else:
                V('dve', 'tensor_copy', [pk], [b1k], b1[:], phb[:, 0:256].rearrange("p (c n) -> p c n", c=2))

        def sM(n):
            b1 = Bt[n % NR]; b1k = 'Bt%d' % (n % NR)
            ph, pk = ps_half()
            mm(ph[:, 0:128], [(b1[:, 0, :], E2[:, 0:128]), (b1[:, 1, :], E2[:, 128:256])], [b1k, 'E2'], [pk])
            hold[('m', n)] = (ph, pk)

        def sY(n):
            ph, pk = hold.pop(('m', n))
            dst = yf[:].rearrange("p (a b) -> p b a", b=64)[:, 2 * n:2 * n + 2, :]
            if n % 2 == 1:
                act(dst, ph[:, 0:128].rearrange("p (c n) -> p c n", c=2), AF.Copy, [pk], [yk])
            else:
                V('dve', 'tensor_copy', [pk], [yk], dst, ph[:, 0:128].rearrange("p (c n) -> p c n", c=2))

        pipeline(32, [sT, sE, sM, sY], LAGS)
        dma('pool', yf_d[g * 128:(g + 1) * 128, :], yf[:], [yk], ['yf_d'])
    P.barrier()
    sb.reset(m3)
    if stop_after <= 3:
        return finish(nc, es, P)

    Sc = sb.alloc([128, 32, 16], F32); idxs = sb.alloc([128, 64], I32); idx2s = sb.alloc([128, 64], I32)
    m4 = sb.mark()
    S = sb.alloc([128, T], F32)
    wst = [sb.alloc([128, 8, 512], F32) for _ in range(1)]
    WF = sb.alloc([128, 4, D], BF); WL = sb.alloc([128, 8, D], BF); WO = sb.alloc([128, 8, D], BF)
    WRf = sb.alloc([128, 8, 16], F32); WR = sb.alloc([128, 8, 16], BF)
    dma('sp', WRf[:], wr_d.rearrange("p (k e) -> p k e", e=16), [], ['WRf'])
    V('dve', 'tensor_copy', ['WRf'], ['WR'], WR[:], WRf[:])
    cnt = 0
    for (src, dstw, nk) in [(wfour_d, WF, 4), (wlru_d, WL, 8), (wout_d, WO, 8)]:
        for half in range(2):
            w = wst[0]; wk = 'wst0'; cnt += 1
            dma('sp', w[:, 0:nk, :], src[:, half * 512:(half + 1) * 512].rearrange("(k p) e -> p k e", p=128), [], [wk])
            act(dstw[:, :, half * 512:(half + 1) * 512], w[:, 0:nk, :], AF.Copy, [wk], ['W4'])
    yfc = [sb.alloc([128, 4, 512], BF) for _ in range(2)]
    yic = [sb.alloc([128, 8, 512], BF) for _ in range(2)]
    sac = [sb.alloc([128, 8, 512], BF) for _ in range(2)]
    sbc = [sb.alloc([128, 8, 512], BF) for _ in range(2)]
    mg = sb.alloc([128, 8, 512], BF)
    tm = [sb.alloc([128, 512], BF) for _ in range(4)]
    yg = sb.alloc([128, 8, 512], BF)
    xts = [sb.alloc([128, D], F32) for _ in range(3)]
    pts = [sb.alloc([128, D], F32) for _ in range(2)]
    xnb = [sb.alloc([128, D], BF) for _ in range(2)]
    x1b = [sb.alloc([128, D], BF) for _ in range(3)]
    h2T = [sb.alloc([128, 8, 128], BF) for _ in range(2)]
    junk = sb.alloc([128, D], BF)
    sss = [sb.alloc([128, 1], F32) for _ in range(3)]
    LGP = PS[3][:, 512:1024]
    lgk = 'ps7'
    pst4 = {'h': 0}

    def ps_half6():
        i = pst4['h'] % 6
        pst4['h'] += 1
        return PS[i // 2][:, (i % 2) * 512:(i % 2) * 512 + 512], 'ps%d' % i

    def ps_full3():
        i = pst4['h'] % 6
        if i % 2:
            pst4['h'] += 1
            i = pst4['h'] % 6
        pst4['h'] += 2
        return PS[i // 2][:, :], ['ps%d' % i, 'ps%d' % (i + 1)]

    ygs = [yg, sb.alloc([128, 8, 512], BF)]

    def A_steps(tc):
        b = tc % 2
        t0 = tc * 512
        ygc = ygs[tc % 2]
        steps = []

        def ld():
            dma('sp', yfc[b][:], yf_d[:, t0:t0 + 512].rearrange("(g p) t -> p g t", p=128), [], ['yfc%d' % b])
            dma('sp', yic[b][:], yin_d[:, t0:t0 + 512].rearrange("(g p) t -> p g t", p=128), [], ['yic%d' % b])
            dma('sp', sac[b][:], sa_d[:, t0:t0 + 512].rearrange("(g p) t -> p g t", p=128), [], ['sac%d' % b])
            dma('sp', sbc[b][:], sb_d[:, t0:t0 + 512].rearrange("(g p) t -> p g t", p=128), [], ['sbc%d' % b])
        steps.append(ld)
        for ec in range(8):
            def f(ec=ec):
                pf, pfk = ps_half6()
                mm(pf, [(WF[:, g, ec * 128:(ec + 1) * 128], yfc[b][:, g, :]) for g in range(4)], ['W4', 'yfc%d' % b], [pfk])
                pr, prk = ps_half6()
                mm(pr, [(WL[:, k, ec * 128:(ec + 1) * 128], yic[b][:, k, :]) for k in range(8)], ['W4', 'yic%d' % b], [prk])
                t1 = tm[(ec % 2) * 2]; t2 = tm[(ec % 2) * 2 + 1]; k1_ = 'tm%d' % ((ec % 2) * 2); k2_ = 'tm%d' % ((ec % 2) * 2 + 1)
                V('dve', 'tensor_tensor', [pfk, 'sac%d' % b], [k1_], t1[:], pf, sac[b][:, ec, :], ALU.mult)
                V('dve', 'tensor_tensor', [prk, 'sbc%d' % b], [k2_], t2[:], pr, sbc[b][:, ec, :], ALU.mult)
                V('dve', 'tensor_tensor', [k1_, k2_], ['mg%d' % ec], mg[:, ec, :], t1[:], t2[:], ALU.add)
            steps.append(f)
        for ec in range(8):
            def g_(ec=ec):
                po, pok = ps_half6()
                mm(po, [(WO[:, k, ec * 128:(ec + 1) * 128], mg[:, k, :]) for k in range(8)], ['W4'] + ['mg%d' % k for k in range(8)], [pok])
                act(ygc[:, ec, :], po, AF.Identity, [pok, 'ADA', 'zcol'], ['yg%d_%d' % (tc % 2, ec)], scale=ada(2, ec), bias=zcol[:])
            steps.append(g_)
        return steps

    def T_steps(tc):
        ygc = ygs[tc % 2]
        hold = {}

        def T1(j):
            ti = tc * 4 + j
            bb = ti % 2; b3 = ti % 3
            xt = xts[b3]; xk = 'xt%d' % b3; pt = pts[bb]; pk_ = 'pt%d' % bb
            dma('sp', xt[:], x_d[ti * 128:(ti + 1) * 128, :], [], [xk])
            dma('sp', pt[:], pos_d[ti * 128:(ti + 1) * 128, :], [], [pk_])
            V('pool', 'tensor_tensor', [xk, pk_], [xk], xt[:], xt[:], pt[:], ALU.add)
            ph_, phk = ps_half6()
            pfb = ph_.bitcast(BF)
            for k in range(8):
                tr(pfb[:, k * 128:(k + 1) * 128], ygc[:, k, j * 128:(j + 1) * 128], identb[:], ['yg%d_%d' % (tc % 2, k), 'identb'], [phk])
            V('dve', 'tensor_tensor', [xk, phk], [xk], xt[:], xt[:], pfb, ALU.add)
            act(x1b[b3][:], xt[:], AF.Copy, [xk], ['x1b%d' % b3])
            dma('pool', x1_d[ti * 128:(ti + 1) * 128, :], x1b[b3][:], ['x1b%d' % b3], ['x1_d'])
            ss = sss[b3]; sk = 'ss%d' % b3
            rms_tile(xt, xk, ss, sk, junk)
            V('dve', 'tensor_scalar', [xk, sk], ['xnb%d' % bb], xnb[bb][:], xt[:], ss[:, 0:1], None, ALU.mult)
            dma('pool', xn2_d[ti * 128:(ti + 1) * 128, :], xnb[bb][:], ['xnb%d' % bb], ['xn2_d'])

        def T2(j):
            ti = tc * 4 + j
            bb = ti % 2
            ph_a, phk_a = ps_half6()
            ph_b, phk_b = ps_half6()
            pfa = ph_a.bitcast(BF); pfb2 = ph_b.bitcast(BF)
            for k in range(8):
                dstp = pfa if k < 4 else pfb2
                tr(dstp[:, (k % 4) * 128:(k % 4 + 1) * 128], xnb[bb][:, k * 128:(k + 1) * 128], identb[:], ['xnb%d' % bb, 'identb'], [phk_a if k < 4 else phk_b])
            for k in range(8):
                if k < 4:
                    act(h2T[bb][:, k, :], pfa[:, (k % 4) * 128:(k % 4 + 1) * 128], AF.Identity, [phk_a, 'scale2', 'ADA'], ['h2Ta%d' % bb],
                        scale=scale2[:, k:k + 1], bias=ada(3, k))
                else:
                    V('dve', 'tensor_scalar', [phk_b, 'scale2', 'ADA'], ['h2Tb%d' % bb], h2T[bb][:, k, :], pfb2[:, (k % 4) * 128:(k % 4 + 1) * 128],
                      scale2[:, k:k + 1], ada(3, k), ALU.mult, ALU.add)

        def R(j):
            ti = tc * 4 + j
            bb = ti % 2
            mm(LGP[:, ti * 16:(ti + 1) * 16], [(h2T[bb][:, k, :], WR[:, k, :]) for k in range(8)], ['h2Ta%d' % bb, 'h2Tb%d' % bb, 'WR'], [lgk])

        order = [(T1, 0), (T1, 1), (T2, 0), (T1, 2), (R, 0), (T2, 1), (T1, 3), (R, 1), (T2, 2), (R, 2), (T2, 3), (R, 3)]
        return [(lambda f=f, j=j: f(j)) for (f, j) in order]

    for st in A_steps(0):
        st()
    for tc in range(8):
        a_ = A_steps(tc + 1) if tc + 1 < 8 else []
        t_ = T_steps(tc)
        ia = it_ = 0
        while ia < len(a_) or it_ < len(t_):
            for _ in range(3):
                if ia < len(a_):
                    a_[ia](); ia += 1
            for _ in range(2):
                if it_ < len(t_):
                    t_[it_](); it_ += 1
    mx = sb.alloc([128, 32], F32)
    lg3 = LGP.rearrange("p (j e) -> p j e", e=16)
    V('dve', 'tensor_reduce', [lgk], ['mx'], mx[:], lg3, AX.X, ALU.max)
    V('dve', 'tensor_tensor', [lgk, 'mx'], ['Sc'], Sc[:], lg3, mx[:].unsqueeze(2).to_broadcast([128, 32, 16]), ALU.subtract)
    act(Sc[:], Sc[:], AF.Exp, ['Sc'], ['Sc'])
    V('dve', 'tensor_reduce', ['Sc'], ['mx'], mx[:], Sc[:], AX.X, ALU.add)
    V('dve', 'reciprocal', ['mx'], ['mx'], mx[:], mx[:])
    V('dve', 'tensor_tensor', ['Sc', 'mx'], ['Sc'], Sc[:], Sc[:], mx[:].unsqueeze(2).to_broadcast([128, 32, 16]), ALU.mult)
    dma('pool', sc_d.rearrange("(p j) e -> p (j e)", p=128), Sc[:].rearrange("p j e -> p (j e)"), ['Sc'], ['sc_d'])
    for q in range(4):
        pf, pfk = ps_full3()
        for jj in range(8):
            j = q * 8 + jj
            tr(pf[0:16, jj * 128:(jj + 1) * 128], Sc[:, j, :], identf[:], ['Sc', 'identf'], [pfk[jj // 4]])
        V('dve', 'tensor_copy', pfk, ['S'], S[0:16, q * 1024:(q + 1) * 1024], pf[0:16, :])
    P.barrier()
    sb.reset(m4)
    if stop_after <= 4:
        return finish(nc, es, P)

    S = sb.alloc([128, T], F32)
    zt = sb.alloc([128, 2048], F32)
    V('pool', 'memset', [], ['zt'], zt[:], 0.0)
    for i in range(16):
        dma('pool', y_d[i * 256:(i + 1) * 256, :].rearrange("(p r) c -> p (r c)", p=128), zt[:], ['zt'], ['y_d'])
    lo = sb.alloc([128, 1], F32); mid = sb.alloc([128, 1], F32); cntt = sb.alloc([128, 1], F32); ge = sb.alloc([128, 1], F32)
    jk = sb.alloc([128, T], BF)
    V('dve', 'memset', [], ['lo'], lo[0:16, :], 0.0)
    for it in range(30):
        half = 2.0 ** (-(it + 1))
        V('dve', 'tensor_scalar', ['lo'], ['mid'], mid[0:16, :], lo[0:16, :], half, None, ALU.add)
        V('dve', 'tensor_scalar', ['S', 'mid'], ['jk', 'cnt'], jk[0:16, :], S[0:16, :], mid[0:16, 0:1], None, ALU.is_ge, ALU.add, cntt[0:16, :])
        V('dve', 'tensor_scalar', ['cnt'], ['ge'], ge[0:16, :], cntt[0:16, :], float(CAP), half, ALU.is_ge, ALU.mult)
        V('dve', 'tensor_tensor', ['ge', 'lo'], ['lo'], lo[0:16, :], lo[0:16, :], ge[0:16, :], ALU.add)
    Mk = sb.alloc([128, T], F32); Cs = sb.alloc([128, T], F32)
    V('dve', 'tensor_scalar', ['S', 'lo'], ['Mk'], Mk[0:16, :], S[0:16, :], lo[0:16, 0:1], None, ALU.is_ge)
    ones = sb.alloc([128, T], BF)
    V('pool', 'memset', [], ['ones'], ones[0:16, :], 1.0)
    V('dve', 'tensor_tensor_scan', ['Mk', 'ones'], ['Cs'], Cs[0:16, :], ones[0:16, :], Mk[0:16, :], 0.0, ALU.mult, ALU.add)
    dma('sp', cs_d, Cs[0:16, :], ['Cs'], ['cs_d'])
    dma('sp', m_d, Mk[0:16, :], ['Mk'], ['m_d'])
    CSJ = sb.alloc([128, 8, 132], F32); M4 = sb.alloc([128, 8, 128], F32)
    V('pool', 'memset', [], ['CSJ'], CSJ[:], 0.0)
    iotac = sb.alloc([128, 512], F32); cval = sb.alloc([128, 4], F32)
    dma('sp', CSJ[0:64, :, 0:128], cs_d.rearrange("e (j p) -> (e j) p", p=128).rearrange("(q r) p -> r q p", r=64), ['cs_d', 'CSJ'], ['CSJ'])
    dma('sp', M4[0:64, :, :], m_d.rearrange("e (j p) -> (e j) p", p=128).rearrange("(q r) p -> r q p", r=64), ['m_d'], ['M4'])
    for q in range(8):
        dma('sp', CSJ[0:64, q, 128:129], jcol_d[0:64, :], ['CSJ'], ['CSJ'])
    dma('sp', iotac[:], iotac_d, [], ['iotac']); dma('sp', cval[:], cval_d, [], ['cval'])
    hi4 = sb.alloc([128, 8], F32); lo4 = sb.alloc([128, 8], F32)
    J4 = sb.alloc([128, 8, 512], F32); tj = sb.alloc([128, 512], F32)
    V('dve', 'tensor_copy', ['CSJ'], ['hi4'], hi4[0:64, :], CSJ[0:64, :, 127])
    V('dve', 'tensor_tensor', ['CSJ', 'M4'], ['lo4'], lo4[0:64, :], CSJ[0:64, :, 0], M4[0:64, :, 0], ALU.subtract)
    for q in range(8):
        V('dve', 'tensor_scalar', ['iotac', 'lo4'], ['tj'], tj[0:64, :], iotac[0:64, :], lo4[0:64, q:q + 1], None, ALU.is_ge)
        V('dve', 'scalar_tensor_tensor', ['iotac', 'hi4', 'tj'], ['J4'], J4[0:64, q, :], iotac[0:64, :], hi4[0:64, q:q + 1], tj[0:64, :], ALU.is_lt, ALU.mult)
    idxf = sb.alloc([128, 64], F32); rr = sb.alloc([128, 64], F32); idx2f = sb.alloc([128, 64], F32)
    jk3 = sb.alloc([128, 128], F32)
    for e in range(NE):
        q = e // 2; r0 = (e % 2) * 32
        for g in range(4):
            col = e * 4 + g
            ph, pk = ps_half6()
            mm(ph[:, 0:130], [(J4[r0:r0 + 32, q, g * 128:(g + 1) * 128], CSJ[r0:r0 + 32, q, 0:130])], ['J4', 'CSJ'], [pk])
            V('dve', 'tensor_scalar', [pk, 'cval'], ['jk3', 'rr'], jk3[:], ph[:, 0:128], cval[:, g:g + 1], None, ALU.is_le, ALU.add, rr[:, col:col + 1])
            V('dve', 'scalar_tensor_tensor', [pk, 'rr'], ['idxf'], idxf[:, col:col + 1], ph[:, 128:129], 128.0, rr[:, col:col + 1], ALU.mult, ALU.add)
            V('dve', 'scalar_tensor_tensor', [pk, 'rr'], ['idx2f'], idx2f[:, col:col + 1], rr[:, col:col + 1], 32.0, ph[:, 128:129], ALU.mult, ALU.add)
    V('dve', 'tensor_copy', ['idxf'], ['idxs'], idxs[:], idxf[:])
    V('dve', 'tensor_copy', ['idx2f'], ['idx2s'], idx2s[:], idx2f[:])
    if debug:
        dma('sp', idx_dbg, idxf[:], ['idxf'], ['idx_dbg'])
    P.barrier()
    sb.reset(m4)
    if stop_after <= 5:
        return finish(nc, es, P)

    m6 = sb.mark()
    wst = [sb.alloc([128, 8, 512], F32) for _ in range(4)]
    wbf = [sb.alloc([128, 8, 512], BF) for _ in range(7)]
    xrow = [sb.alloc([128, D], BF) for _ in range(8)]
    grow = [sb.alloc([128, 16], F32) for _ in range(16)]
    xgT = [sb.alloc([128, 8, 512], BF) for _ in range(2)]
    hid = sb.alloc([128, 12, 512], BF)
    sgt = [sb.alloc([128, 512], BF) for _ in range(2)]
    og = sb.alloc([128, 8, 512], F32)
    orow = [sb.alloc([128, D], F32) for _ in range(4)]
    pieces = []
    for e in range(NE):
        for fq in range(3):
            pieces.append((wg_d[e, :, fq * 512:(fq + 1) * 512].rearrange("(k p) f -> p k f", p=128), 8))
            pieces.append((wu_d[e, :, fq * 512:(fq + 1) * 512].rearrange("(k p) f -> p k f", p=128), 8))
        for dq in range(3):
            pieces.append((wd_d[e, dq * 512:(dq + 1) * 512, :].rearrange("(k p) c -> p k c", p=128), 4))
    loaded = {}
    wctr = {'dma': 0, 'cast': 0}
    LOOK_DMA = 6
    LOOK_CAST = 3

    def ensure_dma(upto):
        while wctr['dma'] <= min(upto, len(pieces) - 1):
            n = wctr['dma']; wctr['dma'] += 1
            src_ap, a = pieces[n]
            i = n % 4
            dma('sp', wst[i][:].rearrange("p a b -> p (a b)").rearrange("p (a b) -> p a b", a=a), src_ap, [], ['wst%d' % i])

    def ensure_cast(upto):
        while wctr['cast'] <= min(upto, len(pieces) - 1):
            n = wctr['cast']; wctr['cast'] += 1
            ensure_dma(n)
            src_ap, a = pieces[n]
            i = n % 4; jj = n % 7
            w = wst[i]; wb = wbf[jj]
            wv = w[:].rearrange("p a b -> p (a b)"); wbv = wb[:].rearrange("p a b -> p (a b)")
            if n % 2 == 0:
                act(wbv, wv, AF.Copy, ['wst%d' % i], ['wbf%d' % jj])
            else:
                V('dve', 'tensor_copy', ['wst%d' % i], ['wbf%d' % jj], wbv, wv)
            loaded[n] = (wb[:].rearrange("p a b -> p (a b)").rearrange("p (a b) -> p a b", a=a), 'wbf%d' % jj)

    def ensure_loaded(upto):
        ensure_cast(upto)

    def get_piece(n):
        ensure_cast(n + LOOK_CAST)
        ensure_dma(n + LOOK_DMA)
        return loaded[n]

    def gathers(e):
        for g in range(4):
            s = (e % 2) * 4 + g
            s3 = (e % 4) * 4 + g
            col = e * 4 + g
            P.op('pool', lambda en, s=s, col=col: en.indirect_dma_start(
                out=xrow[s][:, :], out_offset=None, in_=xn2_d[:, :],
                in_offset=bass.IndirectOffsetOnAxis(ap=idxs[:, col:col + 1], axis=0)), ['idxs', 'xn2_d'], ['xrow%d' % s], dma=True)
            P.op('pool', lambda en, s3=s3, col=col: en.indirect_dma_start(
                out=grow[s3][:, :], out_offset=None, in_=sc_d[:, :],
                in_offset=bass.IndirectOffsetOnAxis(ap=idx2s[:, col:col + 1], axis=0)), ['idx2s', 'sc_d'], ['grow%d' % s3], dma=True)

    def build_xgT(e):
        xg = xgT[e % 2]; xgk = 'xgT%d' % (e % 2)
        for g in range(4):
            s = (e % 2) * 4 + g
            ph, pk = ps_half()
            phb = ph.bitcast(BF)
            for k in range(8):
                tr(phb[:, k * 128:(k + 1) * 128], xrow[s][:, k * 128:(k + 1) * 128], identb[:], ['xrow%d' % s, 'identb'], [pk])
            for k in range(8):
                act(xg[:, k, g * 128:(g + 1) * 128], phb[:, k * 128:(k + 1) * 128], AF.Identity, [pk, 'scale2', 'ADA'], [xgk],
                    scale=scale2[:, k:k + 1], bias=ada(3, k))

    def outT(e):
        for g in range(4):
            s3 = (e % 4) * 4 + g
            col = e * 4 + g
            pf, pfk = ps_full()
            for k in range(8):
                tr(pf[:, k * 128:(k + 1) * 128], og[:, k, g * 128:(g + 1) * 128], identf[:], ['og%d' % k, 'identf'], [pfk[k // 4]])
            orw = orow[g]; ork = 'orow%d' % g
            V('dve', 'tensor_scalar', pfk + ['grow%d' % s3], [ork], orw[:], pf, grow[s3][:, e:e + 1], None, ALU.mult)
            P.op('pool', lambda en, orw=orw, col=col: en.indirect_dma_start(
                out=y_d[:, :], out_offset=bass.IndirectOffsetOnAxis(ap=idxs[:, col:col + 1], axis=0),
                in_=orw[:, :], in_offset=None, compute_op=ALU.add), [ork, 'idxs'], ['y_d'], dma=True)

    ensure_dma(3)
    ensure_cast(3)
    gathers(0)
    gathers(1)
    build_xgT(0)
    for e in range(NE):
        if e + 2 < NE:
            gathers(e + 2)
        xg = xgT[e % 2]; xgk = 'xgT%d' % (e % 2)
        for fq in range(3):
            wgb, wgk = get_piece(e * 9 + fq * 2)
            wub, wuk = get_piece(e * 9 + fq * 2 + 1)
            for fc in range(4):
                f = fq * 4 + fc
                pg, pgk = ps_half()
                mm(pg, [(wgb[:, k, fc * 128:(fc + 1) * 128], xg[:, k, :]) for k in range(8)], [wgk, xgk], [pgk])
                pu, puk = ps_half()
                mm(pu, [(wub[:, k, fc * 128:(fc + 1) * 128], xg[:, k, :]) for k in range(8)], [wuk, xgk], [puk])
                sg_ = sgt[f % 2]; sgk = 'sgt%d' % (f % 2)
                act(sg_[:], pg, AF.Silu, [pgk], [sgk])
                V('dve', 'tensor_tensor', [sgk, puk], ['hid'], hid[:, f, :], sg_[:], pu, ALU.mult)
            if fq == 0 and e >= 1:
                outT(e - 1)
        if e + 1 < NE:
            build_xgT(e + 1)
        wds = [get_piece(e * 9 + 6 + dq) for dq in range(3)]
        for ec in range(8):
            po, pok = ps_half()
            mm(po, [(wds[f // 4][0][:, f % 4, ec * 128:(ec + 1) * 128], hid[:, f, :]) for f in range(12)],
               [wds[0][1], wds[1][1], wds[2][1], 'hid'], [pok])
            act(og[:, ec, :], po, AF.Identity, [pok, 'ADA', 'zcol'], ['og%d' % ec], scale=ada(5, ec), bias=zcol[:])
    outT(NE - 1)
    P.barrier()
    sb.reset(m6)
    if stop_after <= 6:
        return finish(nc, es, P)

    FG = sb.alloc([128, D], F32)
    dma('sp', FG[:], fg_d.partition_broadcast(128), [], ['FG'])
    xbs = [sb.alloc([128, D], BF) for _ in range(6)]
    yts = [sb.alloc([128, D], F32) for _ in range(6)]
    ots = [sb.alloc([128, D], F32) for _ in range(4)]
    junk = sb.alloc([128, D], BF)
    sss = [sb.alloc([128, 1], F32) for _ in range(6)]
    for ti in range(32):
        b = ti % 6
        xb = xbs[b]; xk = 'xb%d' % b; yt = yts[b]; yk = 'yt%d' % b; ss = sss[b]; sk = 'ss%d' % b
        ot = ots[ti % 4]; ok_ = 'ot%d' % (ti % 4)
        dma('sp', xb[:], x1_d[ti * 128:(ti + 1) * 128, :], ['x1_d'], [xk])
        dma('sp', yt[:], y_d[ti * 128:(ti + 1) * 128, :], ['y_d'], [yk])
        V('dve', 'tensor_tensor', [xk, yk], [yk], yt[:], yt[:], xb[:], ALU.add)
        rms_tile(yt, yk, ss, sk, junk)
        V('dve', 'scalar_tensor_tensor', [yk, sk, 'FG'], [ok_], ot[:], yt[:], ss[:, 0:1], FG[:], ALU.mult, ALU.mult)
        dma('pool', out_d[ti * 128:(ti + 1) * 128, :], ot[:], [ok_], ['out_d'])
    return finish(nc, es, P)


def finish(nc, es, P):
    P.barrier()
    with nc.Block() as block:
        @block.tensor
        def _(e):
            P.emit(e, 'pe')

        @block.scalar
        def _(e):
            P.emit(e, 'act')

        @block.vector
        def _(e):
            P.emit(e, 'dve')

        @block.gpsimd
        def _(e):
            P.emit(e, 'pool')

        @block.sync
        def _(e):
            P.emit(e, 'sp')
    es.close()
    return nc


def _consts():
    bf = ml_dtypes.bfloat16
    n = np.arange(128)
    ang = 2 * np.pi * np.outer(n, n) / 128.0
    cs128 = np.concatenate([np.cos(ang), np.sin(ang)], axis=1)
    t1 = np.arange(64); k1 = np.arange(64)
    e1 = np.zeros((32, 128, 512))
    for m in range(32):
        for t2l in range(2):
            t2 = 2 * m + t2l
            ph = 2 * np.pi * (np.outer(t1, k1) / 64.0 + (k1[None, :] * t2) / 4096.0)
            sl = slice(t2l * 64, t2l * 64 + 64)
            e1[m, sl, 0 + t2l * 64:0 + t2l * 64 + 64] = np.cos(ph)
            e1[m, sl, 128 + t2l * 64:128 + t2l * 64 + 64] = np.sin(ph)
            e1[m, sl, 256 + t2l * 64:256 + t2l * 64 + 64] = -np.sin(ph)
            e1[m, sl, 384 + t2l * 64:384 + t2l * 64 + 64] = np.cos(ph)
    e1 = np.ascontiguousarray(e1.transpose(1, 0, 2).reshape(128, 32 * 512))
    s = 1.0 / math.sqrt(4096.0 * 128.0)
    e2 = np.zeros((128, 256))
    t2 = np.arange(64); k2 = np.arange(64)
    ph = 2 * np.pi * np.outer(t2, k2) / 64.0
    for k1l in range(2):
        e2[k1l * 64:k1l * 64 + 64, k1l * 64:k1l * 64 + 64] = np.cos(ph) * s
        e2[k1l * 64:k1l * 64 + 64, 128 + k1l * 64:128 + k1l * 64 + 64] = -np.sin(ph) * s
    quarter = D // 4
    freqs = np.exp(-math.log(10000.0) * np.arange(quarter, dtype=np.float32) / quarter).astype(np.float32)
    ang_r = np.arange(64, dtype=np.float32)[:, None] * freqs
    emb_r = np.concatenate([np.sin(ang_r), np.cos(ang_r)], axis=-1)
    emb = np.concatenate([np.broadcast_to(emb_r[:, None, :], (64, 64, D // 2)),
                          np.broadcast_to(emb_r[None, :, :], (64, 64, D // 2))], axis=-1).reshape(T, D).astype(np.float32)
    return dict(cs128=cs128.astype(bf), e1=e1.astype(bf), e2=e2.astype(bf), pos=np.ascontiguousarray(emb),
                identf=np.eye(128, dtype=np.float32), identb=np.eye(128).astype(bf),
                iota_c=np.ascontiguousarray(np.broadcast_to(np.arange(512, dtype=np.float32)[None, :], (128, 512))),
                cval=(np.arange(4)[None, :] * 128 + np.arange(128)[:, None]).astype(np.float32),
                jcol=(np.arange(128) % 32).astype(np.float32).reshape(128, 1))


def _fm(v):
    return np.ascontiguousarray(np.asarray(v, np.float32).reshape(8, 128).T)


def make_in_maps(inputs, cores):
    f = lambda a: np.ascontiguousarray(np.asarray(a, np.float32))
    cst = _consts()
    l = 0
    shared = dict(cst)
    shared.update(
        w_ada=f(inputs['w_ada'][l]), b_ada_fm=np.ascontiguousarray(f(inputs['b_ada'][l]).reshape(48, 128).T),
        n1g_fm=_fm(inputs['norm1_g'][l]), n2g_fm=_fm(inputs['norm2_g'][l]), final_g=f(inputs['final_g']).reshape(1, D),
        w_in=f(inputs['w_in'][l]), w_four=f(inputs['w_four'][l]), w_lru=f(inputs['w_lru'][l]), w_out=f(inputs['w_out'][l]),
        conv_w_fm=np.ascontiguousarray(f(inputs['conv_w'][l]).reshape(4, 8, 128).transpose(2, 1, 0).reshape(128, 32)),
        conv_b_fm=_fm(inputs['conv_b'][l]),
        lam_fm=np.ascontiguousarray(f(inputs['lru_lambda'][l]).reshape(2, 8, 128).transpose(2, 0, 1).reshape(128, 16)),
        ba_fm=np.ascontiguousarray(f(inputs['lru_ba'][l]).reshape(2, 8, 128).transpose(2, 0, 1).reshape(128, 16)),
        bi_fm=np.ascontiguousarray(f(inputs['lru_bi'][l]).reshape(2, 8, 128).transpose(2, 0, 1).reshape(128, 16)),
        lru_wa=f(inputs['lru_wa'][l]).reshape(16, 128, 128), lru_wi=f(inputs['lru_wi'][l]).reshape(16, 128, 128),
        w_router_fm=np.ascontiguousarray(f(inputs['w_router'][l]).reshape(8, 128, 16).transpose(1, 0, 2).reshape(128, 128)),
        w_gate_e=f(inputs['w_gate_e'][l]), w_up_e=f(inputs['w_up_e'][l]), w_down_e=f(inputs['w_down_e'][l]),
    )
    cctx = _fm(inputs['c_ctx'])
    maps = []
    for b in cores:
        m = dict(shared)
        m['x'] = f(inputs['x'][b]); m['ctx'] = f(inputs['ctx'][b])
        m['cvec'] = np.ascontiguousarray(np.concatenate([_fm(inputs['c'][b]), cctx], axis=1))
        maps.append(m)
    return maps


def kernel(**inputs):
    nc = build_program()
    maps = make_in_maps(inputs, list(range(8)))
    res = run_bass_kernel_spmd(nc, maps, core_ids=list(range(8)))
    return np.stack([np.asarray(r["out"], np.float32) for r in res.results], axis=0)
```

```python
import contextlib
import math
import numpy as np
import ml_dtypes
import concourse.bass as bass
import concourse.mybir as mybir
from concourse.bass_utils import run_bass_kernel_spmd

F32 = mybir.dt.float32
BF = mybir.dt.bfloat16
I32 = mybir.dt.int32
ALU = mybir.AluOpType
AF = mybir.ActivationFunctionType
AX = mybir.AxisListType

T = 4096
TC = 256
TT = T + TC
D = 1024
NE = 16
CAP = 512
DF = 1536
EPS = 1e-6
DEBUG = False
STOP_AFTER = 99
CUT = 0
LAGS = [0, 1, 3, 4]


class Prog:
    def __init__(self, nc, es):
        self.nc = nc
        self.q = {k: [] for k in ['pe', 'act', 'dve', 'pool', 'sp']}
        self.sem = {k: es.enter_context(nc.semaphore('s_' + k)) for k in ['pe', 'act', 'dve', 'pool']}
        self.cnt = {k: 0 for k in self.sem}
        self.dsem = {qn: [es.enter_context(nc.semaphore('d_%s%d' % (qn, i))) for i in range(n)]
                     for qn, n in [('sp', 14), ('pool', 10), ('act', 6)]}
        self.dcnt = {qn: 0 for qn in self.dsem}
        self.dtarget = {}
        self.lastw = {}
        self.readers = {}
        self.waited = {}
        self.semobj = {}

    def _sid(self, s):
        self.semobj[id(s)] = s
        return id(s)

    def op(self, queue, fn, reads=(), writes=(), dma=False):
        deps = set()
        for r in reads:
            if r in self.lastw:
                deps.add(self.lastw[r])
        for w in writes:
            if w in self.lastw:
                deps.add(self.lastw[w])
            for rd in self.readers.get(w, ()):
                deps.add(rd)
        if dma:
            pool = self.dsem[queue]
            i = self.dcnt[queue]
            self.dcnt[queue] += 1
            s = pool[i % len(pool)]
            rnd = i // len(pool)
            ev = (self._sid(s), 16 * (rnd + 1))
            if rnd > 0:
                deps.add((self._sid(s), 16 * rnd))
            inc = (s, 16)
            self.dtarget[self._sid(s)] = 16 * (rnd + 1)
        else:
            self.cnt[queue] += 1
            s = self.sem[queue]
            ev = (self._sid(s), self.cnt[queue])
            inc = (s, 1)
        waits = {}
        for (sid, v) in deps:
            if queue == 'pe' and not dma and sid == id(self.sem['pe']):
                continue
            if self.waited.get((queue, sid), 0) >= v:
                continue
            waits[sid] = max(waits.get(sid, 0), v)
        for sid, v in waits.items():
            self.waited[(queue, sid)] = v
        self.q[queue].append(([(self.semobj[sid], v) for sid, v in waits.items()], fn, inc))
        for r in reads:
            self.readers.setdefault(r, []).append(ev)
        for w in writes:
            self.lastw[w] = ev
            self.readers[w] = []
        return ev

    def barrier(self):
        evs = []
        for k, s in self.sem.items():
            if self.cnt[k] > 0:
                evs.append((self._sid(s), self.cnt[k]))
        for sid, v in self.dtarget.items():
            evs.append((sid, v))
        for queue in self.q:
            waits = []
            for (sid, v) in evs:
                if self.waited.get((queue, sid), 0) >= v:
                    continue
                self.waited[(queue, sid)] = v
                waits.append((self.semobj[sid], v))
            if waits:
                self.q[queue].append((waits, None, None))

    def emit(self, e, queue):
        for waits, fn, inc in self.q[queue]:
            for s, v in waits:
                e.wait_ge(s, v)
            if fn is not None:
                ins = fn(e)
                ins.then_inc(inc[0], inc[1])


class SBAlloc:
    def __init__(self, nc, base=20736, end=229376):
        self.nc = nc
        self.cur = base
        self.end = end
        self.n = 0

    def alloc(self, shape, dtype):
        sz = 1
        for s in shape[1:]:
            sz *= s
        sz *= {F32: 4, BF: 2, I32: 4}[dtype]
        sz = (sz + 63) // 64 * 64
        assert self.cur + sz <= self.end, "SBUF overflow %d" % (self.cur + sz - self.end)
        self.n += 1
        t = self.nc.alloc_sbuf_tensor_at('t%d' % self.n, list(shape), dtype, offset=self.cur)
        self.cur += sz
        return t

    def mark(self):
        return self.cur

    def reset(self, m):
        self.cur = m


def build_program(debug=False, stop_after=99):
    nc = bass.Bass("TRN2", target_bir_lowering=False)
    es = contextlib.ExitStack()

    def din(name, shape, dt=F32):
        return nc.dram_tensor(name, list(shape), dt, kind="ExternalInput").ap()

    def dscr(name, shape, dt=F32):
        kind = "ExternalOutput" if debug else "Internal"
        return nc.dram_tensor(name, list(shape), dt, kind=kind).ap()

    x_d = din("x", [T, D]); ctx_d = din("ctx", [TC, D]); pos_d = din("pos", [T, D])
    cvec_d = din("cvec", [128, 16]); wada_d = din("w_ada", [D, 6 * D]); bada_d = din("b_ada_fm", [128, 48])
    n1g_d = din("n1g_fm", [128, 8]); n2g_d = din("n2g_fm", [128, 8]); fg_d = din("final_g", [1, D])
    win_d = din("w_in", [D, 4608]); wfour_d = din("w_four", [512, D]); wlru_d = din("w_lru", [D, D]); wout_d = din("w_out", [D, D])
    cw_d = din("conv_w_fm", [128, 32]); cb_d = din("conv_b_fm", [128, 8]); lam_d = din("lam_fm", [128, 16])
    ba_d = din("ba_fm", [128, 16]); bi_d = din("bi_fm", [128, 16])
    wa_d = din("lru_wa", [16, 128, 128]); wi_d = din("lru_wi", [16, 128, 128])
    wr_d = din("w_router_fm", [128, 128])
    wg_d = din("w_gate_e", [NE, D, DF]); wu_d = din("w_up_e", [NE, D, DF]); wd_d = din("w_down_e", [NE, DF, D])
    cs128_d = din("cs128", [128, 256], BF); e1_d = din("e1", [128, 32 * 512], BF); e2_d = din("e2", [128, 256], BF)
    identf_d = din("identf", [128, 128]); identb_d = din("identb", [128, 128], BF)
    iotac_d = din("iota_c", [128, 512]); cval_d = din("cval", [128, 4]); jcol_d = din("jcol", [128, 1])
    out_d = nc.dram_tensor("out", [T, D], F32, kind="ExternalOutput").ap()

    zf_d = dscr("zf_s", [512, T], BF); xr_d = dscr("xr_s", [D, TT], BF); gg_d = dscr("gg_s", [D, T], BF)
    sa_d = dscr("sa_s", [D, T], BF); sb_d = dscr("sb_s", [D, T], BF); yin_d = dscr("yin_s", [D, T], BF)
    yf_d = dscr("yf_s", [512, T], BF); x1_d = dscr("x1_s", [T, D], BF); xn2_d = dscr("xn2_s", [T, D], BF)
    sc_d = dscr("sc_s", [T, NE]); cs_d = dscr("cs_s", [NE, T]); m_d = dscr("m_s", [NE, T]); y_d = dscr("y_s", [T, D])
    idx_dbg = dscr("idx_s", [128, 64])

    P = Prog(nc, es)
    sb = SBAlloc(nc)
    PS = [nc.alloc_psum_tensor('ps%d' % i, [128, 1024], F32) for i in range(4)]
    pst = {'h': 0, 'f': 0}

    def ps_half():
        i = pst['h'] % 8
        pst['h'] += 1
        return PS[i // 2][:, (i % 2) * 512:(i % 2) * 512 + 512], 'ps%d' % i

    def ps_full():
        i = pst['f'] % 4
        pst['f'] += 1
        return PS[i][:, :], ['ps%d' % (2 * i), 'ps%d' % (2 * i + 1)]

    def dma(queue, out, in_, reads, writes, **kw):
        P.op(queue, lambda e, out=out, in_=in_, kw=kw: e.dma_start(out=out, in_=in_, **kw), reads, writes, dma=True)

    def mm(out_ap, pairs, reads, writes):
        def fn(e, out_ap=out_ap, pairs=pairs):
            n = len(pairs)
            for i, (l, r) in enumerate(pairs):
                ins = e.matmul(out_ap, l, r, start=(i == 0), stop=(i == n - 1))
            return ins
        P.op('pe', fn, reads, writes)

    def tr(out_ap, in_ap, ident, reads, writes):
        P.op('pe', lambda e, o=out_ap, i=in_ap, idn=ident: e.transpose(o, i, idn), reads, writes)

    def act(out, in_, func, reads, writes, eng='act', **kw):
        P.op('act', lambda e, out=out, in_=in_, func=func, kw=kw: e.activation(out=out, in_=in_, func=func, **kw), reads, writes)

    def V(eng, meth, reads, writes, *a, **kw):
        P.op(eng, lambda e, meth=meth, a=a, kw=kw: getattr(e, meth)(*a, **kw), reads, writes)

    identf = sb.alloc([128, 128], F32); identb = sb.alloc([128, 128], BF)
    ADA = sb.alloc([128, 96], F32)
    scale1 = sb.alloc([128, 8], F32); cscale1 = sb.alloc([128, 8], F32); scale2 = sb.alloc([128, 8], F32)
    n1g = sb.alloc([128, 8], F32); n2g = sb.alloc([128, 8], F32)
    dma('sp', identf[:], identf_d, [], ['identf']); dma('sp', identb[:], identb_d, [], ['identb'])
    dma('sp', n1g[:], n1g_d, [], ['n1g']); dma('sp', n2g[:], n2g_d, [], ['n2g'])

    def ada(t, k=None, ctx=False):
        base = 48 if ctx else 0
        if k is None:
            return ADA[:, base + t * 8: base + t * 8 + 8]
        return ADA[:, base + t * 8 + k: base + t * 8 + k + 1]

    m0 = sb.mark()
    cv = sb.alloc([128, 16], F32); sg = sb.alloc([128, 16], F32); scv = sb.alloc([128, 16], F32)
    bada = sb.alloc([128, 48], F32)
    wst = [sb.alloc([128, 8, 512], F32) for _ in range(3)]
    wbf0 = [sb.alloc([128, 8, 512], BF) for _ in range(2)]
    dma('sp', cv[:], cvec_d, [], ['cv']); dma('sp', bada[:], bada_d, [], ['bada'])
    act(sg[:], cv[:], AF.Sigmoid, ['cv'], ['sg'])
    scvb = sb.alloc([128, 16], BF)
    V('dve', 'tensor_tensor', ['cv', 'sg'], ['scv'], scvb[:], cv[:], sg[:], ALU.mult)
    pada, kada = ps_half()
    scv3 = scvb[:].rearrange("p (c k) -> p c k", k=8)
    for pc in range(12):
        w = wst[pc % 3]; wb = wbf0[pc % 2]; wk = 'wst%d' % (pc % 3); wbk = 'wbf%d' % (pc % 2)
        dma('sp', w[:], wada_d[:, pc * 512:(pc + 1) * 512].rearrange("(k p) e -> p k e", p=128), [], [wk])
        act(wb[:, 0:4, :], w[:, 0:4, :], AF.Copy, [wk], [wbk])
        V('dve', 'tensor_copy', [wk, wbk], [wbk], wb[:, 4:8, :], w[:, 4:8, :])
        for jj in range(4):
            j = pc * 4 + jj
            mm(pada[:, 2 * j:2 * j + 2], [(wb[:, k, jj * 128:(jj + 1) * 128], scv3[:, :, k]) for k in range(8)],
               [wbk, 'scv'], [kada])
    pv = pada[:, 0:96].rearrange("p (j c) -> p c j", c=2)
    V('dve', 'tensor_tensor', [kada, 'bada'], ['ADA'], ADA[:, 0:48], pv[:, 0, :], bada[:], ALU.add)
    V('dve', 'tensor_tensor', [kada, 'bada', 'ADA'], ['ADA'], ADA[:, 48:96], pv[:, 1, :], bada[:], ALU.add)
    V('dve', 'scalar_tensor_tensor', ['ADA', 'n1g'], ['scale1'], scale1[:], ada(1), 1.0, n1g[:], ALU.add, ALU.mult)
    V('dve', 'scalar_tensor_tensor', ['ADA', 'n1g'], ['cscale1'], cscale1[:], ada(1, ctx=True), 1.0, n1g[:], ALU.add, ALU.mult)
    V('dve', 'scalar_tensor_tensor', ['ADA', 'n2g'], ['scale2'], scale2[:], ada(4), 1.0, n2g[:], ALU.add, ALU.mult)
    P.barrier()
    sb.reset(m0)

    def rms_tile(xt, xkey, ss, sskey, junk):
        act(junk[:], xt[:], AF.Square, [xkey], ['junk', sskey], accum_out=ss[:])
        act(ss[:], ss[:], AF.Sqrt, [sskey, 'epsb'], [sskey], scale=1.0 / D, bias=epsb[:])
        V('dve', 'reciprocal', [sskey], [sskey], ss[:], ss[:])

    epsb = sb.alloc([128, 1], F32)
    V('dve', 'memset', [], ['epsb'], epsb[:], EPS)
    zcol = sb.alloc([128, 1], F32)
    V('dve', 'memset', [], ['zcol'], zcol[:], 0.0)
    m1 = sb.mark()
    hT = sb.alloc([128, 8, TT], BF)
    xts = [sb.alloc([128, D], F32) for _ in range(4)]
    pts = [sb.alloc([128, D], F32) for _ in range(3)]
    xn16 = [sb.alloc([128, D], BF) for _ in range(4)]
    junk = sb.alloc([128, D], BF)
    sss = [sb.alloc([128, 1], F32) for _ in range(4)]
    for pr_ in range(17):
        pi = pr_ % 4
        pbf = PS[pi][:, :].bitcast(BF)
        tokp = pr_ * 256
        isctx = (pr_ == 0)
        for t_ in range(2):
            i = pr_ * 2 + t_
            xt = xts[i % 4]; xk = 'xt%d' % (i % 4); ss = sss[i % 4]; sk = 'ss%d' % (i % 4)
            xn = xn16[i % 4]; xnk = 'xn16_%d' % (i % 4)
            if isctx:
                dma('sp', xt[:], ctx_d[i * 128:(i + 1) * 128, :], [], [xk])
            else:
                jx = i - 2
                pt = pts[i % 3]; pk = 'pt%d' % (i % 3)
                dma('sp', xt[:], x_d[jx * 128:(jx + 1) * 128, :], [], [xk])
                dma('sp', pt[:], pos_d[jx * 128:(jx + 1) * 128, :], [], [pk])
                V('pool', 'tensor_tensor', [xk, pk], [xk], xt[:], xt[:], pt[:], ALU.add)
            rms_tile(xt, xk, ss, sk, junk)
            V('dve', 'tensor_scalar', [xk, sk], [xnk], xn[:], xt[:], ss[:, 0:1], None, ALU.mult)
            for k in range(8):
                c0 = k * 256 + t_ * 128
                tr(pbf[:, c0:c0 + 128], xn[:, k * 128:(k + 1) * 128], identb[:], [xnk, 'identb'], ['ps%d' % (2 * pi + k // 4)])
        scl = cscale1 if isctx else scale1
        for k in range(8):
            c0 = k * 256
            pkey = 'ps%d' % (2 * pi + k // 4)
            if k < 4:
                act(hT[:, k, tokp:tokp + 256], pbf[:, c0:c0 + 256], AF.Identity,
                    [pkey, 'scale1', 'cscale1', 'ADA'], ['hT%d_%d' % (pr_, k)], scale=scl[:, k:k + 1], bias=ada(0, k, ctx=isctx))
            else:
                V('dve', 'tensor_scalar', [pkey, 'scale1', 'cscale1', 'ADA'], ['hT%d_%d' % (pr_, k)], hT[:, k, tokp:tokp + 256], pbf[:, c0:c0 + 256],
                  scl[:, k:k + 1], ada(0, k, ctx=isctx), ALU.mult, ALU.add)
    hT_keys = ['hT%d_%d' % (i, k) for i in range(17) for k in range(8)]

    wst = [sb.alloc([128, 8, 512], F32) for _ in range(2)]
    wbf = [sb.alloc([128, 8, 512], BF) for _ in range(2)]
    ost = [sb.alloc([128, TT], BF) for _ in range(2)]
    gt = [sb.alloc([128, 512], F32) for _ in range(6)]
    gz = [sb.alloc([128, 512], F32) for _ in range(6)]
    gctr = [0]
    gpend = []
    chunks = [(0, TC)] + [(TC + 512 * c, 512) for c in range(8)]
    ecount = 0
    def p1_dma(pc):
        dma('sp', wst[pc % 2][:], win_d[:, pc * 512:(pc + 1) * 512].rearrange("(k p) e -> p k e", p=128), [], ['wst%d' % (pc % 2)])

    def p1_cast(pc):
        w = wst[pc % 2]; wb = wbf[pc % 2]; wk = 'wst%d' % (pc % 2); wbk = 'wbf%d' % (pc % 2)
        act(wb[:, 0:4, :], w[:, 0:4, :], AF.Copy, [wk], [wbk])
        V('dve', 'tensor_copy', [wk, wbk], [wbk], wb[:, 4:8, :], w[:, 4:8, :])

    p1_dma(0)
    p1_cast(0)
    p1_dma(1)
    for pc in range(9):
        wb = wbf[pc % 2]; wbk = 'wbf%d' % (pc % 2)
        for jj in range(4):
            ec = pc * 4 + jj
            typ = 'F' if ec < 4 else 'XR' if ec < 12 else 'GG' if ec < 20 else 'SA' if ec < 28 else 'SB'
            o = ost[ecount % 2]; ok = 'ost%d' % (ecount % 2); ecount += 1
            ob = o[:]
            for ci, (t0, n) in enumerate(chunks):
                if ci == 0 and typ != 'XR':
                    continue
                ph, pk = ps_half()
                mm(ph[:, 0:n], [(wb[:, k, jj * 128:(jj + 1) * 128], hT[:, k, t0:t0 + n]) for k in range(8)],
                   [wbk] + hT_keys, [pk])
                if typ == 'XR':
                    act(ob[:, t0:t0 + n], ph[:, 0:n], AF.Copy, [pk], [ok])
                elif typ == 'F':
                    cF = (t0 - TC) // 512
                    zdst = ob[:, 0:T].rearrange("p (b a) -> p a b", a=64)[:, 8 * cF:8 * cF + 8, :]
                    act(zdst, ph[:, 0:n].rearrange("p (a b) -> p a b", b=64), AF.Copy, [pk], [ok])
                elif typ in ('SA', 'SB'):
                    act(ob[:, t0 - TC:t0 - TC + n], ph[:, 0:n], AF.Sigmoid, [pk], [ok])
                else:
                    gi = gctr[0] % 6; gctr[0] += 1
                    g0 = gt[gi]; gk = 'gt%d' % gi; zc = gz[gi]; zk = 'gz%d' % gi
                    act(zc[:], ph, AF.Copy, [pk], [zk])
                    act(g0[:], zc[:], AF.Square, [zk], [gk], scale=0.21145921593541212)
                    V('dve', 'scalar_tensor_tensor', [gk, zk], [gk], g0[:], g0[:], 1.0, zc[:], ALU.add, ALU.mult)
                    def fin(g0=g0, gk=gk, zc=zc, zk=zk, ob=ob, ok=ok, a=t0 - TC, n=n):
                        act(g0[:], g0[:], AF.Sigmoid, [gk], [gk], scale=1.5957691216057308)
                        V('dve', 'tensor_tensor', [gk, zk], [ok], ob[:, a:a + n], g0[:], zc[:], ALU.mult)
                    gpend.append(fin)
                    if len(gpend) > 2:
                        gpend.pop(0)()
            while gpend:
                gpend.pop(0)()
            if typ == 'F':
                dma('pool', zf_d[ec * 128:(ec + 1) * 128, :], ob[:, 0:T], [ok], ['zf_d'])
            elif typ == 'XR':
                dma('pool', xr_d[(ec - 4) * 128:(ec - 3) * 128, :], ob[:, 0:TT], [ok], ['xr_d'])
            else:
                dd = {'GG': gg_d, 'SA': sa_d, 'SB': sb_d}[typ]
                e0 = (ec - 12) % 8
                dma('pool', dd[e0 * 128:(e0 + 1) * 128, :], ob[:, 0:T], [ok], [typ + '_d'])
        if pc + 1 < 9:
            p1_cast(pc + 1)
        if pc + 2 < 9:
            p1_dma(pc + 2)
    P.barrier()
    sb.reset(m1)
    if stop_after <= 1:
        return finish(nc, es, P)

    m2 = sb.mark()
    cw = sb.alloc([128, 32], F32); cb = sb.alloc([128, 8], F32); lam = sb.alloc([128, 16], F32)
    ba = sb.alloc([128, 16], F32); bi = sb.alloc([128, 16], F32); nsp = sb.alloc([128, 16], F32)
    dma('sp', cw[:], cw_d, [], ['cw']); dma('sp', cb[:], cb_d, [], ['cb']); dma('sp', lam[:], lam_d, [], ['lam'])
    dma('sp', ba[:], ba_d, [], ['ba']); dma('sp', bi[:], bi_d, [], ['bi'])
    act(nsp[:], lam[:], AF.Exp, ['lam'], ['nsp'], scale=-1.0)
    act(nsp[:], nsp[:], AF.Ln, ['nsp'], ['nsp'], bias=1.0)
    V('dve', 'tensor_scalar', ['nsp'], ['nsp'], nsp[:], nsp[:], -8.0, None, ALU.mult)
    WA = sb.alloc([128, 16, 128], BF); WI = sb.alloc([128, 16, 128], BF)
    DG = sb.alloc([128, 32, 128], BF)
    ggt = sb.alloc([128, T], BF); yint = sb.alloc([128, T], BF)
    wgs = yint[:].bitcast(F32).rearrange("p (m j) -> p m j", j=128)
    dma('sp', wgs, wa_d.rearrange("m i j -> i m j"), [], ['yint'])
    act(WA[:], wgs, AF.Copy, ['yint'], ['WA'])
    dma('sp', wgs, wi_d.rearrange("m i j -> i m j"), ['yint'], ['yint'])
    act(WI[:], wgs, AF.Copy, ['yint'], ['WI'])
    for h in range(8):
        for k in range(4):
            V('dve', 'tensor_scalar', ['identf', 'cw'], ['DG'], DG[:, h * 4 + k, :], identf[:], cw[:, h * 4 + k:h * 4 + k + 1], None, ALU.mult)
    XPW = 4448
    xpads = [sb.alloc([128, XPW], BF)] * 2
    Ubs = [sb.alloc([128, TT], BF) for _ in range(2)]
    Rb = sb.alloc([128, TT], BF)
    Ibs = [sb.alloc([128, TT], BF) for _ in range(3)]
    Abs = [sb.alloc([128, TT], F32) for _ in range(3)]
    T1s = [sb.alloc([128, TT], F32) for _ in range(2)]; Y = sb.alloc([128, T], F32); Hc = sb.alloc([128, TC], F32)
    V('pool', 'memset', [], ['xpad0'], xpads[0][:], 0.0)
    NCH = len(chunks)

    def ck(nm, ci):
        return '%s%d' % (nm, ci)

    def conv_load(h):
        xp = xpads[0]; xk = 'xpad0'
        dma('sp', xp[:, 32:32 + TC], xr_d[h * 128:(h + 1) * 128, 0:TC], [], [xk])
        dma('sp', xp[:, 320:320 + T], xr_d[h * 128:(h + 1) * 128, TC:TT], [], [xk])

    conv_ps = {}

    def conv_mm(h, ci):
        xp = xpads[0]; xk = 'xpad0'
        t0, n = chunks[ci]
        base = 32 if ci == 0 else 320
        s_ = t0 if ci == 0 else t0 - TC
        ph, pk = ps_half()
        mm(ph[:, 0:n], [(DG[:, h * 4 + k, :], xp[:, base + s_ + k - 2: base + s_ + k - 2 + n]) for k in range(4)],
           ['DG', xk], [pk])
        conv_ps[(h, ci)] = (ph, pk)

    def conv_ev(h, ci):
        t0, n = chunks[ci]
        ph, pk = conv_ps.pop((h, ci))
        V('dve', 'tensor_scalar', [pk, 'cb'], ['Ub%d_%d' % (h % 2, ci)], Ubs[h % 2][:, t0:t0 + n], ph[:, 0:n], cb[:, h:h + 1], None, ALU.add)

    conv_load(0)
    for ci in range(NCH):
        conv_mm(0, ci)
        conv_ev(0, ci)
    ggts = [ggt, ggt]
    yints = [yint]

    def unit_ctx(u):
        h = u // 2; d = u % 2
        return h, d, d * 8 + h, Ubs[h % 2], Ibs[u % 3], Abs[u % 3], T1s[d]

    def ubk_(h, ci):
        return 'Ub%d_%d' % (h % 2, ci)

    def sG(u):
        h, d, col, ub, Ib, Ab, T1 = unit_ctx(u)
        for ci, (t0, n) in enumerate(chunks):
            ph, pk = ps_half()
            mm(ph[:, 0:n], [(WA[:, col, :], ub[:, t0:t0 + n])], ['WA', ubk_(h, ci)], [pk])
            act(Rb[:, t0:t0 + n], ph[:, 0:n], AF.Sigmoid, [pk, 'ba'], [ck('R', ci)], bias=ba[:, col:col + 1], scale=1.0)
            ph, pk = ps_half()
            mm(ph[:, 0:n], [(WI[:, col, :], ub[:, t0:t0 + n])], ['WI', ubk_(h, ci)], [pk])
            act(Ib[:, t0:t0 + n], ph[:, 0:n], AF.Sigmoid, [pk, 'bi'], ['I%d_%d' % (u % 3, ci)], bias=bi[:, col:col + 1], scale=1.0)

    cgroups = [[0, 1, 2], [3, 4, 5], [6, 7, 8]]

    def grange(g_):
        a0 = chunks[g_[0]][0]
        a1 = chunks[g_[-1]][0] + chunks[g_[-1]][1]
        return a0, a1

    def sE(u):
        h, d, col, ub, Ib, Ab, T1 = unit_ctx(u)
        for g_ in cgroups:
            a0, a1 = grange(g_)
            act(Ab[:, a0:a1], Rb[:, a0:a1], AF.Exp, [ck('R', ci) for ci in g_] + ['nsp'], ['A%d_%d' % (u % 3, ci) for ci in g_], scale=nsp[:, col:col + 1])

    def sQ(u):
        h, d, col, ub, Ib, Ab, T1 = unit_ctx(u)
        for ci, (t0, n) in enumerate(chunks):
            V('pool', 'tensor_tensor', ['I%d_%d' % (u % 3, ci), ubk_(h, ci)], ['I%d_%d' % (u % 3, ci)], Ib[:, t0:t0 + n], Ib[:, t0:t0 + n], ub[:, t0:t0 + n], ALU.mult)
        for g_ in cgroups:
            a0, a1 = grange(g_)
            act(T1[:, a0:a1], Ab[:, a0:a1], AF.Square, ['A%d_%d' % (u % 3, ci) for ci in g_], ['T%d_%d' % (d, ci) for ci in g_])

    def sR(u):
        h, d, col, ub, Ib, Ab, T1 = unit_ctx(u)
        for g_ in cgroups:
            a0, a1 = grange(g_)
            act(T1[:, a0:a1], T1[:, a0:a1], AF.Sqrt, ['T%d_%d' % (d, ci) for ci in g_], ['T%d_%d' % (d, ci) for ci in g_], scale=-1.0, bias=1.0)

    def sBm(u):
        h, d, col, ub, Ib, Ab, T1 = unit_ctx(u)
        for ci, (t0, n) in enumerate(chunks):
            V('dve', 'tensor_tensor', ['I%d_%d' % (u % 3, ci), 'T%d_%d' % (d, ci)], ['I%d_%d' % (u % 3, ci)], Ib[:, t0:t0 + n], Ib[:, t0:t0 + n], T1[:, t0:t0 + n], ALU.mult)

    def sSC(u):
        h, d, col, ub, Ib, Ab, T1 = unit_ctx(u)
        allA = ['A%d_%d' % (u % 3, ci) for ci in range(NCH)]; allI = ['I%d_%d' % (u % 3, ci) for ci in range(NCH)]; allT = ['T%d_%d' % (d, ci) for ci in range(NCH)]
        if d == 0:
            V('dve', 'tensor_tensor_scan', [allA[0], allI[0]], ['Hc'], Hc[:], Ab[:, 0:TC], Ib[:, 0:TC], 0.0, ALU.mult, ALU.add)
            V('dve', 'tensor_tensor_scan', allA[1:] + allI[1:] + ['Hc'], ['Y'], Y[:], Ab[:, TC:TT], Ib[:, TC:TT], Hc[:, TC - 1:TC], ALU.mult, ALU.add)
        else:
            V('dve', 'tensor_tensor_scan', [allA[0], allI[0]], ['Hc'], Hc[:, ::-1], Ab[:, 0:TC][:, ::-1], Ib[:, 0:TC][:, ::-1], 0.0, ALU.mult, ALU.add)
            V('dve', 'tensor_tensor_scan', allA[1:] + allI[1:] + allT[1:] + ['Hc'], allT[1:], T1[:, TC:TT][:, ::-1], Ab[:, TC:TT][:, ::-1], Ib[:, TC:TT][:, ::-1],
              Hc[:, 0:1], ALU.mult, ALU.add)
            V('pool', 'tensor_tensor', allT[1:] + ['Y'], ['Y'], Y[:], Y[:], T1[:, TC:TT], ALU.add)
            gg = ggts[0]; ggk = 'ggt0'
            V('pool', 'tensor_tensor', ['Y', ggk], ['yint'], yints[0][:], Y[:], gg[:], ALU.mult)
            dma('pool', yin_d[h * 128:(h + 1) * 128, :], yints[0][:], ['yint'], ['yin_d'])
            if h + 1 < 8:
                head_prefetch(h + 1)

    def head_prefetch(h):
        dma('pool', ggts[0][:], gg_d[h * 128:(h + 1) * 128, :], [], ['ggt0'])

    head_prefetch(0)
    sG(0); sE(0); sQ(0)
    for u in range(16):
        h = u // 2
        if u % 2 == 0 and h + 1 < 8:
            conv_load(h + 1)
        if u + 1 < 16:
            sG(u + 1); sE(u + 1); sQ(u + 1)
        if u % 2 == 0 and h + 1 < 8:
            for ci in range(NCH):
                conv_mm(h + 1, ci)
                conv_ev(h + 1, ci)
        sR(u); sBm(u); sSC(u)
    P.barrier()
    sb.reset(m2)
    if stop_after <= 2:
        return finish(nc, es, P)

    m3 = sb.mark()
    CS = sb.alloc([128, 256], BF); E1 = sb.alloc([128, 32, 512], BF); E2 = sb.alloc([128, 256], BF)
    dma('sp', CS[:], cs128_d, [], ['CS']); dma('sp', E1[:], e1_d.rearrange("p (m c) -> p m c", c=512), [], ['E1'])
    dma('sp', E2[:], e2_d, [], ['E2'])
    ZF = [sb.alloc([128, T], BF) for _ in range(2)]
    Bfm = sb.alloc([128, 2, T], BF)
    Yfm = [sb.alloc([128, T], BF) for _ in range(2)]
    NR = 8
    Zt = [sb.alloc([128, 256], BF) for _ in range(NR)]
    Bt = [sb.alloc([128, 2, 128], BF) for _ in range(NR)]

    def pipeline(n, stages, lags):
        for step in range(n + lags[-1]):
            for st_, lag in zip(stages, lags):
                i_ = step - lag
                if 0 <= i_ < n:
                    st_(i_)

    for g in range(4):
        zf = ZF[g % 2]; zk = 'ZF%d' % (g % 2); yf = Yfm[g % 2]; yk = 'Yfm%d' % (g % 2)
        dma('sp', zf[:], zf_d[g * 128:(g + 1) * 128, :], [], [zk])
        hold = {}

        def sC(m):
            ph, pk = ps_half()
            mm(ph[:, 0:256], [(zf[:, 128 * m:128 * m + 128], CS[:])], [zk, 'CS'], [pk])
            hold[('c', m)] = (ph, pk)

        def sZ(m):
            z1 = Zt[m % NR]; z1k = 'Zt%d' % (m % NR)
            ph, pk = hold.pop(('c', m))
            if m % 2 == 0:
                act(z1[:], ph[:, 0:256], AF.Copy, [pk], [z1k])
            else:
                V('dve', 'tensor_copy', [pk], [z1k], z1[:], ph[:, 0:256])

        def sS(m):
            z1 = Zt[m % NR]; z1k = 'Zt%d' % (m % NR)
            ph, pk = ps_half()
            mm(ph[:, 0:256], [(z1[:, 0:128], E1[:, m, 0:256]), (z1[:, 128:256], E1[:, m, 256:512])], [z1k, 'E1'], [pk])
            hold[('s', m)] = (ph, pk)

        def sB(m):
            ph, pk = hold.pop(('s', m))
            for c in range(2):
                bdst = Bfm[:, c, :].rearrange("p (k t) -> p t k", t=64)[:, 2 * m:2 * m + 2, :]
                bsrc = ph[:, c * 128:(c + 1) * 128].rearrange("p (l k) -> p l k", l=2)
                if c == 0:
                    act(bdst, bsrc, AF.Copy, [pk], ['Bfm%d' % m])
                else:
                    V('dve', 'tensor_copy', [pk, 'Bfm%d' % m], ['Bfm%d' % m], bdst, bsrc)

        pipeline(32, [sC, sZ, sS, sB], LAGS)
        allB = ['Bfm%d' % m for m in range(32)]

        def sT(n):
            ph, pk = ps_half()
            phb = ph.bitcast(BF)
            for c in range(2):
                tr(phb[:, c * 128:(c + 1) * 128], Bfm[:, c, 128 * n:128 * n + 128], identb[:], allB + ['identb'], [pk])
            hold[('t', n)] = (phb, pk)

        def sE(n):
            b1 = Bt[n % NR]; b1k = 'Bt%d' % (n % NR)
            phb, pk = hold.pop(('t', n))
            if n % 2 == 0:
                act(b1[:], phb[:, 0:256].rearrange("p (c n) -> p c n", c=2), AF.Copy, [pk], [b1k])
            else:
                V('dve', 'tensor_copy', [pk], [b1k], b1[:], phb[:, 0:256].rearrange("p (c n) -> p c n", c=2))

        def sM(n):
            b1 = Bt[n % NR]; b1k = 'Bt%d' % (n % NR)
            ph, pk = ps_half()
            mm(ph[:, 0:128], [(b1[:, 0, :], E2[:, 0:128]), (b1[:, 1, :], E2[:, 128:256])], [b1k, 'E2'], [pk])
            hold[('m', n)] = (ph, pk)

        def sY(n):
            ph, pk = hold.pop(('m', n))
            dst = yf[:].rearrange("p (a b) -> p b a", b=64)[:, 2 * n:2 * n + 2, :]
            if n % 2 == 1:
                act(dst, ph[:, 0:128].rearrange("p (c n) -> p c n", c=2), AF.Copy, [pk], [yk])
            else:
                V('dve', 'tensor_copy', [pk], [yk], dst, ph[:, 0:128].rearrange("p (c n) -> p c n", c=2))

        pipeline(32, [sT, sE, sM, sY], LAGS)
        dma('pool', yf_d[g * 128:(g + 1) * 128, :], yf[:], [yk], ['yf_d'])
    P.barrier()
    sb.reset(m3)
    if stop_after <= 3:
        return finish(nc, es, P)

    Sc = sb.alloc([128, 32, 16], F32); idxs = sb.alloc([128, 64], I32); idx2s = sb.alloc([128, 64], I32)
    m4 = sb.mark()
    S = sb.alloc([128, T], F32)
    wst = [sb.alloc([128, 8, 512], F32) for _ in range(1)]
    WF = sb.alloc([128, 4, D], BF); WL = sb.alloc([128, 8, D], BF); WO = sb.alloc([128, 8, D], BF)
    WRf = sb.alloc([128, 8, 16], F32); WR = sb.alloc([128, 8, 16], BF)
    dma('sp', WRf[:], wr_d.rearrange("p (k e) -> p k e", e=16), [], ['WRf'])
    V('dve', 'tensor_copy', ['WRf'], ['WR'], WR[:], WRf[:])
    cnt = 0
    for (src, dstw, nk) in [(wfour_d, WF, 4), (wlru_d, WL, 8), (wout_d, WO, 8)]:
        for half in range(2):
            w = wst[0]; wk = 'wst0'; cnt += 1
            dma('sp', w[:, 0:nk, :], src[:, half * 512:(half + 1) * 512].rearrange("(k p) e -> p k e", p=128), [], [wk])
            act(dstw[:, :, half * 512:(half + 1) * 512], w[:, 0:nk, :], AF.Copy, [wk], ['W4'])
    yfc = [sb.alloc([128, 4, 512], BF) for _ in range(2)]
    yic = [sb.alloc([128, 8, 512], BF) for _ in range(2)]
    sac = [sb.alloc([128, 8, 512], BF) for _ in range(2)]
    sbc = [sb.alloc([128, 8, 512], BF) for _ in range(2)]
    mg = sb.alloc([128, 8, 512], BF)
    tm = [sb.alloc([128, 512], BF) for _ in range(4)]
    yg = sb.alloc([128, 8, 512], BF)
    xts = [sb.alloc([128, D], F32) for _ in range(3)]
    pts = [sb.alloc([128, D], F32) for _ in range(2)]
    xnb = [sb.alloc([128, D], BF) for _ in range(2)]
    x1b = [sb.alloc([128, D], BF) for _ in range(3)]
    h2T = [sb.alloc([128, 8, 128], BF) for _ in range(2)]
    junk = sb.alloc([128, D], BF)
    sss = [sb.alloc([128, 1], F32) for _ in range(3)]
    LGP = PS[3][:, 512:1024]
    lgk = 'ps7'
    pst4 = {'h': 0}

    def ps_half6():
        i = pst4['h'] % 6
        pst4['h'] += 1
        return PS[i // 2][:, (i % 2) * 512:(i % 2) * 512 + 512], 'ps%d' % i

    def ps_full3():
        i = pst4['h'] % 6
        if i % 2:
            pst4['h'] += 1
            i = pst4['h'] % 6
        pst4['h'] += 2
        return PS[i // 2][:, :], ['ps%d' % i, 'ps%d' % (i + 1)]

    ygs = [yg, sb.alloc([128, 8, 512], BF)]

    def A_steps(tc):
        b = tc % 2
        t0 = tc * 512
        ygc = ygs[tc % 2]
        steps = []

        def ld():
            dma('sp', yfc[b][:], yf_d[:, t0:t0 + 512].rearrange("(g p) t -> p g t", p=128), [], ['yfc%d' % b])
            dma('sp', yic[b][:], yin_d[:, t0:t0 + 512].rearrange("(g p) t -> p g t", p=128), [], ['yic%d' % b])
            dma('sp', sac[b][:], sa_d[:, t0:t0 + 512].rearrange("(g p) t -> p g t", p=128), [], ['sac%d' % b])
            dma('sp', sbc[b][:], sb_d[:, t0:t0 + 512].rearrange("(g p) t -> p g t", p=128), [], ['sbc%d' % b])
        steps.append(ld)
        for ec in range(8):
            def f(ec=ec):
                pf, pfk = ps_half6()
                mm(pf, [(WF[:, g, ec * 128:(ec + 1) * 128], yfc[b][:, g, :]) for g in range(4)], ['W4', 'yfc%d' % b], [pfk])
                pr, prk = ps_half6()
                mm(pr, [(WL[:, k, ec * 128:(ec + 1) * 128], yic[b][:, k, :]) for k in range(8)], ['W4', 'yic%d' % b], [prk])
                t1 = tm[(ec % 2) * 2]; t2 = tm[(ec % 2) * 2 + 1]; k1_ = 'tm%d' % ((ec % 2) * 2); k2_ = 'tm%d' % ((ec % 2) * 2 + 1)
                V('dve', 'tensor_tensor', [pfk, 'sac%d' % b], [k1_], t1[:], pf, sac[b][:, ec, :], ALU.mult)
                V('dve', 'tensor_tensor', [prk, 'sbc%d' % b], [k2_], t2[:], pr, sbc[b][:, ec, :], ALU.mult)
                V('dve', 'tensor_tensor', [k1_, k2_], ['mg%d' % ec], mg[:, ec, :], t1[:], t2[:], ALU.add)
            steps.append(f)
        for ec in range(8):
            def g_(ec=ec):
                po, pok = ps_half6()
                mm(po, [(WO[:, k, ec * 128:(ec + 1) * 128], mg[:, k, :]) for k in range(8)], ['W4'] + ['mg%d' % k for k in range(8)], [pok])
                act(ygc[:, ec, :], po, AF.Identity, [pok, 'ADA', 'zcol'], ['yg%d_%d' % (tc % 2, ec)], scale=ada(2, ec), bias=zcol[:])
            steps.append(g_)
        return steps

    def T_steps(tc):
        ygc = ygs[tc % 2]
        hold = {}

        def T1(j):
            ti = tc * 4 + j
            bb = ti % 2; b3 = ti % 3
            xt = xts[b3]; xk = 'xt%d' % b3; pt = pts[bb]; pk_ = 'pt%d' % bb
            dma('sp', xt[:], x_d[ti * 128:(ti + 1) * 128, :], [], [xk])
            dma('sp', pt[:], pos_d[ti * 128:(ti + 1) * 128, :], [], [pk_])
            V('pool', 'tensor_tensor', [xk, pk_], [xk], xt[:], xt[:], pt[:], ALU.add)
            ph_, phk = ps_half6()
            pfb = ph_.bitcast(BF)
            for k in range(8):
                tr(pfb[:, k * 128:(k + 1) * 128], ygc[:, k, j * 128:(j + 1) * 128], identb[:], ['yg%d_%d' % (tc % 2, k), 'identb'], [phk])
            V('dve', 'tensor_tensor', [xk, phk], [xk], xt[:], xt[:], pfb, ALU.add)
            act(x1b[b3][:], xt[:], AF.Copy, [xk], ['x1b%d' % b3])
            dma('pool', x1_d[ti * 128:(ti + 1) * 128, :], x1b[b3][:], ['x1b%d' % b3], ['x1_d'])
            ss = sss[b3]; sk = 'ss%d' % b3
            rms_tile(xt, xk, ss, sk, junk)
            V('dve', 'tensor_scalar', [xk, sk], ['xnb%d' % bb], xnb[bb][:], xt[:], ss[:, 0:1], None, ALU.mult)
            dma('pool', xn2_d[ti * 128:(ti + 1) * 128, :], xnb[bb][:], ['xnb%d' % bb], ['xn2_d'])

        def T2(j):
            ti = tc * 4 + j
            bb = ti % 2
            ph_a, phk_a = ps_half6()
            ph_b, phk_b = ps_half6()
            pfa = ph_a.bitcast(BF); pfb2 = ph_b.bitcast(BF)
            for k in range(8):
                dstp = pfa if k < 4 else pfb2
                tr(dstp[:, (k % 4) * 128:(k % 4 + 1) * 128], xnb[bb][:, k * 128:(k + 1) * 128], identb[:], ['xnb%d' % bb, 'identb'], [phk_a if k < 4 else phk_b])
            for k in range(8):
                if k < 4:
                    act(h2T[bb][:, k, :], pfa[:, (k % 4) * 128:(k % 4 + 1) * 128], AF.Identity, [phk_a, 'scale2', 'ADA'], ['h2Ta%d' % bb],
                        scale=scale2[:, k:k + 1], bias=ada(3, k))
                else:
                    V('dve', 'tensor_scalar', [phk_b, 'scale2', 'ADA'], ['h2Tb%d' % bb], h2T[bb][:, k, :], pfb2[:, (k % 4) * 128:(k % 4 + 1) * 128],
                      scale2[:, k:k + 1], ada(3, k), ALU.mult, ALU.add)

        def R(j):
            ti = tc * 4 + j
            bb = ti % 2
            mm(LGP[:, ti * 16:(ti + 1) * 16], [(h2T[bb][:, k, :], WR[:, k, :]) for k in range(8)], ['h2Ta%d' % bb, 'h2Tb%d' % bb, 'WR'], [lgk])

        order = [(T1, 0), (T1, 1), (T2, 0), (T1, 2), (R, 0), (T2, 1), (T1, 3), (R, 1), (T2, 2), (R, 2), (T2, 3), (R, 3)]
        return [(lambda f=f, j=j: f(j)) for (f, j) in order]

    for st in A_steps(0):
        st()
    for tc in range(8):
        a_ = A_steps(tc + 1) if tc + 1 < 8 else []
        t_ = T_steps(tc)
        ia = it_ = 0
        while ia < len(a_) or it_ < len(t_):
            for _ in range(3):
                if ia < len(a_):
                    a_[ia](); ia += 1
            for _ in range(2):
                if it_ < len(t_):
                    t_[it_](); it_ += 1
    mx = sb.alloc([128, 32], F32)
    lg3 = LGP.rearrange("p (j e) -> p j e", e=16)
    V('dve', 'tensor_reduce', [lgk], ['mx'], mx[:], lg3, AX.X, ALU.max)
    V('dve', 'tensor_tensor', [lgk, 'mx'], ['Sc'], Sc[:], lg3, mx[:].unsqueeze(2).to_broadcast([128, 32, 16]), ALU.subtract)
    act(Sc[:], Sc[:], AF.Exp, ['Sc'], ['Sc'])
    V('dve', 'tensor_reduce', ['Sc'], ['mx'], mx[:], Sc[:], AX.X, ALU.add)
    V('dve', 'reciprocal', ['mx'], ['mx'], mx[:], mx[:])
    V('dve', 'tensor_tensor', ['Sc', 'mx'], ['Sc'], Sc[:], Sc[:], mx[:].unsqueeze(2).to_broadcast([128, 32, 16]), ALU.mult)
    dma('pool', sc_d.rearrange("(p j) e -> p (j e)", p=128), Sc[:].rearrange("p j e -> p (j e)"), ['Sc'], ['sc_d'])
    for q in range(4):
        pf, pfk = ps_full3()
        for jj in range(8):
            j = q * 8 + jj
            tr(pf[0:16, jj * 128:(jj + 1) * 128], Sc[:, j, :], identf[:], ['Sc', 'identf'], [pfk[jj // 4]])
        V('dve', 'tensor_copy', pfk, ['S'], S[0:16, q * 1024:(q + 1) * 1024], pf[0:16, :])
    P.barrier()
    sb.reset(m4)
    if stop_after <= 4:
        return finish(nc, es, P)

    S = sb.alloc([128, T], F32)
    zt = sb.alloc([128, 2048], F32)
    V('pool', 'memset', [], ['zt'], zt[:], 0.0)
    for i in range(16):
        dma('pool', y_d[i * 256:(i + 1) * 256, :].rearrange("(p r) c -> p (r c)", p=128), zt[:], ['zt'], ['y_d'])
    lo = sb.alloc([128, 1], F32); mid = sb.alloc([128, 1], F32); cntt = sb.alloc([128, 1], F32); ge = sb.alloc([128, 1], F32)
    jk = sb.alloc([128, T], BF)
    V('dve', 'memset', [], ['lo'], lo[0:16, :], 0.0)
    for it in range(30):
        half = 2.0 ** (-(it + 1))
        V('dve', 'tensor_scalar', ['lo'], ['mid'], mid[0:16, :], lo[0:16, :], half, None, ALU.add)
        V('dve', 'tensor_scalar', ['S', 'mid'], ['jk', 'cnt'], jk[0:16, :], S[0:16, :], mid[0:16, 0:1], None, ALU.is_ge, ALU.add, cntt[0:16, :])
        V('dve', 'tensor_scalar', ['cnt'], ['ge'], ge[0:16, :], cntt[0:16, :], float(CAP), half, ALU.is_ge, ALU.mult)
        V('dve', 'tensor_tensor', ['ge', 'lo'], ['lo'], lo[0:16, :], lo[0:16, :], ge[0:16, :], ALU.add)
    Mk = sb.alloc([128, T], F32); Cs = sb.alloc([128, T], F32)
    V('dve', 'tensor_scalar', ['S', 'lo'], ['Mk'], Mk[0:16, :], S[0:16, :], lo[0:16, 0:1], None, ALU.is_ge)
    ones = sb.alloc([128, T], BF)
    V('pool', 'memset', [], ['ones'], ones[0:16, :], 1.0)
    V('dve', 'tensor_tensor_scan', ['Mk', 'ones'], ['Cs'], Cs[0:16, :], ones[0:16, :], Mk[0:16, :], 0.0, ALU.mult, ALU.add)
    dma('sp', cs_d, Cs[0:16, :], ['Cs'], ['cs_d'])
    dma('sp', m_d, Mk[0:16, :], ['Mk'], ['m_d'])
    CSJ = sb.alloc([128, 8, 132], F32); M4 = sb.alloc([128, 8, 128], F32)
    V('pool', 'memset', [], ['CSJ'], CSJ[:], 0.0)
    iotac = sb.alloc([128, 512], F32); cval = sb.alloc([128, 4], F32)
    dma('sp', CSJ[0:64, :, 0:128], cs_d.rearrange("e (j p) -> (e j) p", p=128).rearrange("(q r) p -> r q p", r=64), ['cs_d', 'CSJ'], ['CSJ'])
    dma('sp', M4[0:64, :, :], m_d.rearrange("e (j p) -> (e j) p", p=128).rearrange("(q r) p -> r q p", r=64), ['m_d'], ['M4'])
    for q in range(8):
        dma('sp', CSJ[0:64, q, 128:129], jcol_d[0:64, :], ['CSJ'], ['CSJ'])
    dma('sp', iotac[:], iotac_d, [], ['iotac']); dma('sp', cval[:], cval_d, [], ['cval'])
    hi4 = sb.alloc([128, 8], F32); lo4 = sb.alloc([128, 8], F32)
    J4 = sb.alloc([128, 8, 512], F32); tj = sb.alloc([128, 512], F32)
    V('dve', 'tensor_copy', ['CSJ'], ['hi4'], hi4[0:64, :], CSJ[0:64, :, 127])
    V('dve', 'tensor_tensor', ['CSJ', 'M4'], ['lo4'], lo4[0:64, :], CSJ[0:64, :, 0], M4[0:64, :, 0], ALU.subtract)
    for q in range(8):
        V('dve', 'tensor_scalar', ['iotac', 'lo4'], ['tj'], tj[0:64, :], iotac[0:64, :], lo4[0:64, q:q + 1], None, ALU.is_ge)
        V('dve', 'scalar_tensor_tensor', ['iotac', 'hi4', 'tj'], ['J4'], J4[0:64, q, :], iotac[0:64, :], hi4[0:64, q:q + 1], tj[0:64, :], ALU.is_lt, ALU.mult)
    idxf = sb.alloc([128, 64], F32); rr = sb.alloc([128, 64], F32); idx2f = sb.alloc([128, 64], F32)
    jk3 = sb.alloc([128, 128], F32)
    for e in range(NE):
        q = e // 2; r0 = (e % 2) * 32
        for g in range(4):
            col = e * 4 + g
            ph, pk = ps_half6()
            mm(ph[:, 0:130], [(J4[r0:r0 + 32, q, g * 128:(g + 1) * 128], CSJ[r0:r0 + 32, q, 0:130])], ['J4', 'CSJ'], [pk])
            V('dve', 'tensor_scalar', [pk, 'cval'], ['jk3', 'rr'], jk3[:], ph[:, 0:128], cval[:, g:g + 1], None, ALU.is_le, ALU.add, rr[:, col:col + 1])
            V('dve', 'scalar_tensor_tensor', [pk, 'rr'], ['idxf'], idxf[:, col:col + 1], ph[:, 128:129], 128.0, rr[:, col:col + 1], ALU.mult, ALU.add)
            V('dve', 'scalar_tensor_tensor', [pk, 'rr'], ['idx2f'], idx2f[:, col:col + 1], rr[:, col:col + 1], 32.0, ph[:, 128:129], ALU.mult, ALU.add)
    V('dve', 'tensor_copy', ['idxf'], ['idxs'], idxs[:], idxf[:])
    V('dve', 'tensor_copy', ['idx2f'], ['idx2s'], idx2s[:], idx2f[:])
    if debug:
        dma('sp', idx_dbg, idxf[:], ['idxf'], ['idx_dbg'])
    P.barrier()
    sb.reset(m4)
    if stop_after <= 5:
        return finish(nc, es, P)

    m6 = sb.mark()
    wst = [sb.alloc([128, 8, 512], F32) for _ in range(4)]
    wbf = [sb.alloc([128, 8, 512], BF) for _ in range(7)]
    xrow = [sb.alloc([128, D], BF) for _ in range(8)]
    grow = [sb.alloc([128, 16], F32) for _ in range(16)]
    xgT = [sb.alloc([128, 8, 512], BF) for _ in range(2)]
    hid = sb.alloc([128, 12, 512], BF)
    sgt = [sb.alloc([128, 512], BF) for _ in range(2)]
    og = sb.alloc([128, 8, 512], F32)
    orow = [sb.alloc([128, D], F32) for _ in range(4)]
    pieces = []
    for e in range(NE):
        for fq in range(3):
            pieces.append((wg_d[e, :, fq * 512:(fq + 1) * 512].rearrange("(k p) f -> p k f", p=128), 8))
            pieces.append((wu_d[e, :, fq * 512:(fq + 1) * 512].rearrange("(k p) f -> p k f", p=128), 8))
        for dq in range(3):
            pieces.append((wd_d[e, dq * 512:(dq + 1) * 512, :].rearrange("(k p) c -> p k c", p=128), 4))
    loaded = {}
    wctr = {'dma': 0, 'cast': 0}
    LOOK_DMA = 6
    LOOK_CAST = 3

    def ensure_dma(upto):
        while wctr['dma'] <= min(upto, len(pieces) - 1):
            n = wctr['dma']; wctr['dma'] += 1
            src_ap, a = pieces[n]
            i = n % 4
            dma('sp', wst[i][:].rearrange("p a b -> p (a b)").rearrange("p (a b) -> p a b", a=a), src_ap, [], ['wst%d' % i])

    def ensure_cast(upto):
        while wctr['cast'] <= min(upto, len(pieces) - 1):
            n = wctr['cast']; wctr['cast'] += 1
            ensure_dma(n)
            src_ap, a = pieces[n]
            i = n % 4; jj = n % 7
            w = wst[i]; wb = wbf[jj]
            wv = w[:].rearrange("p a b -> p (a b)"); wbv = wb[:].rearrange("p a b -> p (a b)")
            if n % 2 == 0:
                act(wbv, wv, AF.Copy, ['wst%d' % i], ['wbf%d' % jj])
            else:
                V('dve', 'tensor_copy', ['wst%d' % i], ['wbf%d' % jj], wbv, wv)
            loaded[n] = (wb[:].rearrange("p a b -> p (a b)").rearrange("p (a b) -> p a b", a=a), 'wbf%d' % jj)

    def ensure_loaded(upto):
        ensure_cast(upto)

    def get_piece(n):
        ensure_cast(n + LOOK_CAST)
        ensure_dma(n + LOOK_DMA)
        return loaded[n]

    def gathers(e):
        for g in range(4):
            s = (e % 2) * 4 + g
            s3 = (e % 4) * 4 + g
            col = e * 4 + g
            P.op('pool', lambda en, s=s, col=col: en.indirect_dma_start(
                out=xrow[s][:, :], out_offset=None, in_=xn2_d[:, :],
                in_offset=bass.IndirectOffsetOnAxis(ap=idxs[:, col:col + 1], axis=0)), ['idxs', 'xn2_d'], ['xrow%d' % s], dma=True)
            P.op('pool', lambda en, s3=s3, col=col: en.indirect_dma_start(
                out=grow[s3][:, :], out_offset=None, in_=sc_d[:, :],
                in_offset=bass.IndirectOffsetOnAxis(ap=idx2s[:, col:col + 1], axis=0)), ['idx2s', 'sc_d'], ['grow%d' % s3], dma=True)

    def build_xgT(e):
        xg = xgT[e % 2]; xgk = 'xgT%d' % (e % 2)
        for g in range(4):
            s = (e % 2) * 4 + g
            ph, pk = ps_half()
            phb = ph.bitcast(BF)
            for k in range(8):
                tr(phb[:, k * 128:(k + 1) * 128], xrow[s][:, k * 128:(k + 1) * 128], identb[:], ['xrow%d' % s, 'identb'], [pk])
            for k in range(8):
                act(xg[:, k, g * 128:(g + 1) * 128], phb[:, k * 128:(k + 1) * 128], AF.Identity, [pk, 'scale2', 'ADA'], [xgk],
                    scale=scale2[:, k:k + 1], bias=ada(3, k))

    def outT(e):
        for g in range(4):
            s3 = (e % 4) * 4 + g
            col = e * 4 + g
            pf, pfk = ps_full()
            for k in range(8):
                tr(pf[:, k * 128:(k + 1) * 128], og[:, k, g * 128:(g + 1) * 128], identf[:], ['og%d' % k, 'identf'], [pfk[k // 4]])
            orw = orow[g]; ork = 'orow%d' % g
            V('dve', 'tensor_scalar', pfk + ['grow%d' % s3], [ork], orw[:], pf, grow[s3][:, e:e + 1], None, ALU.mult)
            P.op('pool', lambda en, orw=orw, col=col: en.indirect_dma_start(
                out=y_d[:, :], out_offset=bass.IndirectOffsetOnAxis(ap=idxs[:, col:col + 1], axis=0),
                in_=orw[:, :], in_offset=None, compute_op=ALU.add), [ork, 'idxs'], ['y_d'], dma=True)

    ensure_dma(3)
    ensure_cast(3)
    gathers(0)
    gathers(1)
    build_xgT(0)
    for e in range(NE):
        if e + 2 < NE:
            gathers(e + 2)
        xg = xgT[e % 2]; xgk = 'xgT%d' % (e % 2)
        for fq in range(3):
            wgb, wgk = get_piece(e * 9 + fq * 2)
            wub, wuk = get_piece(e * 9 + fq * 2 + 1)
            for fc in range(4):
                f = fq * 4 + fc
                pg, pgk = ps_half()
                mm(pg, [(wgb[:, k, fc * 128:(fc + 1) * 128], xg[:, k, :]) for k in range(8)], [wgk, xgk], [pgk])
                pu, puk = ps_half()
                mm(pu, [(wub[:, k, fc * 128:(fc + 1) * 128], xg[:, k, :]) for k in range(8)], [wuk, xgk], [puk])
                sg_ = sgt[f % 2]; sgk = 'sgt%d' % (f % 2)
                act(sg_[:], pg, AF.Silu, [pgk], [sgk])
                V('dve', 'tensor_tensor', [sgk, puk], ['hid'], hid[:, f, :], sg_[:], pu, ALU.mult)
            if fq == 0 and e >= 1:
                outT(e - 1)
        if e + 1 < NE:
            build_xgT(e + 1)
        wds = [get_piece(e * 9 + 6 + dq) for dq in range(3)]
        for ec in range(8):
            po, pok = ps_half()
            mm(po, [(wds[f // 4][0][:, f % 4, ec * 128:(ec + 1) * 128], hid[:, f, :]) for f in range(12)],
               [wds[0][1], wds[1][1], wds[2][1], 'hid'], [pok])
            act(og[:, ec, :], po, AF.Identity, [pok, 'ADA', 'zcol'], ['og%d' % ec], scale=ada(5, ec), bias=zcol[:])
    outT(NE - 1)
    P.barrier()
    sb.reset(m6)
    if stop_after <= 6:
        return finish(nc, es, P)

    FG = sb.alloc([128, D], F32)
    dma('sp', FG[:], fg_d.partition_broadcast(128), [], ['FG'])
    xbs = [sb.alloc([128, D], BF) for _ in range(6)]
    yts = [sb.alloc([128, D], F32) for _ in range(6)]
    ots = [sb.alloc([128, D], F32) for _ in range(4)]
    junk = sb.alloc([128, D], BF)
    sss = [sb.alloc([128, 1], F32) for _ in range(6)]
    for ti in range(32):
        b = ti % 6
        xb = xbs[b]; xk = 'xb%d' % b; yt = yts[b]; yk = 'yt%d' % b; ss = sss[b]; sk = 'ss%d' % b
        ot = ots[ti % 4]; ok_ = 'ot%d' % (ti % 4)
        dma('sp', xb[:], x1_d[ti * 128:(ti + 1) * 128, :], ['x1_d'], [xk])
        dma('sp', yt[:], y_d[ti * 128:(ti + 1) * 128, :], ['y_d'], [yk])
        V('dve', 'tensor_tensor', [xk, yk], [yk], yt[:], yt[:], xb[:], ALU.add)
        rms_tile(yt, yk, ss, sk, junk)
        V('dve', 'scalar_tensor_tensor', [yk, sk, 'FG'], [ok_], ot[:], yt[:], ss[:, 0:1], FG[:], ALU.mult, ALU.mult)
        dma('pool', out_d[ti * 128:(ti + 1) * 128, :], ot[:], [ok_], ['out_d'])
    return finish(nc, es, P)


def finish(nc, es, P):
    P.barrier()
    with nc.Block() as block:
        @block.tensor
        def _(e):
            P.emit(e, 'pe')

        @block.scalar
        def _(e):
            P.emit(e, 'act')

        @block.vector
        def _(e):
            P.emit(e, 'dve')

        @block.gpsimd
        def _(e):
            P.emit(e, 'pool')

        @block.sync
        def _(e):
            P.emit(e, 'sp')
    es.close()
    return nc


def _consts():
    bf = ml_dtypes.bfloat16
    n = np.arange(128)
    ang = 2 * np.pi * np.outer(n, n) / 128.0
    cs128 = np.concatenate([np.cos(ang), np.sin(ang)], axis=1)
    t1 = np.arange(64); k1 = np.arange(64)
    e1 = np.zeros((32, 128, 512))
    for m in range(32):
        for t2l in range(2):
            t2 = 2 * m + t2l
            ph = 2 * np.pi * (np.outer(t1, k1) / 64.0 + (k1[None, :] * t2) / 4096.0)
            sl = slice(t2l * 64, t2l * 64 + 64)
            e1[m, sl, 0 + t2l * 64:0 + t2l * 64 + 64] = np.cos(ph)
            e1[m, sl, 128 + t2l * 64:128 + t2l * 64 + 64] = np.sin(ph)
            e1[m, sl, 256 + t2l * 64:256 + t2l * 64 + 64] = -np.sin(ph)
            e1[m, sl, 384 + t2l * 64:384 + t2l * 64 + 64] = np.cos(ph)
    e1 = np.ascontiguousarray(e1.transpose(1, 0, 2).reshape(128, 32 * 512))
    s = 1.0 / math.sqrt(4096.0 * 128.0)
    e2 = np.zeros((128, 256))
    t2 = np.arange(64); k2 = np.arange(64)
    ph = 2 * np.pi * np.outer(t2, k2) / 64.0
    for k1l in range(2):
        e2[k1l * 64:k1l * 64 + 64, k1l * 64:k1l * 64 + 64] = np.cos(ph) * s
        e2[k1l * 64:k1l * 64 + 64, 128 + k1l * 64:128 + k1l * 64 + 64] = -np.sin(ph) * s
    quarter = D // 4
    freqs = np.exp(-math.log(10000.0) * np.arange(quarter, dtype=np.float32) / quarter).astype(np.float32)
    ang_r = np.arange(64, dtype=np.float32)[:, None] * freqs
    emb_r = np.concatenate([np.sin(ang_r), np.cos(ang_r)], axis=-1)
    emb = np.concatenate([np.broadcast_to(emb_r[:, None, :], (64, 64, D // 2)),
                          np.broadcast_to(emb_r[None, :, :], (64, 64, D // 2))], axis=-1).reshape(T, D).astype(np.float32)
    return dict(cs128=cs128.astype(bf), e1=e1.astype(bf), e2=e2.astype(bf), pos=np.ascontiguousarray(emb),
                identf=np.eye(128, dtype=np.float32), identb=np.eye(128).astype(bf),
                iota_c=np.ascontiguousarray(np.broadcast_to(np.arange(512, dtype=np.float32)[None, :], (128, 512))),
                cval=(np.arange(4)[None, :] * 128 + np.arange(128)[:, None]).astype(np.float32),
                jcol=(np.arange(128) % 32).astype(np.float32).reshape(128, 1))


def _fm(v):
    return np.ascontiguousarray(np.asarray(v, np.float32).reshape(8, 128).T)


def make_in_maps(inputs, cores):
    f = lambda a: np.ascontiguousarray(np.asarray(a, np.float32))
    cst = _consts()
    l = 0
    shared = dict(cst)
    shared.update(
        w_ada=f(inputs['w_ada'][l]), b_ada_fm=np.ascontiguousarray(f(inputs['b_ada'][l]).reshape(48, 128).T),
        n1g_fm=_fm(inputs['norm1_g'][l]), n2g_fm=_fm(inputs['norm2_g'][l]), final_g=f(inputs['final_g']).reshape(1, D),
        w_in=f(inputs['w_in'][l]), w_four=f(inputs['w_four'][l]), w_lru=f(inputs['w_lru'][l]), w_out=f(inputs['w_out'][l]),
        conv_w_fm=np.ascontiguousarray(f(inputs['conv_w'][l]).reshape(4, 8, 128).transpose(2, 1, 0).reshape(128, 32)),
        conv_b_fm=_fm(inputs['conv_b'][l]),
        lam_fm=np.ascontiguousarray(f(inputs['lru_lambda'][l]).reshape(2, 8, 128).transpose(2, 0, 1).reshape(128, 16)),
        ba_fm=np.ascontiguousarray(f(inputs['lru_ba'][l]).reshape(2, 8, 128).transpose(2, 0, 1).reshape(128, 16)),
        bi_fm=np.ascontiguousarray(f(inputs['lru_bi'][l]).reshape(2, 8, 128).transpose(2, 0, 1).reshape(128, 16)),
        lru_wa=f(inputs['lru_wa'][l]).reshape(16, 128, 128), lru_wi=f(inputs['lru_wi'][l]).reshape(16, 128, 128),
        w_router_fm=np.ascontiguousarray(f(inputs['w_router'][l]).reshape(8, 128, 16).transpose(1, 0, 2).reshape(128, 128)),
        w_gate_e=f(inputs['w_gate_e'][l]), w_up_e=f(inputs['w_up_e'][l]), w_down_e=f(inputs['w_down_e'][l]),
    )
    cctx = _fm(inputs['c_ctx'])
    maps = []
    for b in cores:
        m = dict(shared)
        m['x'] = f(inputs['x'][b]); m['ctx'] = f(inputs['ctx'][b])
        m['cvec'] = np.ascontiguousarray(np.concatenate([_fm(inputs['c'][b]), cctx], axis=1))
        maps.append(m)
    return maps


def kernel(**inputs):
    nc = build_program()
    maps = make_in_maps(inputs, list(range(8)))
    res = run_bass_kernel_spmd(nc, maps, core_ids=list(range(8)))
    return np.stack([np.asarray(r["out"], np.float32) for r in res.results], axis=0)
```

```python
import contextlib
import math
import numpy as np
import ml_dtypes
import concourse.bass as bass
import concourse.mybir as mybir
from concourse.bass_utils import run_bass_kernel_spmd

F32 = mybir.dt.float32
BF = mybir.dt.bfloat16
I32 = mybir.dt.int32
ALU = mybir.AluOpType
AF = mybir.ActivationFunctionType
AX = mybir.AxisListType

T = 4096
TC = 256
TT = T + TC
D = 1024
NE = 16
CAP = 512
DF = 1536
EPS = 1e-6
DEBUG = False
STOP_AFTER = 99
CUT = 0
LAGS = [0, 1, 3, 4]


class Prog:
    def __init__(self, nc, es):
        self.nc = nc
        self.q = {k: [] for k in ['pe', 'act', 'dve', 'pool', 'sp']}
        self.sem = {k: es.enter_context(nc.semaphore('s_' + k)) for k in ['pe', 'act', 'dve', 'pool']}
        self.cnt = {k: 0 for k in self.sem}
        self.dsem = {qn: [es.enter_context(nc.semaphore('d_%s%d' % (qn, i))) for i in range(n)]
                     for qn, n in [('sp', 14), ('pool', 10), ('act', 6)]}
        self.dcnt = {qn: 0 for qn in self.dsem}
        self.dtarget = {}
        self.lastw = {}
        self.readers = {}
        self.waited = {}
        self.semobj = {}

    def _sid(self, s):
        self.semobj[id(s)] = s
        return id(s)

    def op(self, queue, fn, reads=(), writes=(), dma=False):
        deps = set()
        for r in reads:
            if r in self.lastw:
                deps.add(self.lastw[r])
        for w in writes:
            if w in self.lastw:
                deps.add(self.lastw[w])
            for rd in self.readers.get(w, ()):
                deps.add(rd)
        if dma:
            pool = self.dsem[queue]
            i = self.dcnt[queue]
            self.dcnt[queue] += 1
            s = pool[i % len(pool)]
            rnd = i // len(pool)
            ev = (self._sid(s), 16 * (rnd + 1))
            if rnd > 0:
                deps.add((self._sid(s), 16 * rnd))
            inc = (s, 16)
            self.dtarget[self._sid(s)] = 16 * (rnd + 1)
        else:
            self.cnt[queue] += 1
            s = self.sem[queue]
            ev = (self._sid(s), self.cnt[queue])
            inc = (s, 1)
        waits = {}
        for (sid, v) in deps:
            if queue == 'pe' and not dma and sid == id(self.sem['pe']):
                continue
            if self.waited.get((queue, sid), 0) >= v:
                continue
            waits[sid] = max(waits.get(sid, 0), v)
        for sid, v in waits.items():
            self.waited[(queue, sid)] = v
        self.q[queue].append(([(self.semobj[sid], v) for sid, v in waits.items()], fn, inc))
        for r in reads:
            self.readers.setdefault(r, []).append(ev)
        for w in writes:
            self.lastw[w] = ev
            self.readers[w] = []
        return ev

    def barrier(self):
        evs = []
        for k, s in self.sem.items():
            if self.cnt[k] > 0:
                evs.append((self._sid(s), self.cnt[k]))
        for sid, v in self.dtarget.items():
            evs.append((sid, v))
        for queue in self.q:
            waits = []
            for (sid, v) in evs:
                if self.waited.get((queue, sid), 0) >= v:
                    continue
                self.waited[(queue, sid)] = v
                waits.append((self.semobj[sid], v))
            if waits:
                self.q[queue].append((waits, None, None))

    def emit(self, e, queue):
        for waits, fn, inc in self.q[queue]:
            for s, v in waits:
                e.wait_ge(s, v)
            if fn is not None:
                ins = fn(e)
                ins.then_inc(inc[0], inc[1])


class SBAlloc:
    def __init__(self, nc, base=20736, end=229376):
        self.nc = nc
        self.cur = base
        self.end = end
        self.n = 0

    def alloc(self, shape, dtype):
        sz = 1
        for s in shape[1:]:
            sz *= s
        sz *= {F32: 4, BF: 2, I32: 4}[dtype]
        sz = (sz + 63) // 64 * 64
        assert self.cur + sz <= self.end, "SBUF overflow %d" % (self.cur + sz - self.end)
        self.n += 1
        t = self.nc.alloc_sbuf_tensor_at('t%d' % self.n, list(shape), dtype, offset=self.cur)
        self.cur += sz
        return t

    def mark(self):
        return self.cur

    def reset(self, m):
        self.cur = m


def build_program(debug=False, stop_after=99):
    nc = bass.Bass("TRN2", target_bir_lowering=False)
    es = contextlib.ExitStack()

    def din(name, shape, dt=F32):
        return nc.dram_tensor(name, list(shape), dt, kind="ExternalInput").ap()

    def dscr(name, shape, dt=F32):
        kind = "ExternalOutput" if debug else "Internal"
        return nc.dram_tensor(name, list(shape), dt, kind=kind).ap()

    x_d = din("x", [T, D]); ctx_d = din("ctx", [TC, D]); pos_d = din("pos", [T, D])
    cvec_d = din("cvec", [128, 16]); wada_d = din("w_ada", [D, 6 * D]); bada_d = din("b_ada_fm", [128, 48])
    n1g_d = din("n1g_fm", [128, 8]); n2g_d = din("n2g_fm", [128, 8]); fg_d = din("final_g", [1, D])
    win_d = din("w_in", [D, 4608]); wfour_d = din("w_four", [512, D]); wlru_d = din("w_lru", [D, D]); wout_d = din("w_out", [D, D])
    cw_d = din("conv_w_fm", [128, 32]); cb_d = din("conv_b_fm", [128, 8]); lam_d = din("lam_fm", [128, 16])
    ba_d = din("ba_fm", [128, 16]); bi_d = din("bi_fm", [128, 16])
    wa_d = din("lru_wa", [16, 128, 128]); wi_d = din("lru_wi", [16, 128, 128])
    wr_d = din("w_router_fm", [128, 128])
    wg_d = din("w_gate_e", [NE, D, DF]); wu_d = din("w_up_e", [NE, D, DF]); wd_d = din("w_down_e", [NE, DF, D])
    cs128_d = din("cs128", [128, 256], BF); e1_d = din("e1", [128, 32 * 512], BF); e2_d = din("e2", [128, 256], BF)
    identf_d = din("identf", [128, 128]); identb_d = din("identb", [128, 128], BF)
    iotac_d = din("iota_c", [128, 512]); cval_d = din("cval", [128, 4]); jcol_d = din("jcol", [128, 1])
    out_d = nc.dram_tensor("out", [T, D], F32, kind="ExternalOutput").ap()

    zf_d = dscr("zf_s", [512, T], BF); xr_d = dscr("xr_s", [D, TT], BF); gg_d = dscr("gg_s", [D, T], BF)
    sa_d = dscr("sa_s", [D, T], BF); sb_d = dscr("sb_s", [D, T], BF); yin_d = dscr("yin_s", [D, T], BF)
    yf_d = dscr("yf_s", [512, T], BF); x1_d = dscr("x1_s", [T, D], BF); xn2_d = dscr("xn2_s", [T, D], BF)
    sc_d = dscr("sc_s", [T, NE]); cs_d = dscr("cs_s", [NE, T]); m_d = dscr("m_s", [NE, T]); y_d = dscr("y_s", [T, D])
    idx_dbg = dscr("idx_s", [128, 64])

    P = Prog(nc, es)
    sb = SBAlloc(nc)
    PS = [nc.alloc_psum_tensor('ps%d' % i, [128, 1024], F32) for i in range(4)]
    pst = {'h': 0, 'f': 0}

    def ps_half():
        i = pst['h'] % 8
        pst['h'] += 1
        return PS[i // 2][:, (i % 2) * 512:(i % 2) * 512 + 512], 'ps%d' % i

    def ps_full():
        i = pst['f'] % 4
        pst['f'] += 1
        return PS[i][:, :], ['ps%d' % (2 * i), 'ps%d' % (2 * i + 1)]

    def dma(queue, out, in_, reads, writes, **kw):
        P.op(queue, lambda e, out=out, in_=in_, kw=kw: e.dma_start(out=out, in_=in_, **kw), reads, writes, dma=True)

    def mm(out_ap, pairs, reads, writes):
        def fn(e, out_ap=out_ap, pairs=pairs):
            n = len(pairs)
            for i, (l, r) in enumerate(pairs):
                ins = e.matmul(out_ap, l, r, start=(i == 0), stop=(i == n - 1))
            return ins
        P.op('pe', fn, reads, writes)

    def tr(out_ap, in_ap, ident, reads, writes):
        P.op('pe', lambda e, o=out_ap, i=in_ap, idn=ident: e.transpose(o, i, idn), reads, writes)

    def act(out, in_, func, reads, writes, eng='act', **kw):
        P.op('act', lambda e, out=out, in_=in_, func=func, kw=kw: e.activation(out=out, in_=in_, func=func, **kw), reads, writes)

    def V(eng, meth, reads, writes, *a, **kw):
        P.op(eng, lambda e, meth=meth, a=a, kw=kw: getattr(e, meth)(*a, **kw), reads, writes)

    identf = sb.alloc([128, 128], F32); identb = sb.alloc([128, 128], BF)
    ADA = sb.alloc([128, 96], F32)
    scale1 = sb.alloc([128, 8], F32); cscale1 = sb.alloc([128, 8], F32); scale2 = sb.alloc([128, 8], F32)
    n1g = sb.alloc([128, 8], F32); n2g = sb.alloc([128, 8], F32)
    dma('sp', identf[:], identf_d, [], ['identf']); dma('sp', identb[:], identb_d, [], ['identb'])
    dma('sp', n1g[:], n1g_d, [], ['n1g']); dma('sp', n2g[:], n2g_d, [], ['n2g'])

    def ada(t, k=None, ctx=False):
        base = 48 if ctx else 0
        if k is None:
            return ADA[:, base + t * 8: base + t * 8 + 8]
        return ADA[:, base + t * 8 + k: base + t * 8 + k + 1]

    m0 = sb.mark()
    cv = sb.alloc([128, 16], F32); sg = sb.alloc([128, 16], F32); scv = sb.alloc([128, 16], F32)
    bada = sb.alloc([128, 48], F32)
    wst = [sb.alloc([128, 8, 512], F32) for _ in range(3)]
    wbf0 = [sb.alloc([128, 8, 512], BF) for _ in range(2)]
    dma('sp', cv[:], cvec_d, [], ['cv']); dma('sp', bada[:], bada_d, [], ['bada'])
    act(sg[:], cv[:], AF.Sigmoid, ['cv'], ['sg'])
    scvb = sb.alloc([128, 16], BF)
    V('dve', 'tensor_tensor', ['cv', 'sg'], ['scv'], scvb[:], cv[:], sg[:], ALU.mult)
    pada, kada = ps_half()
    scv3 = scvb[:].rearrange("p (c k) -> p c k", k=8)
    for pc in range(12):
        w = wst[pc % 3]; wb = wbf0[pc % 2]; wk = 'wst%d' % (pc % 3); wbk = 'wbf%d' % (pc % 2)
        dma('sp', w[:], wada_d[:, pc * 512:(pc + 1) * 512].rearrange("(k p) e -> p k e", p=128), [], [wk])
        act(wb[:, 0:4, :], w[:, 0:4, :], AF.Copy, [wk], [wbk])
        V('dve', 'tensor_copy', [wk, wbk], [wbk], wb[:, 4:8, :], w[:, 4:8, :])
        for jj in range(4):
            j = pc * 4 + jj
            mm(pada[:, 2 * j:2 * j + 2], [(wb[:, k, jj * 128:(jj + 1) * 128], scv3[:, :, k]) for k in range(8)],
               [wbk, 'scv'], [kada])
    pv = pada[:, 0:96].rearrange("p (j c) -> p c j", c=2)
    V('dve', 'tensor_tensor', [kada, 'bada'], ['ADA'], ADA[:, 0:48], pv[:, 0, :], bada[:], ALU.add)
    V('dve', 'tensor_tensor', [kada, 'bada', 'ADA'], ['ADA'], ADA[:, 48:96], pv[:, 1, :], bada[:], ALU.add)
    V('dve', 'scalar_tensor_tensor', ['ADA', 'n1g'], ['scale1'], scale1[:], ada(1), 1.0, n1g[:], ALU.add, ALU.mult)
    V('dve', 'scalar_tensor_tensor', ['ADA', 'n1g'], ['cscale1'], cscale1[:], ada(1, ctx=True), 1.0, n1g[:], ALU.add, ALU.mult)
    V('dve', 'scalar_tensor_tensor', ['ADA', 'n2g'], ['scale2'], scale2[:], ada(4), 1.0, n2g[:], ALU.add, ALU.mult)
    P.barrier()
    sb.reset(m0)

    def rms_tile(xt, xkey, ss, sskey, junk):
        act(junk[:], xt[:], AF.Square, [xkey], ['junk', sskey], accum_out=ss[:])
        act(ss[:], ss[:], AF.Sqrt, [sskey, 'epsb'], [sskey], scale=1.0 / D, bias=epsb[:])
        V('dve', 'reciprocal', [sskey], [sskey], ss[:], ss[:])

    epsb = sb.alloc([128, 1], F32)
    V('dve', 'memset', [], ['epsb'], epsb[:], EPS)
    zcol = sb.alloc([128, 1], F32)
    V('dve', 'memset', [], ['zcol'], zcol[:], 0.0)
    m1 = sb.mark()
    hT = sb.alloc([128, 8, TT], BF)
    xts = [sb.alloc([128, D], F32) for _ in range(4)]
    pts = [sb.alloc([128, D], F32) for _ in range(3)]
    xn16 = [sb.alloc([128, D], BF) for _ in range(4)]
    junk = sb.alloc([128, D], BF)
    sss = [sb.alloc([128, 1], F32) for _ in range(4)]
    for pr_ in range(17):
        pi = pr_ % 4
        pbf = PS[pi][:, :].bitcast(BF)
        tokp = pr_ * 256
        isctx = (pr_ == 0)
        for t_ in range(2):
            i = pr_ * 2 + t_
            xt = xts[i % 4]; xk = 'xt%d' % (i % 4); ss = sss[i % 4]; sk = 'ss%d' % (i % 4)
            xn = xn16[i % 4]; xnk = 'xn16_%d' % (i % 4)
            if isctx:
                dma('sp', xt[:], ctx_d[i * 128:(i + 1) * 128, :], [], [xk])
            else:
                jx = i - 2
                pt = pts[i % 3]; pk = 'pt%d' % (i % 3)
                dma('sp', xt[:], x_d[jx * 128:(jx + 1) * 128, :], [], [xk])
                dma('sp', pt[:], pos_d[jx * 128:(jx + 1) * 128, :], [], [pk])
                V('pool', 'tensor_tensor', [xk, pk], [xk], xt[:], xt[:], pt[:], ALU.add)
            rms_tile(xt, xk, ss, sk, junk)
            V('dve', 'tensor_scalar', [xk, sk], [xnk], xn[:], xt[:], ss[:, 0:1], None, ALU.mult)
            for k in range(8):
                c0 = k * 256 + t_ * 128
                tr(pbf[:, c0:c0 + 128], xn[:, k * 128:(k + 1) * 128], identb[:], [xnk, 'identb'], ['ps%d' % (2 * pi + k // 4)])
        scl = cscale1 if isctx else scale1
        for k in range(8):
            c0 = k * 256
            pkey = 'ps%d' % (2 * pi + k // 4)
            if k < 4:
                act(hT[:, k, tokp:tokp + 256], pbf[:, c0:c0 + 256], AF.Identity,
                    [pkey, 'scale1', 'cscale1', 'ADA'], ['hT%d_%d' % (pr_, k)], scale=scl[:, k:k + 1], bias=ada(0, k, ctx=isctx))
            else:
                V('dve', 'tensor_scalar', [pkey, 'scale1', 'cscale1', 'ADA'], ['hT%d_%d' % (pr_, k)], hT[:, k, tokp:tokp + 256], pbf[:, c0:c0 + 256],
                  scl[:, k:k + 1], ada(0, k, ctx=isctx), ALU.mult, ALU.add)
    hT_keys = ['hT%d_%d' % (i, k) for i in range(17) for k in range(8)]

    wst = [sb.alloc([128, 8, 512], F32) for _ in range(2)]
    wbf = [sb.alloc([128, 8, 512], BF) for _ in range(2)]
    ost = [sb.alloc([128, TT], BF) for _ in range(2)]
    gt = [sb.alloc([128, 512], F32) for _ in range(6)]
    gz = [sb.alloc([128, 512], F32) for _ in range(6)]
    gctr = [0]
    gpend = []
    chunks = [(0, TC)] + [(TC + 512 * c, 512) for c in range(8)]
    ecount = 0
    def p1_dma(pc):
        dma('sp', wst[pc % 2][:], win_d[:, pc * 512:(pc + 1) * 512].rearrange("(k p) e -> p k e", p=128), [], ['wst%d' % (pc % 2)])

    def p1_cast(pc):
        w = wst[pc % 2]; wb = wbf[pc % 2]; wk = 'wst%d' % (pc % 2); wbk = 'wbf%d' % (pc % 2)
        act(wb[:, 0:4, :], w[:, 0:4, :], AF.Copy, [wk], [wbk])
        V('dve', 'tensor_copy', [wk, wbk], [wbk], wb[:, 4:8, :], w[:, 4:8, :])

    p1_dma(0)
    p1_cast(0)
    p1_dma(1)
    for pc in range(9):
        wb = wbf[pc % 2]; wbk = 'wbf%d' % (pc % 2)
        for jj in range(4):
            ec = pc * 4 + jj
            typ = 'F' if ec < 4 else 'XR' if ec < 12 else 'GG' if ec < 20 else 'SA' if ec < 28 else 'SB'
            o = ost[ecount % 2]; ok = 'ost%d' % (ecount % 2); ecount += 1
            ob = o[:]
            for ci, (t0, n) in enumerate(chunks):
                if ci == 0 and typ != 'XR':
                    continue
                ph, pk = ps_half()
                mm(ph[:, 0:n], [(wb[:, k, jj * 128:(jj + 1) * 128], hT[:, k, t0:t0 + n]) for k in range(8)],
                   [wbk] + hT_keys, [pk])
                if typ == 'XR':
                    act(ob[:, t0:t0 + n], ph[:, 0:n], AF.Copy, [pk], [ok])
                elif typ == 'F':
                    cF = (t0 - TC) // 512
                    zdst = ob[:, 0:T].rearrange("p (b a) -> p a b", a=64)[:, 8 * cF:8 * cF + 8, :]
                    act(zdst, ph[:, 0:n].rearrange("p (a b) -> p a b", b=64), AF.Copy, [pk], [ok])
                elif typ in ('SA', 'SB'):
                    act(ob[:, t0 - TC:t0 - TC + n], ph[:, 0:n], AF.Sigmoid, [pk], [ok])
                else:
                    gi = gctr[0] % 6; gctr[0] += 1
                    g0 = gt[gi]; gk = 'gt%d' % gi; zc = gz[gi]; zk = 'gz%d' % gi
                    act(zc[:], ph, AF.Copy, [pk], [zk])
                    act(g0[:], zc[:], AF.Square, [zk], [gk], scale=0.21145921593541212)
                    V('dve', 'scalar_tensor_tensor', [gk, zk], [gk], g0[:], g0[:], 1.0, zc[:], ALU.add, ALU.mult)
                    def fin(g0=g0, gk=gk, zc=zc, zk=zk, ob=ob, ok=ok, a=t0 - TC, n=n):
                        act(g0[:], g0[:], AF.Sigmoid, [gk], [gk], scale=1.5957691216057308)
                        V('dve', 'tensor_tensor', [gk, zk], [ok], ob[:, a:a + n], g0[:], zc[:], ALU.mult)
                    gpend.append(fin)
                    if len(gpend) > 2:
                        gpend.pop(0)()
            while gpend:
                gpend.pop(0)()
            if typ == 'F':
                dma('pool', zf_d[ec * 128:(ec + 1) * 128, :], ob[:, 0:T], [ok], ['zf_d'])
            elif typ == 'XR':
                dma('pool', xr_d[(ec - 4) * 128:(ec - 3) * 128, :], ob[:, 0:TT], [ok], ['xr_d'])
            else:
                dd = {'GG': gg_d, 'SA': sa_d, 'SB': sb_d}[typ]
                e0 = (ec - 12) % 8
                dma('pool', dd[e0 * 128:(e0 + 1) * 128, :], ob[:, 0:T], [ok], [typ + '_d'])
            if jj == 2 and pc + 1 < 9:
                p1_cast(pc + 1)
        if pc + 2 < 9:
            p1_dma(pc + 2)
    P.barrier()
    sb.reset(m1)
    if stop_after <= 1:
        return finish(nc, es, P)

    m2 = sb.mark()
    cw = sb.alloc([128, 32], F32); cb = sb.alloc([128, 8], F32); lam = sb.alloc([128, 16], F32)
    ba = sb.alloc([128, 16], F32); bi = sb.alloc([128, 16], F32); nsp = sb.alloc([128, 16], F32)
    dma('sp', cw[:], cw_d, [], ['cw']); dma('sp', cb[:], cb_d, [], ['cb']); dma('sp', lam[:], lam_d, [], ['lam'])
    dma('sp', ba[:], ba_d, [], ['ba']); dma('sp', bi[:], bi_d, [], ['bi'])
    act(nsp[:], lam[:], AF.Exp, ['lam'], ['nsp'], scale=-1.0)
    act(nsp[:], nsp[:], AF.Ln, ['nsp'], ['nsp'], bias=1.0)
    V('dve', 'tensor_scalar', ['nsp'], ['nsp'], nsp[:], nsp[:], -8.0, None, ALU.mult)
    WA = sb.alloc([128, 16, 128], BF); WI = sb.alloc([128, 16, 128], BF)
    DG = sb.alloc([128, 32, 128], BF)
    ggt = sb.alloc([128, T], BF); yint = sb.alloc([128, T], BF)
    wgs = yint[:].bitcast(F32).rearrange("p (m j) -> p m j", j=128)
    dma('sp', wgs, wa_d.rearrange("m i j -> i m j"), [], ['yint'])
    act(WA[:], wgs, AF.Copy, ['yint'], ['WA'])
    dma('sp', wgs, wi_d.rearrange("m i j -> i m j"), ['yint'], ['yint'])
    act(WI[:], wgs, AF.Copy, ['yint'], ['WI'])
    for h in range(8):
        for k in range(4):
            V('dve', 'tensor_scalar', ['identf', 'cw'], ['DG'], DG[:, h * 4 + k, :], identf[:], cw[:, h * 4 + k:h * 4 + k + 1], None, ALU.mult)
    XPW = 4448
    xpads = [sb.alloc([128, XPW], BF)] * 2
    Ubs = [sb.alloc([128, TT], BF) for _ in range(2)]
    Rb = sb.alloc([128, TT], BF)
    Ibs = [sb.alloc([128, TT], BF) for _ in range(3)]
    Abs = [sb.alloc([128, TT], F32) for _ in range(3)]
    T1s = [sb.alloc([128, TT], F32) for _ in range(2)]; Y = sb.alloc([128, T], F32); Hc = sb.alloc([128, TC], F32)
    V('pool', 'memset', [], ['xpad0'], xpads[0][:], 0.0)
    NCH = len(chunks)

    def ck(nm, ci):
        return '%s%d' % (nm, ci)

    def conv_load(h):
        xp = xpads[0]; xk = 'xpad0'
        dma('sp', xp[:, 32:32 + TC], xr_d[h * 128:(h + 1) * 128, 0:TC], [], [xk])
        dma('sp', xp[:, 320:320 + T], xr_d[h * 128:(h + 1) * 128, TC:TT], [], [xk])

    conv_ps = {}

    def conv_mm(h, ci):
        xp = xpads[0]; xk = 'xpad0'
        t0, n = chunks[ci]
        base = 32 if ci == 0 else 320
        s_ = t0 if ci == 0 else t0 - TC
        ph, pk = ps_half()
        mm(ph[:, 0:n], [(DG[:, h * 4 + k, :], xp[:, base + s_ + k - 2: base + s_ + k - 2 + n]) for k in range(4)],
           ['DG', xk], [pk])
        conv_ps[(h, ci)] = (ph, pk)

    def conv_ev(h, ci):
        t0, n = chunks[ci]
        ph, pk = conv_ps.pop((h, ci))
        V('dve', 'tensor_scalar', [pk, 'cb'], ['Ub%d_%d' % (h % 2, ci)], Ubs[h % 2][:, t0:t0 + n], ph[:, 0:n], cb[:, h:h + 1], None, ALU.add)

    conv_load(0)
    for ci in range(NCH):
        conv_mm(0, ci)
        conv_ev(0, ci)
    ggts = [ggt, ggt]
    yints = [yint]

    def unit_ctx(u):
        h = u // 2; d = u % 2
        return h, d, d * 8 + h, Ubs[h % 2], Ibs[u % 3], Abs[u % 3], T1s[d]

    def ubk_(h, ci):
        return 'Ub%d_%d' % (h % 2, ci)

    def sG(u):
        h, d, col, ub, Ib, Ab, T1 = unit_ctx(u)
        for ci, (t0, n) in enumerate(chunks):
            ph, pk = ps_half()
            mm(ph[:, 0:n], [(WA[:, col, :], ub[:, t0:t0 + n])], ['WA', ubk_(h, ci)], [pk])
            act(Rb[:, t0:t0 + n], ph[:, 0:n], AF.Sigmoid, [pk, 'ba'], [ck('R', ci)], bias=ba[:, col:col + 1], scale=1.0)
            ph, pk = ps_half()
            mm(ph[:, 0:n], [(WI[:, col, :], ub[:, t0:t0 + n])], ['WI', ubk_(h, ci)], [pk])
            act(Ib[:, t0:t0 + n], ph[:, 0:n], AF.Sigmoid, [pk, 'bi'], ['I%d_%d' % (u % 3, ci)], bias=bi[:, col:col + 1], scale=1.0)

    cgroups = [[0, 1, 2], [3, 4, 5], [6, 7, 8]]

    def grange(g_):
        a0 = chunks[g_[0]][0]
        a1 = chunks[g_[-1]][0] + chunks[g_[-1]][1]
        return a0, a1

    def sE(u):
        h, d, col, ub, Ib, Ab, T1 = unit_ctx(u)
        for g_ in cgroups:
            a0, a1 = grange(g_)
            act(Ab[:, a0:a1], Rb[:, a0:a1], AF.Exp, [ck('R', ci) for ci in g_] + ['nsp'], ['A%d_%d' % (u % 3, ci) for ci in g_], scale=nsp[:, col:col + 1])

    def sQ(u):
        h, d, col, ub, Ib, Ab, T1 = unit_ctx(u)
        for ci, (t0, n) in enumerate(chunks):
            V('pool', 'tensor_tensor', ['I%d_%d' % (u % 3, ci), ubk_(h, ci)], ['I%d_%d' % (u % 3, ci)], Ib[:, t0:t0 + n], Ib[:, t0:t0 + n], ub[:, t0:t0 + n], ALU.mult)
        for g_ in cgroups:
            a0, a1 = grange(g_)
            act(T1[:, a0:a1], Ab[:, a0:a1], AF.Square, ['A%d_%d' % (u % 3, ci) for ci in g_], ['T%d_%d' % (d, ci) for ci in g_])

    def sR(u):
        h, d, col, ub, Ib, Ab, T1 = unit_ctx(u)
        for g_ in cgroups:
            a0, a1 = grange(g_)
            act(T1[:, a0:a1], T1[:, a0:a1], AF.Sqrt, ['T%d_%d' % (d, ci) for ci in g_], ['T%d_%d' % (d, ci) for ci in g_], scale=-1.0, bias=1.0)

    def sBm(u):
        h, d, col, ub, Ib, Ab, T1 = unit_ctx(u)
        for ci, (t0, n) in enumerate(chunks):
            V('dve', 'tensor_tensor', ['I%d_%d' % (u % 3, ci), 'T%d_%d' % (d, ci)], ['I%d_%d' % (u % 3, ci)], Ib[:, t0:t0 + n], Ib[:, t0:t0 + n], T1[:, t0:t0 + n], ALU.mult)

    def sSC(u):
        h, d, col, ub, Ib, Ab, T1 = unit_ctx(u)
        allA = ['A%d_%d' % (u % 3, ci) for ci in range(NCH)]; allI = ['I%d_%d' % (u % 3, ci) for ci in range(NCH)]; allT = ['T%d_%d' % (d, ci) for ci in range(NCH)]
        if d == 0:
            V('dve', 'tensor_tensor_scan', [allA[0], allI[0]], ['Hc'], Hc[:], Ab[:, 0:TC], Ib[:, 0:TC], 0.0, ALU.mult, ALU.add)
            V('dve', 'tensor_tensor_scan', allA[1:] + allI[1:] + ['Hc'], ['Y'], Y[:], Ab[:, TC:TT], Ib[:, TC:TT], Hc[:, TC - 1:TC], ALU.mult, ALU.add)
        else:
            V('dve', 'tensor_tensor_scan', [allA[0], allI[0]], ['Hc'], Hc[:, ::-1], Ab[:, 0:TC][:, ::-1], Ib[:, 0:TC][:, ::-1], 0.0, ALU.mult, ALU.add)
            V('dve', 'tensor_tensor_scan', allA[1:] + allI[1:] + allT[1:] + ['Hc'], allT[1:], T1[:, TC:TT][:, ::-1], Ab[:, TC:TT][:, ::-1], Ib[:, TC:TT][:, ::-1],
              Hc[:, 0:1], ALU.mult, ALU.add)
            V('pool', 'tensor_tensor', allT[1:] + ['Y'], ['Y'], Y[:], Y[:], T1[:, TC:TT], ALU.add)
            gg = ggts[0]; ggk = 'ggt0'
            V('pool', 'tensor_tensor', ['Y', ggk], ['yint'], yints[0][:], Y[:], gg[:], ALU.mult)
            dma('pool', yin_d[h * 128:(h + 1) * 128, :], yints[0][:], ['yint'], ['yin_d'])
            if h + 1 < 8:
                head_prefetch(h + 1)

    def head_prefetch(h):
        dma('pool', ggts[0][:], gg_d[h * 128:(h + 1) * 128, :], [], ['ggt0'])

    head_prefetch(0)
    sG(0); sE(0); sQ(0)
    for u in range(16):
        h = u // 2
        if u % 2 == 0 and h + 1 < 8:
            conv_load(h + 1)
        if u + 1 < 16:
            sG(u + 1); sE(u + 1); sQ(u + 1)
        if u % 2 == 0 and h + 1 < 8:
            for ci in range(NCH):
                conv_mm(h + 1, ci)
                conv_ev(h + 1, ci)
        sR(u); sBm(u); sSC(u)
    P.barrier()
    sb.reset(m2)
    if stop_after <= 2:
        return finish(nc, es, P)

    m3 = sb.mark()
    CS = sb.alloc([128, 256], BF); E1 = sb.alloc([128, 32, 512], BF); E2 = sb.alloc([128, 256], BF)
    dma('sp', CS[:], cs128_d, [], ['CS']); dma('sp', E1[:], e1_d.rearrange("p (m c) -> p m c", c=512), [], ['E1'])
    dma('sp', E2[:], e2_d, [], ['E2'])
    ZF = [sb.alloc([128, T], BF) for _ in range(2)]
    Bfm = sb.alloc([128, 2, T], BF)
    Yfm = [sb.alloc([128, T], BF) for _ in range(2)]
    NR = 8
    Zt = [sb.alloc([128, 256], BF) for _ in range(NR)]
    Bt = [sb.alloc([128, 2, 128], BF) for _ in range(NR)]

    def pipeline(n, stages, lags):
        for step in range(n + lags[-1]):
            for st_, lag in zip(stages, lags):
                i_ = step - lag
                if 0 <= i_ < n:
                    st_(i_)

    for g in range(4):
        zf = ZF[g % 2]; zk = 'ZF%d' % (g % 2); yf = Yfm[g % 2]; yk = 'Yfm%d' % (g % 2)
        dma('sp', zf[:], zf_d[g * 128:(g + 1) * 128, :], [], [zk])
        hold = {}

        def sC(m):
            ph, pk = ps_half()
            mm(ph[:, 0:256], [(zf[:, 128 * m:128 * m + 128], CS[:])], [zk, 'CS'], [pk])
            hold[('c', m)] = (ph, pk)

        def sZ(m):
            z1 = Zt[m % NR]; z1k = 'Zt%d' % (m % NR)
            ph, pk = hold.pop(('c', m))
            if m % 2 == 0:
                act(z1[:], ph[:, 0:256], AF.Copy, [pk], [z1k])
            else:
                V('dve', 'tensor_copy', [pk], [z1k], z1[:], ph[:, 0:256])

        def sS(m):
            z1 = Zt[m % NR]; z1k = 'Zt%d' % (m % NR)
            ph, pk = ps_half()
            mm(ph[:, 0:256], [(z1[:, 0:128], E1[:, m, 0:256]), (z1[:, 128:256], E1[:, m, 256:512])], [z1k, 'E1'], [pk])
            hold[('s', m)] = (ph, pk)

        def sB(m):
            ph, pk = hold.pop(('s', m))
            for c in range(2):
                bdst = Bfm[:, c, :].rearrange("p (k t) -> p t k", t=64)[:, 2 * m:2 * m + 2, :]
                bsrc = ph[:, c * 128:(c + 1) * 128].rearrange("p (l k) -> p l k", l=2)
                if c == 0:
                    act(bdst, bsrc, AF.Copy, [pk], ['Bfm%d' % m])
                else:
                    V('dve', 'tensor_copy', [pk, 'Bfm%d' % m], ['Bfm%d' % m], bdst, bsrc)

        pipeline(32, [sC, sZ, sS, sB], LAGS)
        allB = ['Bfm%d' % m for m in range(32)]

        def sT(n):
            ph, pk = ps_half()
            phb = ph.bitcast(BF)
            for c in range(2):
                tr(phb[:, c * 128:(c + 1) * 128], Bfm[:, c, 128 * n:128 * n + 128], identb[:], allB + ['identb'], [pk])
            hold[('t', n)] = (phb, pk)

        def sE(n):
            b1 = Bt[n % NR]; b1k = 'Bt%d' % (n % NR)
            phb, pk = hold.pop(('t', n))
            if n % 2 == 0:
                act(b1[:], phb[:, 0:256].rearrange("p (c n) -> p c n", c=2), AF.Copy, [pk], [b1k])
            else:
                V('dve', 'tensor_copy', [pk], [b1k], b1[:], phb[:, 0:256].rearrange("p (c n) -> p c n", c=2))

        def sM(n):
            b1 = Bt[n % NR]; b1k = 'Bt%d' % (n % NR)
            ph, pk = ps_half()
            mm(ph[:, 0:128], [(b1[:, 0, :], E2[:, 0:128]), (b1[:, 1, :], E2[:, 128:256])], [b1k, 'E2'], [pk])
            hold[('m', n)] = (ph, pk)

        def sY(n):
            ph, pk = hold.pop(('m', n))
            dst = yf[:].rearrange("p (a b) -> p b a", b=64)[:, 2 * n:2 * n + 2, :]
            if n % 2 == 1:
                act(dst, ph[:, 0:128].rearrange("p (c n) -> p c n", c=2), AF.Copy, [pk], [yk])
            else:
                V('dve', 'tensor_copy', [pk], [yk], dst, ph[:, 0:128].rearrange("p (c n) -> p c n", c=2))

        pipeline(32, [sT, sE, sM, sY], LAGS)
        dma('pool', yf_d[g * 128:(g + 1) * 128, :], yf[:], [yk], ['yf_d'])
    P.barrier()
    sb.reset(m3)
    if stop_after <= 3:
        return finish(nc, es, P)

    Sc = sb.alloc([128, 32, 16], F32); idxs = sb.alloc([128, 64], I32); idx2s = sb.alloc([128, 64], I32)
    m4 = sb.mark()
    S = sb.alloc([128, T], F32)
    wst = [sb.alloc([128, 8, 512], F32) for _ in range(1)]
    WF = sb.alloc([128, 4, D], BF); WL = sb.alloc([128, 8, D], BF); WO = sb.alloc([128, 8, D], BF)
    WRf = sb.alloc([128, 8, 16], F32); WR = sb.alloc([128, 8, 16], BF)
    dma('sp', WRf[:], wr_d.rearrange("p (k e) -> p k e", e=16), [], ['WRf'])
    V('dve', 'tensor_copy', ['WRf'], ['WR'], WR[:], WRf[:])
    cnt = 0
    for (src, dstw, nk) in [(wfour_d, WF, 4), (wlru_d, WL, 8), (wout_d, WO, 8)]:
        for half in range(2):
            w = wst[0]; wk = 'wst0'; cnt += 1
            dma('sp', w[:, 0:nk, :], src[:, half * 512:(half + 1) * 512].rearrange("(k p) e -> p k e", p=128), [], [wk])
            act(dstw[:, :, half * 512:(half + 1) * 512], w[:, 0:nk, :], AF.Copy, [wk], ['W4'])
    yfc = [sb.alloc([128, 4, 512], BF) for _ in range(2)]
    yic = [sb.alloc([128, 8, 512], BF) for _ in range(2)]
    sac = [sb.alloc([128, 8, 512], BF) for _ in range(2)]
    sbc = [sb.alloc([128, 8, 512], BF) for _ in range(2)]
    mg = sb.alloc([128, 8, 512], BF)
    tm = [sb.alloc([128, 512], BF) for _ in range(4)]
    yg = sb.alloc([128, 8, 512], BF)
    xts = [sb.alloc([128, D], F32) for _ in range(3)]
    pts = [sb.alloc([128, D], F32) for _ in range(2)]
    xnb = [sb.alloc([128, D], BF) for _ in range(2)]
    x1b = [sb.alloc([128, D], BF) for _ in range(3)]
    h2T = [sb.alloc([128, 8, 128], BF) for _ in range(2)]
    junk = sb.alloc([128, D], BF)
    sss = [sb.alloc([128, 1], F32) for _ in range(3)]
    LGP = PS[3][:, 512:1024]
    lgk = 'ps7'
    pst4 = {'h': 0}

    def ps_half6():
        i = pst4['h'] % 6
        pst4['h'] += 1
        return PS[i // 2][:, (i % 2) * 512:(i % 2) * 512 + 512], 'ps%d' % i

    def ps_full3():
        i = pst4['h'] % 6
        if i % 2:
            pst4['h'] += 1
            i = pst4['h'] % 6
        pst4['h'] += 2
        return PS[i // 2][:, :], ['ps%d' % i, 'ps%d' % (i + 1)]

    ygs = [yg, sb.alloc([128, 8, 512], BF)]

    def A_steps(tc):
        b = tc % 2
        t0 = tc * 512
        ygc = ygs[tc % 2]
        steps = []

        def ld():
            dma('sp', yfc[b][:], yf_d[:, t0:t0 + 512].rearrange("(g p) t -> p g t", p=128), [], ['yfc%d' % b])
            dma('sp', yic[b][:], yin_d[:, t0:t0 + 512].rearrange("(g p) t -> p g t", p=128), [], ['yic%d' % b])
            dma('sp', sac[b][:], sa_d[:, t0:t0 + 512].rearrange("(g p) t -> p g t", p=128), [], ['sac%d' % b])
            dma('sp', sbc[b][:], sb_d[:, t0:t0 + 512].rearrange("(g p) t -> p g t", p=128), [], ['sbc%d' % b])
        steps.append(ld)
        for ec in range(8):
            def f(ec=ec):
                pf, pfk = ps_half6()
                mm(pf, [(WF[:, g, ec * 128:(ec + 1) * 128], yfc[b][:, g, :]) for g in range(4)], ['W4', 'yfc%d' % b], [pfk])
                pr, prk = ps_half6()
                mm(pr, [(WL[:, k, ec * 128:(ec + 1) * 128], yic[b][:, k, :]) for k in range(8)], ['W4', 'yic%d' % b], [prk])
                t1 = tm[(ec % 2) * 2]; t2 = tm[(ec % 2) * 2 + 1]; k1_ = 'tm%d' % ((ec % 2) * 2); k2_ = 'tm%d' % ((ec % 2) * 2 + 1)
                V('dve', 'tensor_tensor', [pfk, 'sac%d' % b], [k1_], t1[:], pf, sac[b][:, ec, :], ALU.mult)
                V('dve', 'tensor_tensor', [prk, 'sbc%d' % b], [k2_], t2[:], pr, sbc[b][:, ec, :], ALU.mult)
                V('dve', 'tensor_tensor', [k1_, k2_], ['mg%d' % ec], mg[:, ec, :], t1[:], t2[:], ALU.add)
            steps.append(f)
        for ec in range(8):
            def g_(ec=ec):
                po, pok = ps_half6()
                mm(po, [(WO[:, k, ec * 128:(ec + 1) * 128], mg[:, k, :]) for k in range(8)], ['W4'] + ['mg%d' % k for k in range(8)], [pok])
                act(ygc[:, ec, :], po, AF.Identity, [pok, 'ADA', 'zcol'], ['yg%d_%d' % (tc % 2, ec)], scale=ada(2, ec), bias=zcol[:])
            steps.append(g_)
        return steps

    def T_steps(tc):
        ygc = ygs[tc % 2]
        hold = {}

        def L(j):
            ti = tc * 4 + j
            if ti >= 32:
                return
            bb = ti % 2; b3 = ti % 3
            xt = xts[b3]; xk = 'xt%d' % b3; pt = pts[bb]; pk_ = 'pt%d' % bb
            dma('sp', xt[:], x_d[ti * 128:(ti + 1) * 128, :], [], [xk])
            dma('sp', pt[:], pos_d[ti * 128:(ti + 1) * 128, :], [], [pk_])
            V('pool', 'tensor_tensor', [xk, pk_], [xk], xt[:], xt[:], pt[:], ALU.add)

        def T1(j):
            ti = tc * 4 + j
            bb = ti % 2; b3 = ti % 3
            xt = xts[b3]; xk = 'xt%d' % b3
            if ti == 0:
                L(0)
            L(j + 1)
            ph_, phk = ps_half6()
            pfb = ph_.bitcast(BF)
            for k in range(8):
                tr(pfb[:, k * 128:(k + 1) * 128], ygc[:, k, j * 128:(j + 1) * 128], identb[:], ['yg%d_%d' % (tc % 2, k), 'identb'], [phk])
            V('dve', 'tensor_tensor', [xk, phk], [xk], xt[:], xt[:], pfb, ALU.add)
            act(x1b[b3][:], xt[:], AF.Copy, [xk], ['x1b%d' % b3])
            dma('pool', x1_d[ti * 128:(ti + 1) * 128, :], x1b[b3][:], ['x1b%d' % b3], ['x1_d'])
            ss = sss[b3]; sk = 'ss%d' % b3
            rms_tile(xt, xk, ss, sk, junk)
            V('dve', 'tensor_scalar', [xk, sk], ['xnb%d' % bb], xnb[bb][:], xt[:], ss[:, 0:1], None, ALU.mult)
            dma('pool', xn2_d[ti * 128:(ti + 1) * 128, :], xnb[bb][:], ['xnb%d' % bb], ['xn2_d'])

        def T2(j):
            ti = tc * 4 + j
            bb = ti % 2
            ph_a, phk_a = ps_half6()
            ph_b, phk_b = ps_half6()
            pfa = ph_a.bitcast(BF); pfb2 = ph_b.bitcast(BF)
            for k in range(8):
                dstp = pfa if k < 4 else pfb2
                tr(dstp[:, (k % 4) * 128:(k % 4 + 1) * 128], xnb[bb][:, k * 128:(k + 1) * 128], identb[:], ['xnb%d' % bb, 'identb'], [phk_a if k < 4 else phk_b])
            for k in range(8):
                if k < 4:
                    act(h2T[bb][:, k, :], pfa[:, (k % 4) * 128:(k % 4 + 1) * 128], AF.Identity, [phk_a, 'scale2', 'ADA'], ['h2Ta%d' % bb],
                        scale=scale2[:, k:k + 1], bias=ada(3, k))
                else:
                    V('dve', 'tensor_scalar', [phk_b, 'scale2', 'ADA'], ['h2Tb%d' % bb], h2T[bb][:, k, :], pfb2[:, (k % 4) * 128:(k % 4 + 1) * 128],
                      scale2[:, k:k + 1], ada(3, k), ALU.mult, ALU.add)

        def R(j):
            ti = tc * 4 + j
            bb = ti % 2
            mm(LGP[:, ti * 16:(ti + 1) * 16], [(h2T[bb][:, k, :], WR[:, k, :]) for k in range(8)], ['h2Ta%d' % bb, 'h2Tb%d' % bb, 'WR'], [lgk])

        order = [(T1, 0), (T1, 1), (T2, 0), (T1, 2), (R, 0), (T2, 1), (T1, 3), (R, 1), (T2, 2), (R, 2), (T2, 3), (R, 3)]
        return [(lambda f=f, j=j: f(j)) for (f, j) in order]

    for st in A_steps(0):
        st()
    for tc in range(8):
        a_ = A_steps(tc + 1) if tc + 1 < 8 else []
        t_ = T_steps(tc)
        ia = it_ = 0
        while ia < len(a_) or it_ < len(t_):
            for _ in range(3):
                if ia < len(a_):
                    a_[ia](); ia += 1
            for _ in range(2):
                if it_ < len(t_):
                    t_[it_](); it_ += 1
    mx = sb.alloc([128, 32], F32)
    lg3 = LGP.rearrange("p (j e) -> p j e", e=16)
    V('dve', 'tensor_reduce', [lgk], ['mx'], mx[:], lg3, AX.X, ALU.max)
    V('dve', 'tensor_tensor', [lgk, 'mx'], ['Sc'], Sc[:], lg3, mx[:].unsqueeze(2).to_broadcast([128, 32, 16]), ALU.subtract)
    act(Sc[:], Sc[:], AF.Exp, ['Sc'], ['Sc'])
    V('dve', 'tensor_reduce', ['Sc'], ['mx'], mx[:], Sc[:], AX.X, ALU.add)
    V('dve', 'reciprocal', ['mx'], ['mx'], mx[:], mx[:])
    V('dve', 'tensor_tensor', ['Sc', 'mx'], ['Sc'], Sc[:], Sc[:], mx[:].unsqueeze(2).to_broadcast([128, 32, 16]), ALU.mult)
    dma('pool', sc_d.rearrange("(p j) e -> p (j e)", p=128), Sc[:].rearrange("p j e -> p (j e)"), ['Sc'], ['sc_d'])
    for q in range(4):
        pf, pfk = ps_full3()
        for jj in range(8):
            j = q * 8 + jj
            tr(pf[0:16, jj * 128:(jj + 1) * 128], Sc[:, j, :], identf[:], ['Sc', 'identf'], [pfk[jj // 4]])
        V('dve', 'tensor_copy', pfk, ['S'], S[0:16, q * 1024:(q + 1) * 1024], pf[0:16, :])
    P.barrier()
    sb.reset(m4)
    if stop_after <= 4:
        return finish(nc, es, P)

    S = sb.alloc([128, T], F32)
    zt = sb.alloc([128, 2048], F32)
    V('pool', 'memset', [], ['zt'], zt[:], 0.0)
    for i in range(16):
        dma('pool', y_d[i * 256:(i + 1) * 256, :].rearrange("(p r) c -> p (r c)", p=128), zt[:], ['zt'], ['y_d'])
    lo = sb.alloc([128, 1], F32); mid = sb.alloc([128, 1], F32); cntt = sb.alloc([128, 1], F32); ge = sb.alloc([128, 1], F32)
    jk = sb.alloc([128, T], BF)
    V('dve', 'memset', [], ['lo'], lo[0:16, :], 0.0)
    for it in range(30):
        half = 2.0 ** (-(it + 1))
        V('dve', 'tensor_scalar', ['lo'], ['mid'], mid[0:16, :], lo[0:16, :], half, None, ALU.add)
        V('dve', 'tensor_scalar', ['S', 'mid'], ['jk', 'cnt'], jk[0:16, :], S[0:16, :], mid[0:16, 0:1], None, ALU.is_ge, ALU.add, cntt[0:16, :])
        V('dve', 'tensor_scalar', ['cnt'], ['ge'], ge[0:16, :], cntt[0:16, :], float(CAP), half, ALU.is_ge, ALU.mult)
        V('dve', 'tensor_tensor', ['ge', 'lo'], ['lo'], lo[0:16, :], lo[0:16, :], ge[0:16, :], ALU.add)
    Mk = sb.alloc([128, T], F32); Cs = sb.alloc([128, T], F32)
    V('dve', 'tensor_scalar', ['S', 'lo'], ['Mk'], Mk[0:16, :], S[0:16, :], lo[0:16, 0:1], None, ALU.is_ge)
    ones = sb.alloc([128, T], BF)
    V('pool', 'memset', [], ['ones'], ones[0:16, :], 1.0)
    V('dve', 'tensor_tensor_scan', ['Mk', 'ones'], ['Cs'], Cs[0:16, :], ones[0:16, :], Mk[0:16, :], 0.0, ALU.mult, ALU.add)
    dma('sp', cs_d, Cs[0:16, :], ['Cs'], ['cs_d'])
    dma('sp', m_d, Mk[0:16, :], ['Mk'], ['m_d'])
    CSJ = sb.alloc([128, 8, 132], F32); M4 = sb.alloc([128, 8, 128], F32)
    V('pool', 'memset', [], ['CSJ'], CSJ[:], 0.0)
    iotac = sb.alloc([128, 512], F32); cval = sb.alloc([128, 4], F32)
    dma('sp', CSJ[0:64, :, 0:128], cs_d.rearrange("e (j p) -> (e j) p", p=128).rearrange("(q r) p -> r q p", r=64), ['cs_d', 'CSJ'], ['CSJ'])
    dma('sp', M4[0:64, :, :], m_d.rearrange("e (j p) -> (e j) p", p=128).rearrange("(q r) p -> r q p", r=64), ['m_d'], ['M4'])
    for q in range(8):
        dma('sp', CSJ[0:64, q, 128:129], jcol_d[0:64, :], ['CSJ'], ['CSJ'])
    dma('sp', iotac[:], iotac_d, [], ['iotac']); dma('sp', cval[:], cval_d, [], ['cval'])
    hi4 = sb.alloc([128, 8], F32); lo4 = sb.alloc([128, 8], F32)
    J4 = sb.alloc([128, 8, 512], F32); tj = sb.alloc([128, 512], F32)
    V('dve', 'tensor_copy', ['CSJ'], ['hi4'], hi4[0:64, :], CSJ[0:64, :, 127])
    V('dve', 'tensor_tensor', ['CSJ', 'M4'], ['lo4'], lo4[0:64, :], CSJ[0:64, :, 0], M4[0:64, :, 0], ALU.subtract)
    for q in range(8):
        V('dve', 'tensor_scalar', ['iotac', 'lo4'], ['tj'], tj[0:64, :], iotac[0:64, :], lo4[0:64, q:q + 1], None, ALU.is_ge)
        V('dve', 'scalar_tensor_tensor', ['iotac', 'hi4', 'tj'], ['J4'], J4[0:64, q, :], iotac[0:64, :], hi4[0:64, q:q + 1], tj[0:64, :], ALU.is_lt, ALU.mult)
    idxf = sb.alloc([128, 64], F32); rr = sb.alloc([128, 64], F32); idx2f = sb.alloc([128, 64], F32)
    jk3 = sb.alloc([128, 128], F32)
    for e in range(NE):
        q = e // 2; r0 = (e % 2) * 32
        for g in range(4):
            col = e * 4 + g
            ph, pk = ps_half6()
            mm(ph[:, 0:130], [(J4[r0:r0 + 32, q, g * 128:(g + 1) * 128], CSJ[r0:r0 + 32, q, 0:130])], ['J4', 'CSJ'], [pk])
            V('dve', 'tensor_scalar', [pk, 'cval'], ['jk3', 'rr'], jk3[:], ph[:, 0:128], cval[:, g:g + 1], None, ALU.is_le, ALU.add, rr[:, col:col + 1])
            V('dve', 'scalar_tensor_tensor', [pk, 'rr'], ['idxf'], idxf[:, col:col + 1], ph[:, 128:129], 128.0, rr[:, col:col + 1], ALU.mult, ALU.add)
            V('dve', 'scalar_tensor_tensor', [pk, 'rr'], ['idx2f'], idx2f[:, col:col + 1], rr[:, col:col + 1], 32.0, ph[:, 128:129], ALU.mult, ALU.add)
    V('dve', 'tensor_copy', ['idxf'], ['idxs'], idxs[:], idxf[:])
    V('dve', 'tensor_copy', ['idx2f'], ['idx2s'], idx2s[:], idx2f[:])
    if debug:
        dma('sp', idx_dbg, idxf[:], ['idxf'], ['idx_dbg'])
    P.barrier()
    sb.reset(m4)
    if stop_after <= 5:
        return finish(nc, es, P)

    m6 = sb.mark()
    wst = [sb.alloc([128, 8, 512], F32) for _ in range(4)]
    wbf = [sb.alloc([128, 8, 512], BF) for _ in range(7)]
    xrow = [sb.alloc([128, D], BF) for _ in range(8)]
    grow = [sb.alloc([128, 16], F32) for _ in range(16)]
    xgT = [sb.alloc([128, 8, 512], BF) for _ in range(2)]
    hid = sb.alloc([128, 12, 512], BF)
    sgt = [sb.alloc([128, 512], BF) for _ in range(2)]
    og = sb.alloc([128, 8, 512], F32)
    orow = [sb.alloc([128, D], F32) for _ in range(4)]
    pieces = []
    for e in range(NE):
        for fq in range(3):
            pieces.append((wg_d[e, :, fq * 512:(fq + 1) * 512].rearrange("(k p) f -> p k f", p=128), 8))
            pieces.append((wu_d[e, :, fq * 512:(fq + 1) * 512].rearrange("(k p) f -> p k f", p=128), 8))
        for dq in range(3):
            pieces.append((wd_d[e, dq * 512:(dq + 1) * 512, :].rearrange("(k p) c -> p k c", p=128), 4))
    loaded = {}
    wctr = {'dma': 0, 'cast': 0}
    LOOK_DMA = 6
    LOOK_CAST = 3

    def ensure_dma(upto):
        while wctr['dma'] <= min(upto, len(pieces) - 1):
            n = wctr['dma']; wctr['dma'] += 1
            src_ap, a = pieces[n]
            i = n % 4
            dma('sp', wst[i][:].rearrange("p a b -> p (a b)").rearrange("p (a b) -> p a b", a=a), src_ap, [], ['wst%d' % i])

    def ensure_cast(upto):
        while wctr['cast'] <= min(upto, len(pieces) - 1):
            n = wctr['cast']; wctr['cast'] += 1
            ensure_dma(n)
            src_ap, a = pieces[n]
            i = n % 4; jj = n % 7
            w = wst[i]; wb = wbf[jj]
            wv = w[:].rearrange("p a b -> p (a b)"); wbv = wb[:].rearrange("p a b -> p (a b)")
            if n % 2 == 0:
                act(wbv, wv, AF.Copy, ['wst%d' % i], ['wbf%d' % jj])
            else:
                V('dve', 'tensor_copy', ['wst%d' % i], ['wbf%d' % jj], wbv, wv)
            loaded[n] = (wb[:].rearrange("p a b -> p (a b)").rearrange("p (a b) -> p a b", a=a), 'wbf%d' % jj)

    def ensure_loaded(upto):
        ensure_cast(upto)

    def get_piece(n):
        ensure_cast(n + LOOK_CAST)
        ensure_dma(n + LOOK_DMA)
        return loaded[n]

    def gathers(e):
        for g in range(4):
            s = (e % 2) * 4 + g
            s3 = (e % 4) * 4 + g
            col = e * 4 + g
            P.op('pool', lambda en, s=s, col=col: en.indirect_dma_start(
                out=xrow[s][:, :], out_offset=None, in_=xn2_d[:, :],
                in_offset=bass.IndirectOffsetOnAxis(ap=idxs[:, col:col + 1], axis=0)), ['idxs', 'xn2_d'], ['xrow%d' % s], dma=True)
            P.op('pool', lambda en, s3=s3, col=col: en.indirect_dma_start(
                out=grow[s3][:, :], out_offset=None, in_=sc_d[:, :],
                in_offset=bass.IndirectOffsetOnAxis(ap=idx2s[:, col:col + 1], axis=0)), ['idx2s', 'sc_d'], ['grow%d' % s3], dma=True)

    def build_xgT(e):
        xg = xgT[e % 2]; xgk = 'xgT%d' % (e % 2)
        for g in range(4):
            s = (e % 2) * 4 + g
            ph, pk = ps_half()
            phb = ph.bitcast(BF)
            for k in range(8):
                tr(phb[:, k * 128:(k + 1) * 128], xrow[s][:, k * 128:(k + 1) * 128], identb[:], ['xrow%d' % s, 'identb'], [pk])
            for k in range(8):
                act(xg[:, k, g * 128:(g + 1) * 128], phb[:, k * 128:(k + 1) * 128], AF.Identity, [pk, 'scale2', 'ADA'], [xgk],
                    scale=scale2[:, k:k + 1], bias=ada(3, k))

    def outT(e):
        for g in range(4):
            s3 = (e % 4) * 4 + g
            col = e * 4 + g
            pf, pfk = ps_full()
            for k in range(8):
                tr(pf[:, k * 128:(k + 1) * 128], og[:, k, g * 128:(g + 1) * 128], identf[:], ['og%d' % k, 'identf'], [pfk[k // 4]])
            orw = orow[g]; ork = 'orow%d' % g
            V('dve', 'tensor_scalar', pfk + ['grow%d' % s3], [ork], orw[:], pf, grow[s3][:, e:e + 1], None, ALU.mult)
            P.op('pool', lambda en, orw=orw, col=col: en.indirect_dma_start(
                out=y_d[:, :], out_offset=bass.IndirectOffsetOnAxis(ap=idxs[:, col:col + 1], axis=0),
                in_=orw[:, :], in_offset=None, compute_op=ALU.add), [ork, 'idxs'], ['y_d'], dma=True)

    ensure_dma(3)
    ensure_cast(3)
    gathers(0)
    gathers(1)
    build_xgT(0)
    for e in range(NE):
        if e + 2 < NE:
            gathers(e + 2)
        xg = xgT[e % 2]; xgk = 'xgT%d' % (e % 2)
        for fq in range(3):
            wgb, wgk = get_piece(e * 9 + fq * 2)
            wub, wuk = get_piece(e * 9 + fq * 2 + 1)
            for fc in range(4):
                f = fq * 4 + fc
                pg, pgk = ps_half()
                mm(pg, [(wgb[:, k, fc * 128:(fc + 1) * 128], xg[:, k, :]) for k in range(8)], [wgk, xgk], [pgk])
                pu, puk = ps_half()
                mm(pu, [(wub[:, k, fc * 128:(fc + 1) * 128], xg[:, k, :]) for k in range(8)], [wuk, xgk], [puk])
                sg_ = sgt[f % 2]; sgk = 'sgt%d' % (f % 2)
                act(sg_[:], pg, AF.Silu, [pgk], [sgk])
                V('dve', 'tensor_tensor', [sgk, puk], ['hid'], hid[:, f, :], sg_[:], pu, ALU.mult)
            if fq == 0 and e >= 1:
                outT(e - 1)
        if e + 1 < NE:
            build_xgT(e + 1)
        wds = [get_piece(e * 9 + 6 + dq) for dq in range(3)]
        for ec in range(8):
            po, pok = ps_half()
            mm(po, [(wds[f // 4][0][:, f % 4, ec * 128:(ec + 1) * 128], hid[:, f, :]) for f in range(12)],
               [wds[0][1], wds[1][1], wds[2][1], 'hid'], [pok])
            act(og[:, ec, :], po, AF.Identity, [pok, 'ADA', 'zcol'], ['og%d' % ec], scale=ada(5, ec), bias=zcol[:])
    outT(NE - 1)
    P.barrier()
    sb.reset(m6)
    if stop_after <= 6:
        return finish(nc, es, P)

    FG = sb.alloc([128, D], F32)
    dma('sp', FG[:], fg_d.partition_broadcast(128), [], ['FG'])
    xbs = [sb.alloc([128, D], BF) for _ in range(6)]
    yts = [sb.alloc([128, D], F32) for _ in range(6)]
    ots = [sb.alloc([128, D], F32) for _ in range(4)]
    junk = sb.alloc([128, D], BF)
    sss = [sb.alloc([128, 1], F32) for _ in range(6)]
    for ti in range(32):
        b = ti % 6
        xb = xbs[b]; xk = 'xb%d' % b; yt = yts[b]; yk = 'yt%d' % b; ss = sss[b]; sk = 'ss%d' % b
        ot = ots[ti % 4]; ok_ = 'ot%d' % (ti % 4)
        dma('sp', xb[:], x1_d[ti * 128:(ti + 1) * 128, :], ['x1_d'], [xk])
        dma('sp', yt[:], y_d[ti * 128:(ti + 1) * 128, :], ['y_d'], [yk])
        V('dve', 'tensor_tensor', [xk, yk], [yk], yt[:], yt[:], xb[:], ALU.add)
        rms_tile(yt, yk, ss, sk, junk)
        V('dve', 'scalar_tensor_tensor', [yk, sk, 'FG'], [ok_], ot[:], yt[:], ss[:, 0:1], FG[:], ALU.mult, ALU.mult)
        dma('pool', out_d[ti * 128:(ti + 1) * 128, :], ot[:], [ok_], ['out_d'])
    return finish(nc, es, P)


def finish(nc, es, P):
    P.barrier()
    with nc.Block() as block:
        @block.tensor
        def _(e):
            P.emit(e, 'pe')

        @block.scalar
        def _(e):
            P.emit(e, 'act')

        @block.vector
        def _(e):
            P.emit(e, 'dve')

        @block.gpsimd
        def _(e):
            P.emit(e, 'pool')

        @block.sync
        def _(e):
            P.emit(e, 'sp')
    es.close()
    return nc


def _consts():
    bf = ml_dtypes.bfloat16
    n = np.arange(128)
    ang = 2 * np.pi * np.outer(n, n) / 128.0
    cs128 = np.concatenate([np.cos(ang), np.sin(ang)], axis=1)
    t1 = np.arange(64); k1 = np.arange(64)
    e1 = np.zeros((32, 128, 512))
    for m in range(32):
        for t2l in range(2):
            t2 = 2 * m + t2l
            ph = 2 * np.pi * (np.outer(t1, k1) / 64.0 + (k1[None, :] * t2) / 4096.0)
            sl = slice(t2l * 64, t2l * 64 + 64)
            e1[m, sl, 0 + t2l * 64:0 + t2l * 64 + 64] = np.cos(ph)
            e1[m, sl, 128 + t2l * 64:128 + t2l * 64 + 64] = np.sin(ph)
            e1[m, sl, 256 + t2l * 64:256 + t2l * 64 + 64] = -np.sin(ph)
            e1[m, sl, 384 + t2l * 64:384 + t2l * 64 + 64] = np.cos(ph)
    e1 = np.ascontiguousarray(e1.transpose(1, 0, 2).reshape(128, 32 * 512))
    s = 1.0 / math.sqrt(4096.0 * 128.0)
    e2 = np.zeros((128, 256))
    t2 = np.arange(64); k2 = np.arange(64)
    ph = 2 * np.pi * np.outer(t2, k2) / 64.0
    for k1l in range(2):
        e2[k1l * 64:k1l * 64 + 64, k1l * 64:k1l * 64 + 64] = np.cos(ph) * s
        e2[k1l * 64:k1l * 64 + 64, 128 + k1l * 64:128 + k1l * 64 + 64] = -np.sin(ph) * s
    quarter = D // 4
    freqs = np.exp(-math.log(10000.0) * np.arange(quarter, dtype=np.float32) / quarter).astype(np.float32)
    ang_r = np.arange(64, dtype=np.float32)[:, None] * freqs
    emb_r = np.concatenate([np.sin(ang_r), np.cos(ang_r)], axis=-1)
    emb = np.concatenate([np.broadcast_to(emb_r[:, None, :], (64, 64, D // 2)),
                          np.broadcast_to(emb_r[None, :, :], (64, 64, D // 2))], axis=-1).reshape(T, D).astype(np.float32)
    return dict(cs128=cs128.astype(bf), e1=e1.astype(bf), e2=e2.astype(bf), pos=np.ascontiguousarray(emb),
                identf=np.eye(128, dtype=np.float32), identb=np.eye(128).astype(bf),
                iota_c=np.ascontiguousarray(np.broadcast_to(np.arange(512, dtype=np.float32)[None, :], (128, 512))),
                cval=(np.arange(4)[None, :] * 128 + np.arange(128)[:, None]).astype(np.float32),
                jcol=(np.arange(128) % 32).astype(np.float32).reshape(128, 1))


def _fm(v):
    return np.ascontiguousarray(np.asarray(v, np.float32).reshape(8, 128).T)


def make_in_maps(inputs, cores):
    f = lambda a: np.ascontiguousarray(np.asarray(a, np.float32))
    cst = _consts()
    l = 0
    shared = dict(cst)
    shared.update(
        w_ada=f(inputs['w_ada'][l]), b_ada_fm=np.ascontiguousarray(f(inputs['b_ada'][l]).reshape(48, 128).T),
        n1g_fm=_fm(inputs['norm1_g'][l]), n2g_fm=_fm(inputs['norm2_g'][l]), final_g=f(inputs['final_g']).reshape(1, D),
        w_in=f(inputs['w_in'][l]), w_four=f(inputs['w_four'][l]), w_lru=f(inputs['w_lru'][l]), w_out=f(inputs['w_out'][l]),
        conv_w_fm=np.ascontiguousarray(f(inputs['conv_w'][l]).reshape(4, 8, 128).transpose(2, 1, 0).reshape(128, 32)),
        conv_b_fm=_fm(inputs['conv_b'][l]),
        lam_fm=np.ascontiguousarray(f(inputs['lru_lambda'][l]).reshape(2, 8, 128).transpose(2, 0, 1).reshape(128, 16)),
        ba_fm=np.ascontiguousarray(f(inputs['lru_ba'][l]).reshape(2, 8, 128).transpose(2, 0, 1).reshape(128, 16)),
        bi_fm=np.ascontiguousarray(f(inputs['lru_bi'][l]).reshape(2, 8, 128).transpose(2, 0, 1).reshape(128, 16)),
        lru_wa=f(inputs['lru_wa'][l]).reshape(16, 128, 128), lru_wi=f(inputs['lru_wi'][l]).reshape(16, 128, 128),
        w_router_fm=np.ascontiguousarray(f(inputs['w_router'][l]).reshape(8, 128, 16).transpose(1, 0, 2).reshape(128, 128)),
        w_gate_e=f(inputs['w_gate_e'][l]), w_up_e=f(inputs['w_up_e'][l]), w_down_e=f(inputs['w_down_e'][l]),
    )
    cctx = _fm(inputs['c_ctx'])
    maps = []
    for b in cores:
        m = dict(shared)
        m['x'] = f(inputs['x'][b]); m['ctx'] = f(inputs['ctx'][b])
        m['cvec'] = np.ascontiguousarray(np.concatenate([_fm(inputs['c'][b]), cctx], axis=1))
        maps.append(m)
    return maps


def kernel(**inputs):
    nc = build_program()
    maps = make_in_maps(inputs, list(range(8)))
    res = run_bass_kernel_spmd(nc, maps, core_ids=list(range(8)))
    return np.stack([np.asarray(r["out"], np.float32) for r in res.results], axis=0)
```

```python
import contextlib
import math
import numpy as np
import ml_dtypes
import concourse.bass as bass
import concourse.mybir as mybir
from concourse.bass_utils import run_bass_kernel_spmd

F32 = mybir.dt.float32
BF = mybir.dt.bfloat16
I32 = mybir.dt.int32
ALU = mybir.AluOpType
AF = mybir.ActivationFunctionType
AX = mybir.AxisListType

T = 4096
TC = 256
TT = T + TC
D = 1024
NE = 16
CAP = 512
DF = 1536
EPS = 1e-6
DEBUG = False
STOP_AFTER = 99
CUT = 0
LAGS = [0, 1, 3, 4]


class Prog:
    def __init__(self, nc, es):
        self.nc = nc
        self.q = {k: [] for k in ['pe', 'act', 'dve', 'pool', 'sp']}
        self.sem = {k: es.enter_context(nc.semaphore('s_' + k)) for k in ['pe', 'act', 'dve', 'pool']}
        self.cnt = {k: 0 for k in self.sem}
        self.dsem = {qn: [es.enter_context(nc.semaphore('d_%s%d' % (qn, i))) for i in range(n)]
                     for qn, n in [('sp', 14), ('pool', 10), ('act', 6)]}
        self.dcnt = {qn: 0 for qn in self.dsem}
        self.dtarget = {}
        self.lastw = {}
        self.readers = {}
        self.waited = {}
        self.semobj = {}

    def _sid(self, s):
        self.semobj[id(s)] = s
        return id(s)

    def op(self, queue, fn, reads=(), writes=(), dma=False):
        deps = set()
        for r in reads:
            if r in self.lastw:
                deps.add(self.lastw[r])
        for w in writes:
            if w in self.lastw:
                deps.add(self.lastw[w])
            for rd in self.readers.get(w, ()):
                deps.add(rd)
        if dma:
            pool = self.dsem[queue]
            i = self.dcnt[queue]
            self.dcnt[queue] += 1
            s = pool[i % len(pool)]
            rnd = i // len(pool)
            ev = (self._sid(s), 16 * (rnd + 1))
            if rnd > 0:
                deps.add((self._sid(s), 16 * rnd))
            inc = (s, 16)
            self.dtarget[self._sid(s)] = 16 * (rnd + 1)
        else:
            self.cnt[queue] += 1
            s = self.sem[queue]
            ev = (self._sid(s), self.cnt[queue])
            inc = (s, 1)
        waits = {}
        for (sid, v) in deps:
            if queue == 'pe' and not dma and sid == id(self.sem['pe']):
                continue
            if self.waited.get((queue, sid), 0) >= v:
                continue
            waits[sid] = max(waits.get(sid, 0), v)
        for sid, v in waits.items():
            self.waited[(queue, sid)] = v
        self.q[queue].append(([(self.semobj[sid], v) for sid, v in waits.items()], fn, inc))
        for r in reads:
            self.readers.setdefault(r, []).append(ev)
        for w in writes:
            self.lastw[w] = ev
            self.readers[w] = []
        return ev

    def barrier(self):
        evs = []
        for k, s in self.sem.items():
            if self.cnt[k] > 0:
                evs.append((self._sid(s), self.cnt[k]))
        for sid, v in self.dtarget.items():
            evs.append((sid, v))
        for queue in self.q:
            waits = []
            for (sid, v) in evs:
                if self.waited.get((queue, sid), 0) >= v:
                    continue
                self.waited[(queue, sid)] = v
                waits.append((self.semobj[sid], v))
            if waits:
                self.q[queue].append((waits, None, None))

    def emit(self, e, queue):
        for waits, fn, inc in self.q[queue]:
            for s, v in waits:
                e.wait_ge(s, v)
            if fn is not None:
                ins = fn(e)
                ins.then_inc(inc[0], inc[1])


class SBAlloc:
    def __init__(self, nc, base=20736, end=229376):
        self.nc = nc
        self.cur = base
        self.end = end
        self.n = 0

    def alloc(self, shape, dtype):
        sz = 1
        for s in shape[1:]:
            sz *= s
        sz *= {F32: 4, BF: 2, I32: 4}[dtype]
        sz = (sz + 63) // 64 * 64
        assert self.cur + sz <= self.end, "SBUF overflow %d" % (self.cur + sz - self.end)
        self.n += 1
        t = self.nc.alloc_sbuf_tensor_at('t%d' % self.n, list(shape), dtype, offset=self.cur)
        self.cur += sz
        return t

    def mark(self):
        return self.cur

    def reset(self, m):
        self.cur = m


def build_program(debug=False, stop_after=99):
    nc = bass.Bass("TRN2", target_bir_lowering=False)
    es = contextlib.ExitStack()

    def din(name, shape, dt=F32):
        return nc.dram_tensor(name, list(shape), dt, kind="ExternalInput").ap()

    def dscr(name, shape, dt=F32):
        kind = "ExternalOutput" if debug else "Internal"
        return nc.dram_tensor(name, list(shape), dt, kind=kind).ap()

    x_d = din("x", [T, D]); ctx_d = din("ctx", [TC, D]); pos_d = din("pos", [T, D])
    cvec_d = din("cvec", [128, 16]); wada_d = din("w_ada", [D, 6 * D]); bada_d = din("b_ada_fm", [128, 48])
    n1g_d = din("n1g_fm", [128, 8]); n2g_d = din("n2g_fm", [128, 8]); fg_d = din("final_g", [1, D])
    win_d = din("w_in", [D, 4608]); wfour_d = din("w_four", [512, D]); wlru_d = din("w_lru", [D, D]); wout_d = din("w_out", [D, D])
    cw_d = din("conv_w_fm", [128, 32]); cb_d = din("conv_b_fm", [128, 8]); lam_d = din("lam_fm", [128, 16])
    ba_d = din("ba_fm", [128, 16]); bi_d = din("bi_fm", [128, 16])
    wa_d = din("lru_wa", [16, 128, 128]); wi_d = din("lru_wi", [16, 128, 128])
    wr_d = din("w_router_fm", [128, 128])
    wg_d = din("w_gate_e", [NE, D, DF]); wu_d = din("w_up_e", [NE, D, DF]); wd_d = din("w_down_e", [NE, DF, D])
    cs128_d = din("cs128", [128, 256], BF); e1_d = din("e1", [128, 32 * 512], BF); e2_d = din("e2", [128, 256], BF)
    identf_d = din("identf", [128, 128]); identb_d = din("identb", [128, 128], BF)
    iotac_d = din("iota_c", [128, 512]); cval_d = din("cval", [128, 4]); jcol_d = din("jcol", [128, 1])
    out_d = nc.dram_tensor("out", [T, D], F32, kind="ExternalOutput").ap()

    zf_d = dscr("zf_s", [512, T], BF); xr_d = dscr("xr_s", [D, TT], BF); gg_d = dscr("gg_s", [D, T], BF)
    sa_d = dscr("sa_s", [D, T], BF); sb_d = dscr("sb_s", [D, T], BF); yin_d = dscr("yin_s", [D, T], BF)
    yf_d = dscr("yf_s", [512, T], BF); x1_d = dscr("x1_s", [T, D], BF); xn2_d = dscr("xn2_s", [T, D], BF)
    sc_d = dscr("sc_s", [T, NE]); cs_d = dscr("cs_s", [NE, T]); m_d = dscr("m_s", [NE, T]); y_d = dscr("y_s", [T, D])
    idx_dbg = dscr("idx_s", [128, 64])

    P = Prog(nc, es)
    sb = SBAlloc(nc)
    PS = [nc.alloc_psum_tensor('ps%d' % i, [128, 1024], F32) for i in range(4)]
    pst = {'h': 0, 'f': 0}

    def ps_half():
        i = pst['h'] % 8
        pst['h'] += 1
        return PS[i // 2][:, (i % 2) * 512:(i % 2) * 512 + 512], 'ps%d' % i

    def ps_full():
        i = pst['f'] % 4
        pst['f'] += 1
        return PS[i][:, :], ['ps%d' % (2 * i), 'ps%d' % (2 * i + 1)]

    def dma(queue, out, in_, reads, writes, **kw):
        P.op(queue, lambda e, out=out, in_=in_, kw=kw: e.dma_start(out=out, in_=in_, **kw), reads, writes, dma=True)

    def mm(out_ap, pairs, reads, writes):
        def fn(e, out_ap=out_ap, pairs=pairs):
            n = len(pairs)
            for i, (l, r) in enumerate(pairs):
                ins = e.matmul(out_ap, l, r, start=(i == 0), stop=(i == n - 1))
            return ins
        P.op('pe', fn, reads, writes)

    def tr(out_ap, in_ap, ident, reads, writes):
        P.op('pe', lambda e, o=out_ap, i=in_ap, idn=ident: e.transpose(o, i, idn), reads, writes)

    def act(out, in_, func, reads, writes, eng='act', **kw):
        P.op('act', lambda e, out=out, in_=in_, func=func, kw=kw: e.activation(out=out, in_=in_, func=func, **kw), reads, writes)

    def V(eng, meth, reads, writes, *a, **kw):
        P.op(eng, lambda e, meth=meth, a=a, kw=kw: getattr(e, meth)(*a, **kw), reads, writes)

    identf = sb.alloc([128, 128], F32); identb = sb.alloc([128, 128], BF)
    ADA = sb.alloc([128, 96], F32)
    scale1 = sb.alloc([128, 8], F32); cscale1 = sb.alloc([128, 8], F32); scale2 = sb.alloc([128, 8], F32)
    n1g = sb.alloc([128, 8], F32); n2g = sb.alloc([128, 8], F32)
    dma('sp', identf[:], identf_d, [], ['identf']); dma('sp', identb[:], identb_d, [], ['identb'])
    dma('sp', n1g[:], n1g_d, [], ['n1g']); dma('sp', n2g[:], n2g_d, [], ['n2g'])

    def ada(t, k=None, ctx=False):
        base = 48 if ctx else 0
        if k is None:
            return ADA[:, base + t * 8: base + t * 8 + 8]
        return ADA[:, base + t * 8 + k: base + t * 8 + k + 1]

    m0 = sb.mark()
    cv = sb.alloc([128, 16], F32); sg = sb.alloc([128, 16], F32); scv = sb.alloc([128, 16], F32)
    bada = sb.alloc([128, 48], F32)
    wst = [sb.alloc([128, 8, 512], F32) for _ in range(3)]
    wbf0 = [sb.alloc([128, 8, 512], BF) for _ in range(2)]
    dma('sp', cv[:], cvec_d, [], ['cv']); dma('sp', bada[:], bada_d, [], ['bada'])
    act(sg[:], cv[:], AF.Sigmoid, ['cv'], ['sg'])
    scvb = sb.alloc([128, 16], BF)
    V('dve', 'tensor_tensor', ['cv', 'sg'], ['scv'], scvb[:], cv[:], sg[:], ALU.mult)
    pada, kada = ps_half()
    scv3 = scvb[:].rearrange("p (c k) -> p c k", k=8)
    for pc in range(12):
        w = wst[pc % 3]; wb = wbf0[pc % 2]; wk = 'wst%d' % (pc % 3); wbk = 'wbf%d' % (pc % 2)
        dma('sp', w[:], wada_d[:, pc * 512:(pc + 1) * 512].rearrange("(k p) e -> p k e", p=128), [], [wk])
        act(wb[:, 0:4, :], w[:, 0:4, :], AF.Copy, [wk], [wbk])
        V('dve', 'tensor_copy', [wk, wbk], [wbk], wb[:, 4:8, :], w[:, 4:8, :])
        for jj in range(4):
            j = pc * 4 + jj
            mm(pada[:, 2 * j:2 * j + 2], [(wb[:, k, jj * 128:(jj + 1) * 128], scv3[:, :, k]) for k in range(8)],
               [wbk, 'scv'], [kada])
    pv = pada[:, 0:96].rearrange("p (j c) -> p c j", c=2)
    V('dve', 'tensor_tensor', [kada, 'bada'], ['ADA'], ADA[:, 0:48], pv[:, 0, :], bada[:], ALU.add)
    V('dve', 'tensor_tensor', [kada, 'bada', 'ADA'], ['ADA'], ADA[:, 48:96], pv[:, 1, :], bada[:], ALU.add)
    V('dve', 'scalar_tensor_tensor', ['ADA', 'n1g'], ['scale1'], scale1[:], ada(1), 1.0, n1g[:], ALU.add, ALU.mult)
    V('dve', 'scalar_tensor_tensor', ['ADA', 'n1g'], ['cscale1'], cscale1[:], ada(1, ctx=True), 1.0, n1g[:], ALU.add, ALU.mult)
    V('dve', 'scalar_tensor_tensor', ['ADA', 'n2g'], ['scale2'], scale2[:], ada(4), 1.0, n2g[:], ALU.add, ALU.mult)
    P.barrier()
    sb.reset(m0)

    def rms_tile(xt, xkey, ss, sskey, junk):
        act(junk[:], xt[:], AF.Square, [xkey], ['junk', sskey], accum_out=ss[:])
        act(ss[:], ss[:], AF.Sqrt, [sskey, 'epsb'], [sskey], scale=1.0 / D, bias=epsb[:])
        V('dve', 'reciprocal', [sskey], [sskey], ss[:], ss[:])

    epsb = sb.alloc([128, 1], F32)
    V('dve', 'memset', [], ['epsb'], epsb[:], EPS)
    zcol = sb.alloc([128, 1], F32)
    V('dve', 'memset', [], ['zcol'], zcol[:], 0.0)
    m1 = sb.mark()
    hT = sb.alloc([128, 8, TT], BF)
    xts = [sb.alloc([128, D], F32) for _ in range(4)]
    pts = [sb.alloc([128, D], F32) for _ in range(3)]
    xn16 = [sb.alloc([128, D], BF) for _ in range(4)]
    junk = sb.alloc([128, D], BF)
    sss = [sb.alloc([128, 1], F32) for _ in range(4)]
    for pr_ in range(17):
        pi = pr_ % 4
        pbf = PS[pi][:, :].bitcast(BF)
        tokp = pr_ * 256
        isctx = (pr_ == 0)
        for t_ in range(2):
            i = pr_ * 2 + t_
            xt = xts[i % 4]; xk = 'xt%d' % (i % 4); ss = sss[i % 4]; sk = 'ss%d' % (i % 4)
            xn = xn16[i % 4]; xnk = 'xn16_%d' % (i % 4)
            if isctx:
                dma('sp', xt[:], ctx_d[i * 128:(i + 1) * 128, :], [], [xk])
            else:
                jx = i - 2
                pt = pts[i % 3]; pk = 'pt%d' % (i % 3)
                dma('sp', xt[:], x_d[jx * 128:(jx + 1) * 128, :], [], [xk])
                dma('sp', pt[:], pos_d[jx * 128:(jx + 1) * 128, :], [], [pk])
                V('pool', 'tensor_tensor', [xk, pk], [xk], xt[:], xt[:], pt[:], ALU.add)
            rms_tile(xt, xk, ss, sk, junk)
            V('dve', 'tensor_scalar', [xk, sk], [xnk], xn[:], xt[:], ss[:, 0:1], None, ALU.mult)
            for k in range(8):
                c0 = k * 256 + t_ * 128
                tr(pbf[:, c0:c0 + 128], xn[:, k * 128:(k + 1) * 128], identb[:], [xnk, 'identb'], ['ps%d' % (2 * pi + k // 4)])
        scl = cscale1 if isctx else scale1
        for k in range(8):
            c0 = k * 256
            pkey = 'ps%d' % (2 * pi + k // 4)
            if k < 4:
                act(hT[:, k, tokp:tokp + 256], pbf[:, c0:c0 + 256], AF.Identity,
                    [pkey, 'scale1', 'cscale1', 'ADA'], ['hT%d_%d' % (pr_, k)], scale=scl[:, k:k + 1], bias=ada(0, k, ctx=isctx))
            else:
                V('dve', 'tensor_scalar', [pkey, 'scale1', 'cscale1', 'ADA'], ['hT%d_%d' % (pr_, k)], hT[:, k, tokp:tokp + 256], pbf[:, c0:c0 + 256],
                  scl[:, k:k + 1], ada(0, k, ctx=isctx), ALU.mult, ALU.add)
    hT_keys = ['hT%d_%d' % (i, k) for i in range(17) for k in range(8)]

    wst = [sb.alloc([128, 8, 512], F32) for _ in range(2)]
    wbf = [sb.alloc([128, 8, 512], BF) for _ in range(2)]
    ost = [sb.alloc([128, TT], BF) for _ in range(2)]
    gt = [sb.alloc([128, 512], F32) for _ in range(6)]
    gz = [sb.alloc([128, 512], F32) for _ in range(6)]
    gctr = [0]
    gpend = []
    chunks = [(0, TC)] + [(TC + 512 * c, 512) for c in range(8)]
    ecount = 0
    def p1_dma(pc):
        dma('sp', wst[pc % 2][:], win_d[:, pc * 512:(pc + 1) * 512].rearrange("(k p) e -> p k e", p=128), [], ['wst%d' % (pc % 2)])

    def p1_cast(pc):
        w = wst[pc % 2]; wb = wbf[pc % 2]; wk = 'wst%d' % (pc % 2); wbk = 'wbf%d' % (pc % 2)
        act(wb[:, 0:4, :], w[:, 0:4, :], AF.Copy, [wk], [wbk])
        V('dve', 'tensor_copy', [wk, wbk], [wbk], wb[:, 4:8, :], w[:, 4:8, :])

    p1_dma(0)
    p1_cast(0)
    p1_dma(1)
    for pc in range(9):
        wb = wbf[pc % 2]; wbk = 'wbf%d' % (pc % 2)
        for jj in range(4):
            ec = pc * 4 + jj
            typ = 'F' if ec < 4 else 'XR' if ec < 12 else 'GG' if ec < 20 else 'SA' if ec < 28 else 'SB'
            o = ost[ecount % 2]; ok = 'ost%d' % (ecount % 2); ecount += 1
            ob = o[:]
            for ci, (t0, n) in enumerate(chunks):
                if ci == 0 and typ != 'XR':
                    continue
                ph, pk = ps_half()
                mm(ph[:, 0:n], [(wb[:, k, jj * 128:(jj + 1) * 128], hT[:, k, t0:t0 + n]) for k in range(8)],
                   [wbk] + hT_keys, [pk])
                if typ == 'XR':
                    act(ob[:, t0:t0 + n], ph[:, 0:n], AF.Copy, [pk], [ok])
                elif typ == 'F':
                    cF = (t0 - TC) // 512
                    zdst = ob[:, 0:T].rearrange("p (b a) -> p a b", a=64)[:, 8 * cF:8 * cF + 8, :]
                    act(zdst, ph[:, 0:n].rearrange("p (a b) -> p a b", b=64), AF.Copy, [pk], [ok])
                elif typ in ('SA', 'SB'):
                    act(ob[:, t0 - TC:t0 - TC + n], ph[:, 0:n], AF.Sigmoid, [pk], [ok])
                else:
                    gi = gctr[0] % 6; gctr[0] += 1
                    g0 = gt[gi]; gk = 'gt%d' % gi; zc = gz[gi]; zk = 'gz%d' % gi
                    act(zc[:], ph, AF.Copy, [pk], [zk])
                    act(g0[:], zc[:], AF.Square, [zk], [gk], scale=0.21145921593541212)
                    V('dve', 'scalar_tensor_tensor', [gk, zk], [gk], g0[:], g0[:], 1.0, zc[:], ALU.add, ALU.mult)
                    def fin(g0=g0, gk=gk, zc=zc, zk=zk, ob=ob, ok=ok, a=t0 - TC, n=n):
                        act(g0[:], g0[:], AF.Sigmoid, [gk], [gk], scale=1.5957691216057308)
                        V('dve', 'tensor_tensor', [gk, zk], [ok], ob[:, a:a + n], g0[:], zc[:], ALU.mult)
                    gpend.append(fin)
                    if len(gpend) > 2:
                        gpend.pop(0)()
            while gpend:
                gpend.pop(0)()
            if typ == 'F':
                dma('pool', zf_d[ec * 128:(ec + 1) * 128, :], ob[:, 0:T], [ok], ['zf_d'])
            elif typ == 'XR':
                dma('pool', xr_d[(ec - 4) * 128:(ec - 3) * 128, :], ob[:, 0:TT], [ok], ['xr_d'])
            else:
                dd = {'GG': gg_d, 'SA': sa_d, 'SB': sb_d}[typ]
                e0 = (ec - 12) % 8
                dma('pool', dd[e0 * 128:(e0 + 1) * 128, :], ob[:, 0:T], [ok], [typ + '_d'])
            if jj == 2 and pc + 1 < 9:
                p1_cast(pc + 1)
        if pc + 2 < 9:
            p1_dma(pc + 2)
    P.barrier()
    sb.reset(m1)
    if stop_after <= 1:
        return finish(nc, es, P)

    m2 = sb.mark()
    cw = sb.alloc([128, 32], F32); cb = sb.alloc([128, 8], F32); lam = sb.alloc([128, 16], F32)
    ba = sb.alloc([128, 16], F32); bi = sb.alloc([128, 16], F32); nsp = sb.alloc([128, 16], F32)
    dma('sp', cw[:], cw_d, [], ['cw']); dma('sp', cb[:], cb_d, [], ['cb']); dma('sp', lam[:], lam_d, [], ['lam'])
    dma('sp', ba[:], ba_d, [], ['ba']); dma('sp', bi[:], bi_d, [], ['bi'])
    act(nsp[:], lam[:], AF.Exp, ['lam'], ['nsp'], scale=-1.0)
    act(nsp[:], nsp[:], AF.Ln, ['nsp'], ['nsp'], bias=1.0)
    V('dve', 'tensor_scalar', ['nsp'], ['nsp'], nsp[:], nsp[:], -8.0, None, ALU.mult)
    WA = sb.alloc([128, 16, 128], BF); WI = sb.alloc([128, 16, 128], BF)
    DG = sb.alloc([128, 32, 128], BF)
    ggt = sb.alloc([128, T], BF); yint = sb.alloc([128, T], BF)
    wgs = yint[:].bitcast(F32).rearrange("p (m j) -> p m j", j=128)
    dma('sp', wgs, wa_d.rearrange("m i j -> i m j"), [], ['yint'])
    act(WA[:], wgs, AF.Copy, ['yint'], ['WA'])
    dma('sp', wgs, wi_d.rearrange("m i j -> i m j"), ['yint'], ['yint'])
    act(WI[:], wgs, AF.Copy, ['yint'], ['WI'])
    for h in range(8):
        for k in range(4):
            V('dve', 'tensor_scalar', ['identf', 'cw'], ['DG'], DG[:, h * 4 + k, :], identf[:], cw[:, h * 4 + k:h * 4 + k + 1], None, ALU.mult)
    XPW = 4448
    xpads = [sb.alloc([128, XPW], BF)] * 2
    Ubs = [sb.alloc([128, TT], BF) for _ in range(2)]
    Rb = sb.alloc([128, TT], BF)
    Ibs = [sb.alloc([128, TT], BF) for _ in range(3)]
    Abs = [sb.alloc([128, TT], F32) for _ in range(3)]
    T1s = [sb.alloc([128, TT], F32) for _ in range(2)]; Y = sb.alloc([128, T], F32); Hc = sb.alloc([128, TC], F32)
    V('pool', 'memset', [], ['xpad0'], xpads[0][:], 0.0)
    NCH = len(chunks)

    def ck(nm, ci):
        return '%s%d' % (nm, ci)

    def conv_load(h):
        xp = xpads[0]; xk = 'xpad0'
        dma('sp', xp[:, 32:32 + TC], xr_d[h * 128:(h + 1) * 128, 0:TC], [], [xk])
        dma('sp', xp[:, 320:320 + T], xr_d[h * 128:(h + 1) * 128, TC:TT], [], [xk])

    conv_ps = {}

    def conv_mm(h, ci):
        xp = xpads[0]; xk = 'xpad0'
        t0, n = chunks[ci]
        base = 32 if ci == 0 else 320
        s_ = t0 if ci == 0 else t0 - TC
        ph, pk = ps_half()
        mm(ph[:, 0:n], [(DG[:, h * 4 + k, :], xp[:, base + s_ + k - 2: base + s_ + k - 2 + n]) for k in range(4)],
           ['DG', xk], [pk])
        conv_ps[(h, ci)] = (ph, pk)

    def conv_ev(h, ci):
        t0, n = chunks[ci]
        ph, pk = conv_ps.pop((h, ci))
        V('dve', 'tensor_scalar', [pk, 'cb'], ['Ub%d_%d' % (h % 2, ci)], Ubs[h % 2][:, t0:t0 + n], ph[:, 0:n], cb[:, h:h + 1], None, ALU.add)

    conv_load(0)
    for ci in range(NCH):
        conv_mm(0, ci)
        conv_ev(0, ci)
    ggts = [ggt, ggt]
    yints = [yint]

    def unit_ctx(u):
        h = u // 2; d = u % 2
        return h, d, d * 8 + h, Ubs[h % 2], Ibs[u % 3], Abs[u % 3], T1s[d]

    def ubk_(h, ci):
        return 'Ub%d_%d' % (h % 2, ci)

    def sG(u):
        h, d, col, ub, Ib, Ab, T1 = unit_ctx(u)
        for ci, (t0, n) in enumerate(chunks):
            ph, pk = ps_half()
            mm(ph[:, 0:n], [(WA[:, col, :], ub[:, t0:t0 + n])], ['WA', ubk_(h, ci)], [pk])
            act(Rb[:, t0:t0 + n], ph[:, 0:n], AF.Sigmoid, [pk, 'ba'], [ck('R', ci)], bias=ba[:, col:col + 1], scale=1.0)
            ph, pk = ps_half()
            mm(ph[:, 0:n], [(WI[:, col, :], ub[:, t0:t0 + n])], ['WI', ubk_(h, ci)], [pk])
            act(Ib[:, t0:t0 + n], ph[:, 0:n], AF.Sigmoid, [pk, 'bi'], ['I%d_%d' % (u % 3, ci)], bias=bi[:, col:col + 1], scale=1.0)

    cgroups = [[0, 1, 2], [3, 4, 5], [6, 7, 8]]

    def grange(g_):
        a0 = chunks[g_[0]][0]
        a1 = chunks[g_[-1]][0] + chunks[g_[-1]][1]
        return a0, a1

    def sE(u):
        h, d, col, ub, Ib, Ab, T1 = unit_ctx(u)
        for g_ in cgroups:
            a0, a1 = grange(g_)
            act(Ab[:, a0:a1], Rb[:, a0:a1], AF.Exp, [ck('R', ci) for ci in g_] + ['nsp'], ['A%d_%d' % (u % 3, ci) for ci in g_], scale=nsp[:, col:col + 1])

    def sQ(u):
        h, d, col, ub, Ib, Ab, T1 = unit_ctx(u)
        for ci, (t0, n) in enumerate(chunks):
            V('pool', 'tensor_tensor', ['I%d_%d' % (u % 3, ci), ubk_(h, ci)], ['I%d_%d' % (u % 3, ci)], Ib[:, t0:t0 + n], Ib[:, t0:t0 + n], ub[:, t0:t0 + n], ALU.mult)
        for g_ in cgroups:
            a0, a1 = grange(g_)
            act(T1[:, a0:a1], Ab[:, a0:a1], AF.Square, ['A%d_%d' % (u % 3, ci) for ci in g_], ['T%d_%d' % (d, ci) for ci in g_])

    def sR(u):
        h, d, col, ub, Ib, Ab, T1 = unit_ctx(u)
        for g_ in cgroups:
            a0, a1 = grange(g_)
            act(T1[:, a0:a1], T1[:, a0:a1], AF.Sqrt, ['T%d_%d' % (d, ci) for ci in g_], ['T%d_%d' % (d, ci) for ci in g_], scale=-1.0, bias=1.0)

    def sBm(u):
        h, d, col, ub, Ib, Ab, T1 = unit_ctx(u)
        for ci, (t0, n) in enumerate(chunks):
            V('dve', 'tensor_tensor', ['I%d_%d' % (u % 3, ci), 'T%d_%d' % (d, ci)], ['I%d_%d' % (u % 3, ci)], Ib[:, t0:t0 + n], Ib[:, t0:t0 + n], T1[:, t0:t0 + n], ALU.mult)

    def sSC(u):
        h, d, col, ub, Ib, Ab, T1 = unit_ctx(u)
        allA = ['A%d_%d' % (u % 3, ci) for ci in range(NCH)]; allI = ['I%d_%d' % (u % 3, ci) for ci in range(NCH)]; allT = ['T%d_%d' % (d, ci) for ci in range(NCH)]
        if d == 0:
            V('dve', 'tensor_tensor_scan', [allA[0], allI[0]], ['Hc'], Hc[:], Ab[:, 0:TC], Ib[:, 0:TC], 0.0, ALU.mult, ALU.add)
            V('dve', 'tensor_tensor_scan', allA[1:] + allI[1:] + ['Hc'], ['Y'], Y[:], Ab[:, TC:TT], Ib[:, TC:TT], Hc[:, TC - 1:TC], ALU.mult, ALU.add)
        else:
            V('dve', 'tensor_tensor_scan', [allA[0], allI[0]], ['Hc'], Hc[:, ::-1], Ab[:, 0:TC][:, ::-1], Ib[:, 0:TC][:, ::-1], 0.0, ALU.mult, ALU.add)
            V('dve', 'tensor_tensor_scan', allA[1:] + allI[1:] + allT[1:] + ['Hc'], allT[1:], T1[:, TC:TT][:, ::-1], Ab[:, TC:TT][:, ::-1], Ib[:, TC:TT][:, ::-1],
              Hc[:, 0:1], ALU.mult, ALU.add)
            V('pool', 'tensor_tensor', allT[1:] + ['Y'], ['Y'], Y[:], Y[:], T1[:, TC:TT], ALU.add)
            gg = ggts[0]; ggk = 'ggt0'
            V('pool', 'tensor_tensor', ['Y', ggk], ['yint'], yints[0][:], Y[:], gg[:], ALU.mult)
            dma('pool', yin_d[h * 128:(h + 1) * 128, :], yints[0][:], ['yint'], ['yin_d'])
            if h + 1 < 8:
                head_prefetch(h + 1)

    def head_prefetch(h):
        dma('pool', ggts[0][:], gg_d[h * 128:(h + 1) * 128, :], [], ['ggt0'])

    head_prefetch(0)
    sG(0); sE(0); sQ(0)
    for u in range(16):
        h = u // 2
        if u % 2 == 0 and h + 1 < 8:
            conv_load(h + 1)
        if u + 1 < 16:
            sG(u + 1); sE(u + 1); sQ(u + 1)
        if u % 2 == 0 and h + 1 < 8:
            for ci in range(NCH):
                conv_mm(h + 1, ci)
                conv_ev(h + 1, ci)
        sR(u); sBm(u); sSC(u)
    P.barrier()
    sb.reset(m2)
    if stop_after <= 2:
        return finish(nc, es, P)

    m3 = sb.mark()
    CS = sb.alloc([128, 256], BF); E1 = sb.alloc([128, 32, 512], BF); E2 = sb.alloc([128, 256], BF)
    dma('sp', CS[:], cs128_d, [], ['CS']); dma('sp', E1[:], e1_d.rearrange("p (m c) -> p m c", c=512), [], ['E1'])
    dma('sp', E2[:], e2_d, [], ['E2'])
    ZF = [sb.alloc([128, T], BF) for _ in range(2)]
    Bfm = sb.alloc([128, 2, T], BF)
    Yfm = [sb.alloc([128, T], BF) for _ in range(2)]
    NR = 8
    Zt = [sb.alloc([128, 256], BF) for _ in range(NR)]
    Bt = [sb.alloc([128, 2, 128], BF) for _ in range(NR)]

    def pipeline(n, stages, lags):
        for step in range(n + lags[-1]):
            for st_, lag in zip(stages, lags):
                i_ = step - lag
                if 0 <= i_ < n:
                    st_(i_)

    for g in range(4):
        zf = ZF[g % 2]; zk = 'ZF%d' % (g % 2); yf = Yfm[g % 2]; yk = 'Yfm%d' % (g % 2)
        dma('sp', zf[:], zf_d[g * 128:(g + 1) * 128, :], [], [zk])
        hold = {}

        def sC(m):
            ph, pk = ps_half()
            mm(ph[:, 0:256], [(zf[:, 128 * m:128 * m + 128], CS[:])], [zk, 'CS'], [pk])
            hold[('c', m)] = (ph, pk)

        def sZ(m):
            z1 = Zt[m % NR]; z1k = 'Zt%d' % (m % NR)
            ph, pk = hold.pop(('c', m))
            if m % 2 == 0:
                act(z1[:], ph[:, 0:256], AF.Copy, [pk], [z1k])
            else:
                V('dve', 'tensor_copy', [pk], [z1k], z1[:], ph[:, 0:256])

        def sS(m):
            z1 = Zt[m % NR]; z1k = 'Zt%d' % (m % NR)
            ph, pk = ps_half()
            mm(ph[:, 0:256], [(z1[:, 0:128], E1[:, m, 0:256]), (z1[:, 128:256], E1[:, m, 256:512])], [z1k, 'E1'], [pk])
            hold[('s', m)] = (ph, pk)

        def sB(m):
            ph, pk = hold.pop(('s', m))
            for c in range(2):
                bdst = Bfm[:, c, :].rearrange("p (k t) -> p t k", t=64)[:, 2 * m:2 * m + 2, :]
                bsrc = ph[:, c * 128:(c + 1) * 128].rearrange("p (l k) -> p l k", l=2)
                if c == 0:
                    act(bdst, bsrc, AF.Copy, [pk], ['Bfm%d' % m])
                else:
                    V('dve', 'tensor_copy', [pk, 'Bfm%d' % m], ['Bfm%d' % m], bdst, bsrc)

        pipeline(32, [sC, sZ, sS, sB], LAGS)
        allB = ['Bfm%d' % m for m in range(32)]

        def sT(n):
            ph, pk = ps_half()
            phb = ph.bitcast(BF)
            for c in range(2):
                tr(phb[:, c * 128:(c + 1) * 128], Bfm[:, c, 128 * n:128 * n + 128], identb[:], allB + ['identb'], [pk])
            hold[('t', n)] = (phb, pk)

        def sE(n):
            b1 = Bt[n % NR]; b1k = 'Bt%d' % (n % NR)
            phb, pk = hold.pop(('t', n))
            if n % 2 == 0:
                act(b1[:], phb[:, 0:256].rearrange("p (c n) -> p c n", c=2), AF.Copy, [pk], [b1k])
            else:
                V('dve', 'tensor_copy', [pk], [b1k], b1[:], phb[:, 0:256].rearrange("p (c n) -> p c n", c=2))

        def sM(n):
            b1 = Bt[n % NR]; b1k = 'Bt%d' % (n % NR)
            ph, pk = ps_half()
            mm(ph[:, 0:128], [(b1[:, 0, :], E2[:, 0:128]), (b1[:, 1, :], E2[:, 128:256])], [b1k, 'E2'], [pk])
            hold[('m', n)] = (ph, pk)

        def sY(n):
            ph, pk = hold.pop(('m', n))
            dst = yf[:].rearrange("p (a b) -> p b a", b=64)[:, 2 * n:2 * n + 2, :]
            if n % 2 == 1:
                act(dst, ph[:, 0:128].rearrange("p (c n) -> p c n", c=2), AF.Copy, [pk], [yk])
            else:
                V('dve', 'tensor_copy', [pk], [yk], dst, ph[:, 0:128].rearrange("p (c n) -> p c n", c=2))

        pipeline(32, [sT, sE, sM, sY], LAGS)
        dma('pool', yf_d[g * 128:(g + 1) * 128, :], yf[:], [yk], ['yf_d'])
    P.barrier()
    sb.reset(m3)
    if stop_after <= 3:
        return finish(nc, es, P)

    Sc = sb.alloc([128, 32, 16], F32); idxs = sb.alloc([128, 64], I32); idx2s = sb.alloc([128, 64], I32)
    m4 = sb.mark()
    S = sb.alloc([128, T], F32)
    wst = [sb.alloc([128, 8, 512], F32) for _ in range(1)]
    WF = sb.alloc([128, 4, D], BF); WL = sb.alloc([128, 8, D], BF); WO = sb.alloc([128, 8, D], BF)
    WRf = sb.alloc([128, 8, 16], F32); WR = sb.alloc([128, 8, 16], BF)
    dma('sp', WRf[:], wr_d.rearrange("p (k e) -> p k e", e=16), [], ['WRf'])
    V('dve', 'tensor_copy', ['WRf'], ['WR'], WR[:], WRf[:])
    cnt = 0
    for (src, dstw, nk) in [(wfour_d, WF, 4), (wlru_d, WL, 8), (wout_d, WO, 8)]:
        for half in range(2):
            w = wst[0]; cnt += 1
            srcv = src[:, half * 512:(half + 1) * 512].rearrange("(k p) e -> p k e", p=128)
            h2_ = nk // 2
            dma('sp', w[:, 0:h2_, :], srcv[:, 0:h2_, :], [], ['wst0a'])
            act(dstw[:, 0:h2_, half * 512:(half + 1) * 512], w[:, 0:h2_, :], AF.Copy, ['wst0a'], ['W4a_%d' % cnt])
            dma('sp', w[:, 4:4 + h2_, :], srcv[:, h2_:nk, :], [], ['wst0b'])
            V('dve', 'tensor_copy', ['wst0b'], ['W4b_%d' % cnt], dstw[:, h2_:nk, half * 512:(half + 1) * 512], w[:, 4:4 + h2_, :])
    W4K = ['W4a_%d' % i for i in range(1, 7)] + ['W4b_%d' % i for i in range(1, 7)]
    yfc = [sb.alloc([128, 4, 512], BF) for _ in range(2)]
    yic = [sb.alloc([128, 8, 512], BF) for _ in range(2)]
    sac = [sb.alloc([128, 8, 512], BF) for _ in range(2)]
    sbc = [sb.alloc([128, 8, 512], BF) for _ in range(2)]
    mg = sb.alloc([128, 8, 512], BF)
    tm = [sb.alloc([128, 512], BF) for _ in range(4)]
    yg = sb.alloc([128, 8, 512], BF)
    xts = [sb.alloc([128, D], F32) for _ in range(3)]
    pts = [sb.alloc([128, D], F32) for _ in range(2)]
    xnb = [sb.alloc([128, D], BF) for _ in range(2)]
    x1b = [sb.alloc([128, D], BF) for _ in range(3)]
    h2T = [sb.alloc([128, 8, 128], BF) for _ in range(2)]
    junk = sb.alloc([128, D], BF)
    sss = [sb.alloc([128, 1], F32) for _ in range(3)]
    LGP = PS[3][:, 512:1024]
    lgk = 'ps7'
    pst4 = {'h': 0}

    def ps_half6():
        i = pst4['h'] % 6
        pst4['h'] += 1
        return PS[i // 2][:, (i % 2) * 512:(i % 2) * 512 + 512], 'ps%d' % i

    def ps_full3():
        i = pst4['h'] % 6
        if i % 2:
            pst4['h'] += 1
            i = pst4['h'] % 6
        pst4['h'] += 2
        return PS[i // 2][:, :], ['ps%d' % i, 'ps%d' % (i + 1)]

    ygs = [yg, sb.alloc([128, 8, 512], BF)]

    def A_steps(tc):
        b = tc % 2
        t0 = tc * 512
        ygc = ygs[tc % 2]
        steps = []

        def ld():
            dma('sp', yfc[b][:], yf_d[:, t0:t0 + 512].rearrange("(g p) t -> p g t", p=128), [], ['yfc%d' % b])
            dma('sp', yic[b][:], yin_d[:, t0:t0 + 512].rearrange("(g p) t -> p g t", p=128), [], ['yic%d' % b])
            dma('sp', sac[b][:], sa_d[:, t0:t0 + 512].rearrange("(g p) t -> p g t", p=128), [], ['sac%d' % b])
            dma('sp', sbc[b][:], sb_d[:, t0:t0 + 512].rearrange("(g p) t -> p g t", p=128), [], ['sbc%d' % b])
        steps.append(ld)
        for ec in range(8):
            def f(ec=ec):
                pf, pfk = ps_half6()
                mm(pf, [(WF[:, g, ec * 128:(ec + 1) * 128], yfc[b][:, g, :]) for g in range(4)], W4K + ['yfc%d' % b], [pfk])
                pr, prk = ps_half6()
                mm(pr, [(WL[:, k, ec * 128:(ec + 1) * 128], yic[b][:, k, :]) for k in range(8)], W4K + ['yic%d' % b], [prk])
                t1 = tm[(ec % 2) * 2]; t2 = tm[(ec % 2) * 2 + 1]; k1_ = 'tm%d' % ((ec % 2) * 2); k2_ = 'tm%d' % ((ec % 2) * 2 + 1)
                V('dve', 'tensor_tensor', [pfk, 'sac%d' % b], [k1_], t1[:], pf, sac[b][:, ec, :], ALU.mult)
                V('dve', 'tensor_tensor', [prk, 'sbc%d' % b], [k2_], t2[:], pr, sbc[b][:, ec, :], ALU.mult)
                V('dve', 'tensor_tensor', [k1_, k2_], ['mg%d' % ec], mg[:, ec, :], t1[:], t2[:], ALU.add)
            steps.append(f)
        for ec in range(8):
            def g_(ec=ec):
                po, pok = ps_half6()
                mm(po, [(WO[:, k, ec * 128:(ec + 1) * 128], mg[:, k, :]) for k in range(8)], W4K + ['mg%d' % k for k in range(8)], [pok])
                act(ygc[:, ec, :], po, AF.Identity, [pok, 'ADA', 'zcol'], ['yg%d_%d' % (tc % 2, ec)], scale=ada(2, ec), bias=zcol[:])
            steps.append(g_)
        return steps

    def T_steps(tc):
        ygc = ygs[tc % 2]
        hold = {}

        def L(j):
            ti = tc * 4 + j
            if ti >= 32:
                return
            bb = ti % 2; b3 = ti % 3
            xt = xts[b3]; xk = 'xt%d' % b3; pt = pts[bb]; pk_ = 'pt%d' % bb
            dma('sp', xt[:], x_d[ti * 128:(ti + 1) * 128, :], [], [xk])
            dma('sp', pt[:], pos_d[ti * 128:(ti + 1) * 128, :], [], [pk_])
            V('pool', 'tensor_tensor', [xk, pk_], [xk], xt[:], xt[:], pt[:], ALU.add)

        def T1(j):
            ti = tc * 4 + j
            bb = ti % 2; b3 = ti % 3
            xt = xts[b3]; xk = 'xt%d' % b3
            if ti == 0:
                L(0)
            L(j + 1)
            ph_, phk = ps_half6()
            pfb = ph_.bitcast(BF)
            for k in range(8):
                tr(pfb[:, k * 128:(k + 1) * 128], ygc[:, k, j * 128:(j + 1) * 128], identb[:], ['yg%d_%d' % (tc % 2, k), 'identb'], [phk])
            V('dve', 'tensor_tensor', [xk, phk], [xk], xt[:], xt[:], pfb, ALU.add)
            act(x1b[b3][:], xt[:], AF.Copy, [xk], ['x1b%d' % b3])
            dma('pool', x1_d[ti * 128:(ti + 1) * 128, :], x1b[b3][:], ['x1b%d' % b3], ['x1_d'])
            ss = sss[b3]; sk = 'ss%d' % b3
            rms_tile(xt, xk, ss, sk, junk)
            V('dve', 'tensor_scalar', [xk, sk], ['xnb%d' % bb], xnb[bb][:], xt[:], ss[:, 0:1], None, ALU.mult)
            dma('pool', xn2_d[ti * 128:(ti + 1) * 128, :], xnb[bb][:], ['xnb%d' % bb], ['xn2_d'])

        def T2(j):
            ti = tc * 4 + j
            bb = ti % 2
            ph_a, phk_a = ps_half6()
            ph_b, phk_b = ps_half6()
            pfa = ph_a.bitcast(BF); pfb2 = ph_b.bitcast(BF)
            for k in range(8):
                dstp = pfa if k < 4 else pfb2
                tr(dstp[:, (k % 4) * 128:(k % 4 + 1) * 128], xnb[bb][:, k * 128:(k + 1) * 128], identb[:], ['xnb%d' % bb, 'identb'], [phk_a if k < 4 else phk_b])
            for k in range(8):
                if k < 4:
                    act(h2T[bb][:, k, :], pfa[:, (k % 4) * 128:(k % 4 + 1) * 128], AF.Identity, [phk_a, 'scale2', 'ADA'], ['h2Ta%d' % bb],
                        scale=scale2[:, k:k + 1], bias=ada(3, k))
                else:
                    V('dve', 'tensor_scalar', [phk_b, 'scale2', 'ADA'], ['h2Tb%d' % bb], h2T[bb][:, k, :], pfb2[:, (k % 4) * 128:(k % 4 + 1) * 128],
                      scale2[:, k:k + 1], ada(3, k), ALU.mult, ALU.add)

        def R(j):
            ti = tc * 4 + j
            bb = ti % 2
            mm(LGP[:, ti * 16:(ti + 1) * 16], [(h2T[bb][:, k, :], WR[:, k, :]) for k in range(8)], ['h2Ta%d' % bb, 'h2Tb%d' % bb, 'WR'], [lgk])

        order = [(T1, 0), (T1, 1), (T2, 0), (T1, 2), (R, 0), (T2, 1), (T1, 3), (R, 1), (T2, 2), (R, 2), (T2, 3), (R, 3)]
        return [(lambda f=f, j=j: f(j)) for (f, j) in order]

    for st in A_steps(0):
        st()
    for tc in range(8):
        a_ = A_steps(tc + 1) if tc + 1 < 8 else []
        t_ = T_steps(tc)
        ia = it_ = 0
        while ia < len(a_) or it_ < len(t_):
            for _ in range(3):
                if ia < len(a_):
                    a_[ia](); ia += 1
            for _ in range(2):
                if it_ < len(t_):
                    t_[it_](); it_ += 1
    mx = sb.alloc([128, 32], F32)
    lg3 = LGP.rearrange("p (j e) -> p j e", e=16)
    V('dve', 'tensor_reduce', [lgk], ['mx'], mx[:], lg3, AX.X, ALU.max)
    V('dve', 'tensor_tensor', [lgk, 'mx'], ['Sc'], Sc[:], lg3, mx[:].unsqueeze(2).to_broadcast([128, 32, 16]), ALU.subtract)
    act(Sc[:], Sc[:], AF.Exp, ['Sc'], ['Sc'])
    V('dve', 'tensor_reduce', ['Sc'], ['mx'], mx[:], Sc[:], AX.X, ALU.add)
    V('dve', 'reciprocal', ['mx'], ['mx'], mx[:], mx[:])
    V('dve', 'tensor_tensor', ['Sc', 'mx'], ['Sc'], Sc[:], Sc[:], mx[:].unsqueeze(2).to_broadcast([128, 32, 16]), ALU.mult)
    dma('pool', sc_d.rearrange("(p j) e -> p (j e)", p=128), Sc[:].rearrange("p j e -> p (j e)"), ['Sc'], ['sc_d'])
    for q in range(4):
        pf, pfk = ps_full3()
        for jj in range(8):
            j = q * 8 + jj
            tr(pf[0:16, jj * 128:(jj + 1) * 128], Sc[:, j, :], identf[:], ['Sc', 'identf'], [pfk[jj // 4]])
        V('dve', 'tensor_copy', pfk, ['S'], S[0:16, q * 1024:(q + 1) * 1024], pf[0:16, :])
    P.barrier()
    sb.reset(m4)
    if stop_after <= 4:
        return finish(nc, es, P)

    S = sb.alloc([128, T], F32)
    zt = sb.alloc([128, 2048], F32)
    V('pool', 'memset', [], ['zt'], zt[:], 0.0)
    for i in range(16):
        dma('pool', y_d[i * 256:(i + 1) * 256, :].rearrange("(p r) c -> p (r c)", p=128), zt[:], ['zt'], ['y_d'])
    lo = sb.alloc([128, 1], F32); mid = sb.alloc([128, 1], F32); cntt = sb.alloc([128, 1], F32); ge = sb.alloc([128, 1], F32)
    jk = sb.alloc([128, T], BF)
    V('dve', 'memset', [], ['lo'], lo[0:16, :], 0.0)
    for it in range(30):
        half = 2.0 ** (-(it + 1))
        V('dve', 'tensor_scalar', ['lo'], ['mid'], mid[0:16, :], lo[0:16, :], half, None, ALU.add)
        V('dve', 'tensor_scalar', ['S', 'mid'], ['jk', 'cnt'], jk[0:16, :], S[0:16, :], mid[0:16, 0:1], None, ALU.is_ge, ALU.add, cntt[0:16, :])
        V('dve', 'tensor_scalar', ['cnt'], ['ge'], ge[0:16, :], cntt[0:16, :], float(CAP), half, ALU.is_ge, ALU.mult)
        V('dve', 'tensor_tensor', ['ge', 'lo'], ['lo'], lo[0:16, :], lo[0:16, :], ge[0:16, :], ALU.add)
    Mk = sb.alloc([128, T], F32); Cs = sb.alloc([128, T], F32)
    V('dve', 'tensor_scalar', ['S', 'lo'], ['Mk'], Mk[0:16, :], S[0:16, :], lo[0:16, 0:1], None, ALU.is_ge)
    ones = sb.alloc([128, T], BF)
    V('pool', 'memset', [], ['ones'], ones[0:16, :], 1.0)
    V('dve', 'tensor_tensor_scan', ['Mk', 'ones'], ['Cs'], Cs[0:16, :], ones[0:16, :], Mk[0:16, :], 0.0, ALU.mult, ALU.add)
    dma('sp', cs_d, Cs[0:16, :], ['Cs'], ['cs_d'])
    dma('sp', m_d, Mk[0:16, :], ['Mk'], ['m_d'])
    CSJ = sb.alloc([128, 8, 132], F32); M4 = sb.alloc([128, 8, 128], F32)
    V('pool', 'memset', [], ['CSJ'], CSJ[:], 0.0)
    iotac = sb.alloc([128, 512], F32); cval = sb.alloc([128, 4], F32)
    dma('sp', CSJ[0:64, :, 0:128], cs_d.rearrange("e (j p) -> (e j) p", p=128).rearrange("(q r) p -> r q p", r=64), ['cs_d', 'CSJ'], ['CSJ'])
    dma('sp', M4[0:64, :, :], m_d.rearrange("e (j p) -> (e j) p", p=128).rearrange("(q r) p -> r q p", r=64), ['m_d'], ['M4'])
    for q in range(8):
        dma('sp', CSJ[0:64, q, 128:129], jcol_d[0:64, :], ['CSJ'], ['CSJ'])
    dma('sp', iotac[:], iotac_d, [], ['iotac']); dma('sp', cval[:], cval_d, [], ['cval'])
    hi4 = sb.alloc([128, 8], F32); lo4 = sb.alloc([128, 8], F32)
    J4 = sb.alloc([128, 8, 512], F32); tj = sb.alloc([128, 512], F32)
    V('dve', 'tensor_copy', ['CSJ'], ['hi4'], hi4[0:64, :], CSJ[0:64, :, 127])
    V('dve', 'tensor_tensor', ['CSJ', 'M4'], ['lo4'], lo4[0:64, :], CSJ[0:64, :, 0], M4[0:64, :, 0], ALU.subtract)
    for q in range(8):
        V('dve', 'tensor_scalar', ['iotac', 'lo4'], ['tj'], tj[0:64, :], iotac[0:64, :], lo4[0:64, q:q + 1], None, ALU.is_ge)
        V('dve', 'scalar_tensor_tensor', ['iotac', 'hi4', 'tj'], ['J4'], J4[0:64, q, :], iotac[0:64, :], hi4[0:64, q:q + 1], tj[0:64, :], ALU.is_lt, ALU.mult)
    idxf = sb.alloc([128, 64], F32); rr = sb.alloc([128, 64], F32); idx2f = sb.alloc([128, 64], F32)
    jk3 = sb.alloc([128, 128], F32)
    for e in range(NE):
        q = e // 2; r0 = (e % 2) * 32
        for g in range(4):
            col = e * 4 + g
            ph, pk = ps_half6()
            mm(ph[:, 0:130], [(J4[r0:r0 + 32, q, g * 128:(g + 1) * 128], CSJ[r0:r0 + 32, q, 0:130])], ['J4', 'CSJ'], [pk])
            V('dve', 'tensor_scalar', [pk, 'cval'], ['jk3', 'rr'], jk3[:], ph[:, 0:128], cval[:, g:g + 1], None, ALU.is_le, ALU.add, rr[:, col:col + 1])
            V('dve', 'scalar_tensor_tensor', [pk, 'rr'], ['idxf'], idxf[:, col:col + 1], ph[:, 128:129], 128.0, rr[:, col:col + 1], ALU.mult, ALU.add)
            V('dve', 'scalar_tensor_tensor', [pk, 'rr'], ['idx2f'], idx2f[:, col:col + 1], rr[:, col:col + 1], 32.0, ph[:, 128:129], ALU.mult, ALU.add)
    V('dve', 'tensor_copy', ['idxf'], ['idxs'], idxs[:], idxf[:])
    V('dve', 'tensor_copy', ['idx2f'], ['idx2s'], idx2s[:], idx2f[:])
    if debug:
        dma('sp', idx_dbg, idxf[:], ['idxf'], ['idx_dbg'])
    P.barrier()
    sb.reset(m4)
    if stop_after <= 5:
        return finish(nc, es, P)

    m6 = sb.mark()
    wst = [sb.alloc([128, 8, 512], F32) for _ in range(4)]
    wbf = [sb.alloc([128, 8, 512], BF) for _ in range(7)]
    xrow = [sb.alloc([128, D], BF) for _ in range(8)]
    grow = [sb.alloc([128, 16], F32) for _ in range(16)]
    xgT = [sb.alloc([128, 8, 512], BF) for _ in range(2)]
    hid = sb.alloc([128, 12, 512], BF)
    sgt = [sb.alloc([128, 512], BF) for _ in range(2)]
    og = sb.alloc([128, 8, 512], F32)
    orow = [sb.alloc([128, D], F32) for _ in range(4)]
    pieces = []
    for e in range(NE):
        for fq in range(3):
            pieces.append((wg_d[e, :, fq * 512:(fq + 1) * 512].rearrange("(k p) f -> p k f", p=128), 8))
            pieces.append((wu_d[e, :, fq * 512:(fq + 1) * 512].rearrange("(k p) f -> p k f", p=128), 8))
        for dq in range(3):
            pieces.append((wd_d[e, dq * 512:(dq + 1) * 512, :].rearrange("(k p) c -> p k c", p=128), 4))
    loaded = {}
    wctr = {'dma': 0, 'cast': 0}
    LOOK_DMA = 6
    LOOK_CAST = 3

    def ensure_dma(upto):
        while wctr['dma'] <= min(upto, len(pieces) - 1):
            n = wctr['dma']; wctr['dma'] += 1
            src_ap, a = pieces[n]
            i = n % 4
            dma('sp', wst[i][:].rearrange("p a b -> p (a b)").rearrange("p (a b) -> p a b", a=a), src_ap, [], ['wst%d' % i])

    def ensure_cast(upto):
        while wctr['cast'] <= min(upto, len(pieces) - 1):
            n = wctr['cast']; wctr['cast'] += 1
            ensure_dma(n)
            src_ap, a = pieces[n]
            i = n % 4; jj = n % 7
            w = wst[i]; wb = wbf[jj]
            wv = w[:].rearrange("p a b -> p (a b)"); wbv = wb[:].rearrange("p a b -> p (a b)")
            if n % 2 == 0:
                act(wbv, wv, AF.Copy, ['wst%d' % i], ['wbf%d' % jj])
            else:
                V('dve', 'tensor_copy', ['wst%d' % i], ['wbf%d' % jj], wbv, wv)
            loaded[n] = (wb[:].rearrange("p a b -> p (a b)").rearrange("p (a b) -> p a b", a=a), 'wbf%d' % jj)

    def ensure_loaded(upto):
        ensure_cast(upto)

    def get_piece(n):
        ensure_cast(n + LOOK_CAST)
        ensure_dma(n + LOOK_DMA)
        return loaded[n]

    def gathers(e):
        for g in range(4):
            s = (e % 2) * 4 + g
            s3 = (e % 4) * 4 + g
            col = e * 4 + g
            P.op('pool', lambda en, s=s, col=col: en.indirect_dma_start(
                out=xrow[s][:, :], out_offset=None, in_=xn2_d[:, :],
                in_offset=bass.IndirectOffsetOnAxis(ap=idxs[:, col:col + 1], axis=0)), ['idxs', 'xn2_d'], ['xrow%d' % s], dma=True)
            P.op('pool', lambda en, s3=s3, col=col: en.indirect_dma_start(
                out=grow[s3][:, :], out_offset=None, in_=sc_d[:, :],
                in_offset=bass.IndirectOffsetOnAxis(ap=idx2s[:, col:col + 1], axis=0)), ['idx2s', 'sc_d'], ['grow%d' % s3], dma=True)

    def build_xgT(e):
        xg = xgT[e % 2]; xgk = 'xgT%d' % (e % 2)
        for g in range(4):
            s = (e % 2) * 4 + g
            ph, pk = ps_half()
            phb = ph.bitcast(BF)
            for k in range(8):
                tr(phb[:, k * 128:(k + 1) * 128], xrow[s][:, k * 128:(k + 1) * 128], identb[:], ['xrow%d' % s, 'identb'], [pk])
            for k in range(8):
                act(xg[:, k, g * 128:(g + 1) * 128], phb[:, k * 128:(k + 1) * 128], AF.Identity, [pk, 'scale2', 'ADA'], [xgk],
                    scale=scale2[:, k:k + 1], bias=ada(3, k))

    def outT(e):
        for g in range(4):
            s3 = (e % 4) * 4 + g
            col = e * 4 + g
            pf, pfk = ps_full()
            for k in range(8):
                tr(pf[:, k * 128:(k + 1) * 128], og[:, k, g * 128:(g + 1) * 128], identf[:], ['og%d' % k, 'identf'], [pfk[k // 4]])
            orw = orow[g]; ork = 'orow%d' % g
            V('dve', 'tensor_scalar', pfk + ['grow%d' % s3], [ork], orw[:], pf, grow[s3][:, e:e + 1], None, ALU.mult)
            P.op('pool', lambda en, orw=orw, col=col: en.indirect_dma_start(
                out=y_d[:, :], out_offset=bass.IndirectOffsetOnAxis(ap=idxs[:, col:col + 1], axis=0),
                in_=orw[:, :], in_offset=None, compute_op=ALU.add), [ork, 'idxs'], ['y_d'], dma=True)

    ensure_dma(3)
    ensure_cast(3)
    gathers(0)
    gathers(1)
    build_xgT(0)
    for e in range(NE):
        if e + 2 < NE:
            gathers(e + 2)
        xg = xgT[e % 2]; xgk = 'xgT%d' % (e % 2)
        for fq in range(3):
            wgb, wgk = get_piece(e * 9 + fq * 2)
            wub, wuk = get_piece(e * 9 + fq * 2 + 1)
            for fc in range(4):
                f = fq * 4 + fc
                pg, pgk = ps_half()
                mm(pg, [(wgb[:, k, fc * 128:(fc + 1) * 128], xg[:, k, :]) for k in range(8)], [wgk, xgk], [pgk])
                pu, puk = ps_half()
                mm(pu, [(wub[:, k, fc * 128:(fc + 1) * 128], xg[:, k, :]) for k in range(8)], [wuk, xgk], [puk])
                sg_ = sgt[f % 2]; sgk = 'sgt%d' % (f % 2)
                act(sg_[:], pg, AF.Silu, [pgk], [sgk])
                V('dve', 'tensor_tensor', [sgk, puk], ['hid'], hid[:, f, :], sg_[:], pu, ALU.mult)
            if fq == 0 and e >= 1:
                outT(e - 1)
        if e + 1 < NE:
            build_xgT(e + 1)
        wds = [get_piece(e * 9 + 6 + dq) for dq in range(3)]
        for ec in range(8):
            po, pok = ps_half()
            mm(po, [(wds[f // 4][0][:, f % 4, ec * 128:(ec + 1) * 128], hid[:, f, :]) for f in range(12)],
               [wds[0][1], wds[1][1], wds[2][1], 'hid'], [pok])
            act(og[:, ec, :], po, AF.Identity, [pok, 'ADA', 'zcol'], ['og%d' % ec], scale=ada(5, ec), bias=zcol[:])
    outT(NE - 1)
    P.barrier()
    sb.reset(m6)
    if stop_after <= 6:
        return finish(nc, es, P)

    FG = sb.alloc([128, D], F32)
    dma('sp', FG[:], fg_d.partition_broadcast(128), [], ['FG'])
    xbs = [sb.alloc([128, D], BF) for _ in range(6)]
    yts = [sb.alloc([128, D], F32) for _ in range(6)]
    ots = [sb.alloc([128, D], F32) for _ in range(4)]
    junk = sb.alloc([128, D], BF)
    sss = [sb.alloc([128, 1], F32) for _ in range(6)]
    for ti in range(32):
        b = ti % 6
        xb = xbs[b]; xk = 'xb%d' % b; yt = yts[b]; yk = 'yt%d' % b; ss = sss[b]; sk = 'ss%d' % b
        ot = ots[ti % 4]; ok_ = 'ot%d' % (ti % 4)
        dma('sp', xb[:], x1_d[ti * 128:(ti + 1) * 128, :], ['x1_d'], [xk])
        dma('sp', yt[:], y_d[ti * 128:(ti + 1) * 128, :], ['y_d'], [yk])
        V('dve', 'tensor_tensor', [xk, yk], [yk], yt[:], yt[:], xb[:], ALU.add)
        rms_tile(yt, yk, ss, sk, junk)
        V('dve', 'scalar_tensor_tensor', [yk, sk, 'FG'], [ok_], ot[:], yt[:], ss[:, 0:1], FG[:], ALU.mult, ALU.mult)
        dma('pool', out_d[ti * 128:(ti + 1) * 128, :], ot[:], [ok_], ['out_d'])
    return finish(nc, es, P)


def finish(nc, es, P):
    P.barrier()
    with nc.Block() as block:
        @block.tensor
        def _(e):
            P.emit(e, 'pe')

        @block.scalar
        def _(e):
            P.emit(e, 'act')

        @block.vector
        def _(e):
            P.emit(e, 'dve')

        @block.gpsimd
        def _(e):
            P.emit(e, 'pool')

        @block.sync
        def _(e):
            P.emit(e, 'sp')
    es.close()
    return nc


def _consts():
    bf = ml_dtypes.bfloat16
    n = np.arange(128)
    ang = 2 * np.pi * np.outer(n, n) / 128.0
    cs128 = np.concatenate([np.cos(ang), np.sin(ang)], axis=1)
    t1 = np.arange(64); k1 = np.arange(64)
    e1 = np.zeros((32, 128, 512))
    for m in range(32):
        for t2l in range(2):
            t2 = 2 * m + t2l
            ph = 2 * np.pi * (np.outer(t1, k1) / 64.0 + (k1[None, :] * t2) / 4096.0)
            sl = slice(t2l * 64, t2l * 64 + 64)
            e1[m, sl, 0 + t2l * 64:0 + t2l * 64 + 64] = np.cos(ph)
            e1[m, sl, 128 + t2l * 64:128 + t2l * 64 + 64] = np.sin(ph)
            e1[m, sl, 256 + t2l * 64:256 + t2l * 64 + 64] = -np.sin(ph)
            e1[m, sl, 384 + t2l * 64:384 + t2l * 64 + 64] = np.cos(ph)
    e1 = np.ascontiguousarray(e1.transpose(1, 0, 2).reshape(128, 32 * 512))
    s = 1.0 / math.sqrt(4096.0 * 128.0)
    e2 = np.zeros((128, 256))
    t2 = np.arange(64); k2 = np.arange(64)
    ph = 2 * np.pi * np.outer(t2, k2) / 64.0
    for k1l in range(2):
        e2[k1l * 64:k1l * 64 + 64, k1l * 64:k1l * 64 + 64] = np.cos(ph) * s
        e2[k1l * 64:k1l * 64 + 64, 128 + k1l * 64:128 + k1l * 64 + 64] = -np.sin(ph) * s
    quarter = D // 4
    freqs = np.exp(-math.log(10000.0) * np.arange(quarter, dtype=np.float32) / quarter).astype(np.float32)
    ang_r = np.arange(64, dtype=np.float32)[:, None] * freqs
    emb_r = np.concatenate([np.sin(ang_r), np.cos(ang_r)], axis=-1)
    emb = np.concatenate([np.broadcast_to(emb_r[:, None, :], (64, 64, D // 2)),
                          np.broadcast_to(emb_r[None, :, :], (64, 64, D // 2))], axis=-1).reshape(T, D).astype(np.float32)
    return dict(cs128=cs128.astype(bf), e1=e1.astype(bf), e2=e2.astype(bf), pos=np.ascontiguousarray(emb),
                identf=np.eye(128, dtype=np.float32), identb=np.eye(128).astype(bf),
                iota_c=np.ascontiguousarray(np.broadcast_to(np.arange(512, dtype=np.float32)[None, :], (128, 512))),
                cval=(np.arange(4)[None, :] * 128 + np.arange(128)[:, None]).astype(np.float32),
                jcol=(np.arange(128) % 32).astype(np.float32).reshape(128, 1))


def _fm(v):
    return np.ascontiguousarray(np.asarray(v, np.float32).reshape(8, 128).T)


def make_in_maps(inputs, cores):
    f = lambda a: np.ascontiguousarray(np.asarray(a, np.float32))
    cst = _consts()
    l = 0
    shared = dict(cst)
    shared.update(
        w_ada=f(inputs['w_ada'][l]), b_ada_fm=np.ascontiguousarray(f(inputs['b_ada'][l]).reshape(48, 128).T),
        n1g_fm=_fm(inputs['norm1_g'][l]), n2g_fm=_fm(inputs['norm2_g'][l]), final_g=f(inputs['final_g']).reshape(1, D),
        w_in=f(inputs['w_in'][l]), w_four=f(inputs['w_four'][l]), w_lru=f(inputs['w_lru'][l]), w_out=f(inputs['w_out'][l]),
        conv_w_fm=np.ascontiguousarray(f(inputs['conv_w'][l]).reshape(4, 8, 128).transpose(2, 1, 0).reshape(128, 32)),
        conv_b_fm=_fm(inputs['conv_b'][l]),
        lam_fm=np.ascontiguousarray(f(inputs['lru_lambda'][l]).reshape(2, 8, 128).transpose(2, 0, 1).reshape(128, 16)),
        ba_fm=np.ascontiguousarray(f(inputs['lru_ba'][l]).reshape(2, 8, 128).transpose(2, 0, 1).reshape(128, 16)),
        bi_fm=np.ascontiguousarray(f(inputs['lru_bi'][l]).reshape(2, 8, 128).transpose(2, 0, 1).reshape(128, 16)),
        lru_wa=f(inputs['lru_wa'][l]).reshape(16, 128, 128), lru_wi=f(inputs['lru_wi'][l]).reshape(16, 128, 128),
        w_router_fm=np.ascontiguousarray(f(inputs['w_router'][l]).reshape(8, 128, 16).transpose(1, 0, 2).reshape(128, 128)),
        w_gate_e=f(inputs['w_gate_e'][l]), w_up_e=f(inputs['w_up_e'][l]), w_down_e=f(inputs['w_down_e'][l]),
    )
    cctx = _fm(inputs['c_ctx'])
    maps = []
    for b in cores:
        m = dict(shared)
        m['x'] = f(inputs['x'][b]); m['ctx'] = f(inputs['ctx'][b])
        m['cvec'] = np.ascontiguousarray(np.concatenate([_fm(inputs['c'][b]), cctx], axis=1))
        maps.append(m)
    return maps


def kernel(**inputs):
    nc = build_program()
    maps = make_in_maps(inputs, list(range(8)))
    res = run_bass_kernel_spmd(nc, maps, core_ids=list(range(8)))
    return np.stack([np.asarray(r["out"], np.float32) for r in res.results], axis=0)
```
